# Optimizing a Trainium2 kernel written in Bass

```python
import math
import jax, jax.numpy as jnp
from jax import lax
import numpy as np


D_MODEL = 2048
BATCH = 8
SEQ = 2048
DEPTH = 2

GRID_W = 64
CTX_LEN = 256
HEAD_DIM = 128
D_CONV = 1536
D_SSM = 512
CONV_HEADS = D_CONV // HEAD_DIM
SSM_HEADS = D_SSM // HEAD_DIM
CONV_W = 3
SSM_GROUP = 16
N_SSM_GROUPS = D_SSM // SSM_GROUP
SSM_STATE = 64
D_IN_PROJ = 3 * D_CONV + D_SSM
N_EXPERTS = 16
D_EXPERT = 2048
EC_FACTOR = 2
N_MOD = 6
EPS = 1e-6
DT_MIN = 0.001
DT_MAX = 0.1

kernel_name = 'hybrid_conv_s5_ec_moe_diffusion_trunk'


def rmsnorm(x, g):
    xf = x.astype(jnp.float32)
    y = xf * lax.rsqrt(jnp.mean(xf * xf, axis=-1, keepdims=True) + EPS)
    return (y * g.astype(jnp.float32)).astype(x.dtype)


def head_rmsnorm(y, g, n_heads):
    shp = y.shape
    yf = y.astype(jnp.float32).reshape(shp[:-1] + (n_heads, shp[-1] // n_heads))
    yf = yf * lax.rsqrt(jnp.mean(yf * yf, axis=-1, keepdims=True) + EPS)
    return (yf.reshape(shp) * g.astype(jnp.float32)).astype(y.dtype)


def modulate(h, shift, scale):
    return h * (1.0 + scale) + shift


def centred_conv3(v, w, axis):
    n = v.shape[axis]
    pad = [(0, 0)] * v.ndim
    pad[axis] = (1, 1)
    vp = jnp.pad(v, pad)
    out = w[0] * lax.slice_in_dim(vp, 0, n, axis=axis)
    for k in range(1, CONV_W):
        out = out + w[k] * lax.slice_in_dim(vp, k, k + n, axis=axis)
    return out


def conv_mixer(p, w_conv, on_grid):
    b_g, c_g, v = jnp.split(p, 3, axis=-1)
    z = c_g * v
    if on_grid:
        bsz, n, ch = z.shape
        rows = n // GRID_W
        z = centred_conv3(z.reshape(bsz, rows, GRID_W, ch), w_conv, axis=2).reshape(bsz, n, ch)
    else:
        z = centred_conv3(z, w_conv, axis=1)
    return b_g * z


def _complex_affine_combine(e1, e2):
    ar1, ai1, br1, bi1 = e1
    ar2, ai2, br2, bi2 = e2
    return (ar2 * ar1 - ai2 * ai1,
            ar2 * ai1 + ai2 * ar1,
            ar2 * br1 - ai2 * bi1 + br2,
            ar2 * bi1 + ai2 * br1 + bi2)


def s5_discretise(lam_re, lam_im, log_dt, b_re, b_im):
    lr = lam_re.astype(jnp.float32)
    li = lam_im.astype(jnp.float32)
    dt = jnp.exp(log_dt.astype(jnp.float32))[:, None]
    mag = jnp.exp(lr * dt)
    ar = mag * jnp.cos(li * dt)
    ai = mag * jnp.sin(li * dt)
    den = lr * lr + li * li
    nr = ar - 1.0
    kr = (nr * lr + ai * li) / den
    ki = (ai * lr - nr * li) / den
    br = b_re.astype(jnp.float32)
    bi = b_im.astype(jnp.float32)
    bbr = kr[..., None] * br - ki[..., None] * bi
    bbi = kr[..., None] * bi + ki[..., None] * br
    return dt, lr, li, ar, ai, bbr, bbi


def s5_scan(u, disc, h0, reverse):
    dt, lr, li, ar, ai, bbr, bbi = disc
    if reverse:
        u = jnp.flip(u, axis=1)
    n = u.shape[1]
    bu_re = jnp.einsum('blgh,gph->lbgp', u, bbr)
    bu_im = jnp.einsum('blgh,gph->lbgp', u, bbi)
    a_shape = (n, 1) + ar.shape
    a_re = jnp.broadcast_to(ar, a_shape)
    a_im = jnp.broadcast_to(ai, a_shape)
    _, _, h_re, h_im = lax.associative_scan(_complex_affine_combine, (a_re, a_im, bu_re, bu_im), axis=0)
    if h0 is not None:
        h0r, h0i = h0
        t = jnp.arange(1, n + 1, dtype=jnp.float32)[:, None, None, None]
        pm = jnp.exp(t * dt * lr)
        pr = pm * jnp.cos(t * dt * li)
        pi = pm * jnp.sin(t * dt * li)
        h_re = h_re + pr * h0r[None] - pi * h0i[None]
        h_im = h_im + pr * h0i[None] + pi * h0r[None]
    return (h_re, h_im), (h_re[-1], h_im[-1])


def s5_readout(h, c_re, c_im, reverse):
    h_re, h_im = h
    y = (jnp.einsum('lbgp,ghp->blgh', h_re, c_re.astype(jnp.float32))
         - jnp.einsum('lbgp,ghp->blgh', h_im, c_im.astype(jnp.float32)))
    if reverse:
        y = jnp.flip(y, axis=1)
    return y


def s5_mixer(u_ctx, u_lat, lam_re, lam_im, log_dt, b_re, b_im, c_re, c_im, d_skip, w_glu, ctx_out):
    dtype = u_lat.dtype

    def groups(u):
        return u.astype(jnp.float32).reshape(u.shape[0], u.shape[1], N_SSM_GROUPS, SSM_GROUP)

    uc = groups(u_ctx)
    ul = groups(u_lat)
    d = d_skip.astype(jnp.float32).reshape(N_SSM_GROUPS, SSM_GROUP)
    y_lat = d * ul
    y_ctx = d * uc if ctx_out else None
    for direction in range(2):
        rev = direction == 1
        disc = s5_discretise(lam_re[direction], lam_im[direction], log_dt[direction],
                             b_re[direction], b_im[direction])
        h_c, fin_c = s5_scan(uc, disc, None, rev)
        h_l, _ = s5_scan(ul, disc, fin_c, rev)
        y_lat = y_lat + s5_readout(h_l, c_re[direction], c_im[direction], rev)
        if ctx_out:
            y_ctx = y_ctx + s5_readout(h_c, c_re[direction], c_im[direction], rev)

    def glu(y):
        y = jax.nn.gelu(y.reshape(y.shape[0], y.shape[1], D_SSM))
        return (y * jax.nn.sigmoid(y @ w_glu.astype(jnp.float32))).astype(dtype)

    return glu(y_lat), (glu(y_ctx) if ctx_out else None)


def ec_moe(h, router_w, w_gate, w_up, w_down):
    bsz, n, dm = h.shape
    cap = EC_FACTOR * n // N_EXPERTS
    logits = jnp.einsum('bnd,de->bne', h, router_w).astype(jnp.float32)
    aff = jnp.swapaxes(jax.nn.softmax(logits, axis=-1), 1, 2)
    vals, idx = lax.top_k(aff, cap)
    xs = jax.vmap(lambda hb, ib: hb[ib])(h, idx)
    g = jnp.einsum('becd,edf->becf', xs, w_gate)
    u = jnp.einsum('becd,edf->becf', xs, w_up)
    y = jnp.einsum('becf,efd->becd', jax.nn.silu(g) * u, w_down)
    y = y * vals[..., None].astype(y.dtype)
    return jax.vmap(lambda yb, ib: jnp.zeros((n, dm), yb.dtype).at[ib.reshape(-1)].add(yb.reshape(-1, dm)))(y, idx)


def setup_inputs(seed: int = 0) -> dict:
    key = jax.random.key(seed)
    ks = jax.random.split(key, 32)
    f32 = jnp.float32

    def nrm(k, shape, scale):
        return jax.random.normal(k, shape, f32) * scale

    G, P, H = N_SSM_GROUPS, SSM_STATE, SSM_GROUP
    lam_im_base = jnp.pi * jnp.arange(P, dtype=f32)
    return {
        'x': nrm(ks[0], (BATCH, SEQ, D_MODEL), 1.0),
        'c': nrm(ks[1], (BATCH, D_MODEL), 1.0),
        'ctx': nrm(ks[2], (BATCH, CTX_LEN, D_MODEL), 1.0),
        'c_ctx': nrm(ks[3], (D_MODEL,), 1.0),
        'ada_w': nrm(ks[4], (DEPTH, D_MODEL, N_MOD * D_MODEL), 0.5 * D_MODEL ** -0.5),
        'ada_b': nrm(ks[5], (DEPTH, N_MOD * D_MODEL), 0.02),
        'norm1_g': 1.0 + nrm(ks[6], (DEPTH, D_MODEL), 0.02),
        'w_in': nrm(ks[7], (DEPTH, D_MODEL, D_IN_PROJ), D_MODEL ** -0.5),
        'conv_w': nrm(ks[8], (DEPTH, CONV_W, D_CONV), CONV_W ** -0.5),
        'ssm_lam_re': -0.5 + nrm(ks[9], (DEPTH, 2, G, P), 0.01),
        'ssm_lam_im': lam_im_base + nrm(ks[10], (DEPTH, 2, G, P), 0.01),
        'ssm_log_dt': jax.random.uniform(ks[11], (DEPTH, 2, G), f32, math.log(DT_MIN), math.log(DT_MAX)),
        'ssm_b_re': nrm(ks[12], (DEPTH, 2, G, P, H), (2.0 * H) ** -0.5),
        'ssm_b_im': nrm(ks[13], (DEPTH, 2, G, P, H), (2.0 * H) ** -0.5),
        'ssm_c_re': nrm(ks[14], (DEPTH, 2, G, H, P), (2.0 * P) ** -0.5),
        'ssm_c_im': nrm(ks[15], (DEPTH, 2, G, H, P), (2.0 * P) ** -0.5),
        'ssm_d': nrm(ks[16], (DEPTH, D_SSM), 1.0),
        'ssm_w_glu': nrm(ks[17], (DEPTH, D_SSM, D_SSM), D_SSM ** -0.5),
        'out_norm_conv_g': 1.0 + nrm(ks[18], (DEPTH, D_CONV), 0.02),
        'out_norm_ssm_g': 1.0 + nrm(ks[19], (DEPTH, D_SSM), 0.02),
        'w_out': nrm(ks[20], (DEPTH, D_CONV + D_SSM, D_MODEL), (D_CONV + D_SSM) ** -0.5),
        'norm2_g': 1.0 + nrm(ks[21], (DEPTH, D_MODEL), 0.02),
        'router_w': nrm(ks[22], (DEPTH, D_MODEL, N_EXPERTS), D_MODEL ** -0.5),
        'exp_w_gate': nrm(ks[23], (DEPTH, N_EXPERTS, D_MODEL, D_EXPERT), D_MODEL ** -0.5),
        'exp_w_up': nrm(ks[24], (DEPTH, N_EXPERTS, D_MODEL, D_EXPERT), D_MODEL ** -0.5),
        'exp_w_down': nrm(ks[25], (DEPTH, N_EXPERTS, D_EXPERT, D_MODEL), D_EXPERT ** -0.5),
        'final_norm_g': 1.0 + nrm(ks[26], (D_MODEL,), 0.02),
    }


def reference(x, c, ctx, c_ctx, ada_w, ada_b, norm1_g, w_in, conv_w, ssm_lam_re, ssm_lam_im,
              ssm_log_dt, ssm_b_re, ssm_b_im, ssm_c_re, ssm_c_im, ssm_d, ssm_w_glu,
              out_norm_conv_g, out_norm_ssm_g, w_out, norm2_g, router_w, exp_w_gate, exp_w_up,
              exp_w_down, final_norm_g):
    xl = x
    xc = ctx
    sc = jax.nn.silu(c)
    scc = jax.nn.silu(c_ctx)
    for l in range(DEPTH):
        last = l == DEPTH - 1
        mod_l = (sc @ ada_w[l] + ada_b[l])[:, None, :]
        mod_c = scc @ ada_w[l] + ada_b[l]
        sh1, sc1, g1, sh2, sc2, g2 = jnp.split(mod_l, N_MOD, axis=-1)
        csh1, csc1, cg1, csh2, csc2, cg2 = jnp.split(mod_c, N_MOD, axis=-1)

        h = modulate(rmsnorm(xl, norm1_g[l]), sh1, sc1)
        hc = modulate(rmsnorm(xc, norm1_g[l]), csh1, csc1)
        proj = h @ w_in[l]
        if last:
            u_ctx = hc @ w_in[l][:, 3 * D_CONV:]
        else:
            proj_c = hc @ w_in[l]
            u_ctx = proj_c[..., 3 * D_CONV:]
        conv_l = conv_mixer(proj[..., :3 * D_CONV], conv_w[l], on_grid=True)
        ssm_l, ssm_c = s5_mixer(u_ctx, proj[..., 3 * D_CONV:], ssm_lam_re[l], ssm_lam_im[l], ssm_log_dt[l],
                                ssm_b_re[l], ssm_b_im[l], ssm_c_re[l], ssm_c_im[l], ssm_d[l], ssm_w_glu[l],
                                ctx_out=not last)
        mix_l = jnp.concatenate([head_rmsnorm(conv_l, out_norm_conv_g[l], CONV_HEADS),
                                 head_rmsnorm(ssm_l, out_norm_ssm_g[l], SSM_HEADS)], axis=-1) @ w_out[l]
        xl = xl + g1 * mix_l
        h2 = modulate(rmsnorm(xl, norm2_g[l]), sh2, sc2)
        xl = xl + g2 * ec_moe(h2, router_w[l], exp_w_gate[l], exp_w_up[l], exp_w_down[l])

        if not last:
            conv_c = conv_mixer(proj_c[..., :3 * D_CONV], conv_w[l], on_grid=False)
            mix_c = jnp.concatenate([head_rmsnorm(conv_c, out_norm_conv_g[l], CONV_HEADS),
                                     head_rmsnorm(ssm_c, out_norm_ssm_g[l], SSM_HEADS)], axis=-1) @ w_out[l]
            xc = xc + cg1 * mix_c
            hc2 = modulate(rmsnorm(xc, norm2_g[l]), csh2, csc2)
            xc = xc + cg2 * ec_moe(hc2, router_w[l], exp_w_gate[l], exp_w_up[l], exp_w_down[l])
    return rmsnorm(xl, final_norm_g)
```

```python
import math
import numpy as np
import concourse.bass as bass
import concourse.mybir as mybir
from concourse.bass_utils import run_bass_kernel_spmd

F32 = mybir.dt.float32
BF16 = mybir.dt.bfloat16
U32 = mybir.dt.uint32
I32 = mybir.dt.int32
ALU = mybir.AluOpType
AF = mybir.ActivationFunctionType
AX = mybir.AxisListType

P = 128
D = 2048
KC = 16
NLAT = 2048
NCTX = 256
NTOK = NLAT + NCTX
DEPTH = 2
NE = 16
EPS = 1e-6
TWO_PI = 2.0 * math.pi
PI_LO = 3.1415925
ENGS = ["pe", "act", "dve", "pool", "sp"]
NDMA = 40


class Sched:
    def __init__(self, nc, stack):
        self.nc = nc
        self.sems = []
        self.eng_sem = {}
        for e in ENGS:
            self.eng_sem[e] = len(self.sems)
            self.sems.append(stack.enter_context(nc.semaphore("s_" + e)))
        self.dma_sem = []
        for i in range(NDMA):
            self.dma_sem.append(len(self.sems))
            self.sems.append(stack.enter_context(nc.semaphore("d_%d" % i)))
        self.dma_val = [0] * NDMA
        self.dma_next = 0
        self.cnt = {e: 0 for e in ENGS}
        self.recs = {e: [] for e in ENGS}
        self.seen = {e: {} for e in ENGS}
        self.lastw = {}
        self.rd_eng = {}
        self.rd_dma = {}

    def _need(self, eng, tok, waits):
        semi, val, src, is_dma = tok
        if self.seen[eng].get(semi, 0) >= val:
            return
        if (not is_dma) and src == eng and eng == "pe":
            return
        self.seen[eng][semi] = val
        waits.append((semi, val))

    def op(self, eng, fn, r=(), w=(), dma=False):
        waits = []
        for k in r:
            t = self.lastw.get(k)
            if t is not None:
                self._need(eng, t, waits)
        for k in w:
            t = self.lastw.get(k)
            if t is not None:
                self._need(eng, t, waits)
            for se, t in self.rd_eng.get(k, {}).items():
                self._need(eng, t, waits)
            for t in self.rd_dma.get(k, ()):
                self._need(eng, t, waits)
        if dma:
            slot = self.dma_next % NDMA
            self.dma_next += 1
            semi = self.dma_sem[slot]
            prev = self.dma_val[slot]
            if prev > self.seen[eng].get(semi, 0):
                waits.append((semi, prev))
                self.seen[eng][semi] = prev
            val = prev + 16
            self.dma_val[slot] = val
            tok = (semi, val, eng, True)
        else:
            self.cnt[eng] += 1
            tok = (self.eng_sem[eng], self.cnt[eng], eng, False)
        self.recs[eng].append((waits, fn, tok))
        for k in r:
            if dma:
                self.rd_dma.setdefault(k, []).append(tok)
            else:
                self.rd_eng.setdefault(k, {})[eng] = tok
        for k in w:
            self.lastw[k] = tok
            self.rd_eng[k] = {}
            self.rd_dma[k] = []
        return tok

    def barrier(self):
        toks = [(self.eng_sem[e], self.cnt[e], e, False) for e in ENGS if self.cnt[e] > 0]
        toks += [(self.dma_sem[i], self.dma_val[i], None, True) for i in range(NDMA) if self.dma_val[i] > 0]
        for e in ENGS:
            waits = []
            for t in toks:
                if t[2] == e and not t[3]:
                    continue
                if self.seen[e].get(t[0], 0) >= t[1]:
                    continue
                self.seen[e][t[0]] = t[1]
                waits.append((t[0], t[1]))
            if waits:
                self.recs[e].append((waits, None, None))
        self.lastw.clear()
        self.rd_eng.clear()
        self.rd_dma.clear()

    def emit(self):
        nc = self.nc
        sems = self.sems
        recs = self.recs

        def mk(name):
            def body(e):
                for waits, fn, tok in recs[name]:
                    for (s, v) in waits:
                        e.wait_ge(sems[s], v)
                    if fn is not None:
                        ins = fn(e)
                        ins.then_inc(sems[tok[0]], 16 if tok[3] else 1)
            return body

        with nc.Block() as block:
            block.tensor(mk("pe"))
            block.scalar(mk("act"))
            block.vector(mk("dve"))
            block.gpsimd(mk("pool"))
            block.sync(mk("sp"))


class Arena:
    def __init__(self, ap, ncols):
        self.ap = ap
        self.n = ncols
        self.off = 0

    def mark(self):
        return self.off

    def release(self, m):
        self.off = m

    def f32(self, cols):
        cols = (cols + 1) // 2 * 2
        assert self.off + cols <= self.n, ("SBUF arena overflow", self.off, cols, self.n)
        a = self.ap[:, self.off:self.off + cols]
        self.off += cols
        return a

    def bf16(self, cols):
        return self.f32((cols + 1) // 2).bitcast(BF16)[:, 0:cols]

    def u32(self, cols):
        return self.f32(cols).bitcast(U32)

    def i32(self, cols):
        return self.f32(cols).bitcast(I32)


def v3(ap, b):
    return ap.rearrange("p (a b) -> p a b", b=b)


def build_program(dbg=None, stop_after=None, n_layers=DEPTH, only=None):
    from contextlib import ExitStack
    dbg = dbg or []
    nc = bass.Bass("TRN2", target_bir_lowering=False)

    def din(name, shape, dt=F32):
        if only == "b2" and int(np.prod(shape)) > 4_000_000:
            shape = [2, 2]
        return nc.dram_tensor(name, list(shape), dt, kind="ExternalInput").ap()

    x_in = din("x", [NLAT, D])
    ctx_in = din("ctx", [NCTX, D])
    ccol = din("ccol", [P, 32])
    ada_w = din("ada_w", [DEPTH, D, 6 * D])
    ada_b2 = din("ada_b2", [DEPTH, 2, 6 * D])
    norm1_g = din("norm1_g", [DEPTH, D])
    norm2_g = din("norm2_g", [DEPTH, D])
    final_g = din("final_g", [1, D])
    w_in_t = din("w_in_t", [DEPTH, 40, P, KC, P])
    conv_wc = din("conv_wc", [DEPTH, P, 36])
    lamre = din("lamre", [DEPTH, 2, P, 16])
    lamim = din("lamim", [DEPTH, 2, P, 16])
    logdt = din("logdt", [DEPTH, 2, P, 16])
    bt_re = din("bt_re", [DEPTH, 2, 4, P, 4 * P])
    bt_im = din("bt_im", [DEPTH, 2, 4, P, 4 * P])
    ct_re = din("ct_re", [DEPTH, 2, 4, P, 4 * P])
    ct_im = din("ct_im", [DEPTH, 2, 4, P, 4 * P])
    dcol = din("dcol", [DEPTH, P, 4])
    w_glu = din("w_glu", [DEPTH, 512, 512])
    gconv = din("gconv", [DEPTH, P, 12])
    gssm = din("gssm", [DEPTH, P, 4])
    w_out = din("w_out", [DEPTH, D, D])
    router_w = din("router_w", [DEPTH, D, NE])
    small_exp = stop_after is not None and stop_after[0] != "c2"
    if small_exp:
        w_gate = w_up = w_down = None
    else:
        w_gate = din("w_gate", [DEPTH, NE, D, D])
        w_up = din("w_up", [DEPTH, NE, D, D])
        w_down = din("w_down", [DEPTH, NE, D, D])
    out = nc.dram_tensor("out", [NLAT, D], F32, kind="ExternalOutput").ap()

    def dscr(name, shape, dt):
        kind = "ExternalOutput" if name in dbg else "Internal"
        return nc.dram_tensor(name, list(shape), dt, kind=kind).ap()

    xl = dscr("xl", [NLAT, D], F32)
    xc = dscr("xc", [NCTX, D], F32)
    h2t = dscr("h2t", [NLAT, D], BF16)
    h2c = dscr("h2c", [NCTX, D], BF16)
    modrow = dscr("modrow", [2, 6 * D], F32)
    mixn = dscr("mixn", [16, P, NTOK], BF16)
    dbg_aff = dscr("dbg_aff", [NE, NTOK], F32)
    dbg_y = dscr("dbg_y", [4, P, NTOK], F32)

    stack = ExitStack()
    NCOLS = 44800
    arena_t = stack.enter_context(nc.sbuf_tensor("arena", [P, NCOLS], F32))
    ps_t = stack.enter_context(nc.psum_tensor("ps", [P, 4096], F32))
    S = Sched(nc, stack)
    A = Arena(arena_t, NCOLS)
    op = S.op

    def bank(i):
        return ps_t[:, i * 512:(i + 1) * 512]

    def bank16(i):
        return ps_t[:, i * 512:(i + 1) * 512].bitcast(BF16)

    def PSK(i):
        return ("ps", i)

    ident_f = A.f32(P)
    ident_b = A.bf16(P)
    ones_b = A.bf16(P)
    iota_i = A.i32(P)
    iota_t = A.f32(P)
    op("pool", lambda e: e.iota(iota_i, [[1, P]], base=0, channel_multiplier=-1), w=["iota_i"])
    op("dve", lambda e: e.tensor_single_scalar(ident_f, iota_i, 0, ALU.is_equal), r=["iota_i"], w=["ident_f"])
    op("dve", lambda e: e.tensor_copy(ident_b, ident_f), r=["ident_f"], w=["ident_b"])
    op("dve", lambda e: e.memset(ones_b, 1.0), w=["ones_b"])
    op("pool", lambda e: e.iota(iota_i, [[1, P]], base=0, channel_multiplier=0), r=["ident_f"], w=["iota_i"])
    op("dve", lambda e: e.tensor_copy(iota_t, iota_i), r=["iota_i"], w=["iota_t"])
    S.barrier()
    base_mark = A.mark()

    def dma(q, out_ap, in_ap, r=(), w=()):
        return op(q, lambda e: e.dma_start(out=out_ap, in_=in_ap), r=r, w=w, dma=True)

    def bc_row(dst, src_row, key):
        dma("sp", dst, src_row.partition_broadcast(P), w=[key])

    def rstd_from_ss(ss, n, tmp1, tmp2, rstd, keyp):
        op("dve", lambda e: e.tensor_scalar(tmp1, ss, 1.0 / n, EPS, ALU.mult, ALU.add), r=[keyp + "ss"], w=[keyp + "t1"])
        op("act", lambda e: e.activation(tmp2, tmp1, AF.Sqrt), r=[keyp + "t1"], w=[keyp + "t2"])
        op("dve", lambda e: e.reciprocal(rstd, tmp2), r=[keyp + "t2"], w=[keyp + "rstd"])

    def tok_src(l, t):
        if t < 2:
            src = ctx_in if l == 0 else xc
            return src[t * P:(t + 1) * P, :]
        src = x_in if l == 0 else xl
        return src[(t - 2) * P:(t - 1) * P, :]

    def tok_dst(t):
        if t < 2:
            return xc[t * P:(t + 1) * P, :]
        return xl[(t - 2) * P:(t - 1) * P, :]

    def phase_ada(l):
        m = A.mark()
        cc = A.f32(32)
        scT = A.bf16(32)
        wsl = [v3(A.bf16(KC * 512), 512) for _ in range(2)]
        bsl = [A.f32(512) for _ in range(2)]
        msl = [A.f32(512) for _ in range(2)]
        dma("sp", cc, ccol, w=["cc"])
        op("act", lambda e: e.activation(scT, cc, AF.Silu), r=["cc"], w=["scT"])
        scT3 = v3(scT, 2)
        awv = ada_w[l].rearrange("(k p) n -> p k n", p=P)
        for n in range(24):
            s = n % 2
            cs = slice(n * 512, (n + 1) * 512)
            dma("pool", wsl[s], awv[:, :, cs], w=[("aw", s)])
            dma("sp", bsl[s][0:2, :], ada_b2[l][:, cs], w=[("ab", s)])
            for k in range(KC):
                op("pe", lambda e, k=k, s=s: e.matmul(bank(s)[0:2, :], scT3[:, k, :], wsl[s][:, k, :], start=(k == 0), stop=(k == KC - 1)),
                   r=["scT", ("aw", s)], w=[PSK(s)])
            op("dve", lambda e, s=s: e.tensor_tensor(msl[s][0:2, :], bank(s)[0:2, :], bsl[s][0:2, :], ALU.add),
               r=[PSK(s), ("ab", s)], w=[("ms", s)])
            dma("sp", modrow[:, cs], msl[s][0:2, :], r=[("ms", s)])
        S.barrier()
        A.release(m)

    def load_mod_bc(dst, r, seg, key):
        bc_row(dst, modrow[r:r + 1, seg * D:(seg + 1) * D], key)

    def phase_b0(l, ctx_any, hT):
        m = A.mark()
        gm = [A.f32(D) for _ in range(2)]
        sh = [A.f32(D) for _ in range(2)]
        xs = [A.f32(D) for _ in range(2)]
        hn = A.f32(D)
        gtmp = hn
        bc_row(gtmp, norm1_g[l:l + 1, :], "hn")
        for r in range(2):
            load_mod_bc(gm[r], r, 1, ("gm", r))
            load_mod_bc(sh[r], r, 0, ("sh", r))
            op("dve", lambda e, r=r: e.scalar_tensor_tensor(gm[r], gm[r], 1.0, gtmp, ALU.add, ALU.mult),
               r=["hn", ("gm", r)], w=[("gm", r)])
        hb = [A.bf16(D) for _ in range(2)]
        junk = hn.bitcast(BF16)[:, 0:D]
        ss = A.f32(2); t1 = A.f32(2); t2 = A.f32(2); rstd = A.f32(2)
        for t in range(NTOK // P):
            s = t % 2
            r = 1 if t < 2 else 0
            dma("sp", xs[s], tok_src(l, t), w=[("x", s)])
            op("act", lambda e, s=s: e.activation(junk, xs[s], AF.Square, accum_out=ss[:, 0:1]), r=[("x", s)], w=["hn", "b0ss"])
            rstd_from_ss(ss[:, 0:1], D, t1[:, 0:1], t2[:, 0:1], rstd[:, 0:1], "b0")
            op("dve", lambda e, s=s, r=r: e.scalar_tensor_tensor(hn, xs[s], rstd[:, 0:1], gm[r], ALU.mult, ALU.mult),
               r=[("x", s), "b0rstd", ("gm", r)], w=["hn"])
            op("pool", lambda e, s=s, r=r: e.tensor_tensor(hb[s], hn, sh[r], ALU.add), r=["hn", ("sh", r)], w=[("hb", s)])
            for k4 in range(4):
                bk = (t * 4 + k4) % 8
                for kk in range(4):
                    k = k4 * 4 + kk
                    op("pe", lambda e, s=s, k=k, kk=kk, bk=bk: e.transpose(bank16(bk)[:, kk * P:(kk + 1) * P], hb[s][:, k * P:(k + 1) * P], ident_b),
                       r=[("hb", s), "ident_b"], w=[PSK(bk)])
                eng = "act" if k4 % 2 == 0 else "dve"
                if eng == "act":
                    op("act", lambda e, k4=k4, bk=bk, t=t: e.copy(hT[:, k4 * 4:k4 * 4 + 4, t * P:(t + 1) * P], v3(bank16(bk)[:, 0:512], P)),
                       r=[PSK(bk)], w=[("hT", t)])
                else:
                    op("dve", lambda e, k4=k4, bk=bk, t=t: e.tensor_copy(hT[:, k4 * 4:k4 * 4 + 4, t * P:(t + 1) * P], v3(bank16(bk)[:, 0:512], P)),
                       r=[PSK(bk)], w=[("hT", t)])
        S.barrier()
        A.release(m)

    def tiles_for(ctx_on):
        tl = []
        if ctx_on:
            tl.append((0, 256, 256))
        for i in range(4):
            tl.append((256 + 512 * i, 512, 64))
        return tl

    def headnorm_store(y, W, gcol, chunk, tok0, bufs, it):
        sq, ms, sd, yn = bufs
        pb = 3 + 4 * (it % 2)
        kp = "hn%d" % (it % 2)
        op("act", lambda e: e.activation(sq[:, 0:W], y[:, 0:W], AF.Square), r=[kp + "y"], w=[kp + "sq"])
        op("pe", lambda e: e.matmul(bank(pb)[:, 0:W], ones_b, sq[:, 0:W], start=True, stop=True), r=[kp + "sq", "ones_b"], w=[PSK(pb)])
        op("dve", lambda e: e.tensor_scalar(ms[:, 0:W], bank(pb)[:, 0:W], 1.0 / P, EPS, ALU.mult, ALU.add), r=[PSK(pb)], w=[kp + "ms"])
        op("act", lambda e: e.activation(sd[:, 0:W], ms[:, 0:W], AF.Sqrt), r=[kp + "ms"], w=[kp + "sd"])
        op("dve", lambda e: e.reciprocal(ms[:, 0:W], sd[:, 0:W]), r=[kp + "sd"], w=[kp + "ms"])
        op("dve", lambda e: e.scalar_tensor_tensor(yn[:, 0:W], y[:, 0:W], gcol, ms[:, 0:W], ALU.mult, ALU.mult),
           r=[kp + "y", kp + "ms", "gcols"], w=[kp + "yn"])
        dma("sp", mixn[chunk][:, tok0:tok0 + W], yn[:, 0:W], r=[kp + "yn"])

    def phase_b1(l, ctx_full, hT, uT):
        m = A.mark()
        cw = A.f32(36)
        gc = A.f32(12)
        dma("sp", cw, conv_wc[l], w=["cw"])
        dma("sp", gc, gconv[l], w=["gcols"])
        cw3 = v3(cw, 3)
        wsl = [[v3(A.bf16(KC * P), P) for _ in range(3)] for _ in range(2)]
        tmp = []
        for i in range(2):
            tmp.append(dict(c=A.f32(512), z=A.f32(512), acc=A.f32(512), y=A.f32(512),
                            sq=A.bf16(512), ms=A.f32(512), sd=A.f32(512), yn=A.bf16(512)))
        it = 0
        for k in range(12):
            ws = wsl[k % 2]
            for j in range(3):
                dma("pool", ws[j], w_in_t[l, j * 12 + k], w=[("win", k % 2, j)])
            for (tok0, W, RW) in tiles_for(ctx_full):
                T = tmp[it % 2]
                b0 = 4 * (it % 2)
                kp = "hn%d" % (it % 2)
                for j in range(3):
                    for kk in range(KC):
                        op("pe", lambda e, j=j, kk=kk, b0=b0, ws=ws, tok0=tok0, W=W: e.matmul(
                            bank(b0 + j)[:, 0:W], ws[j][:, kk, :], hT[:, kk, tok0:tok0 + W], start=(kk == 0), stop=(kk == KC - 1)),
                           r=[("win", k % 2, j)] + [("hT", tt) for tt in range(tok0 // P, (tok0 + W) // P)], w=[PSK(b0 + j)])
                op("act", lambda e, T=T, b0=b0, W=W: e.copy(T["c"][:, 0:W], bank(b0 + 1)[:, 0:W]), r=[PSK(b0 + 1)], w=[kp + "c"])
                op("dve", lambda e, T=T, b0=b0, W=W: e.tensor_tensor(T["z"][:, 0:W], bank(b0 + 2)[:, 0:W], T["c"][:, 0:W], ALU.mult),
                   r=[PSK(b0 + 2), kp + "c"], w=[kp + "z"])
                op("act", lambda e, T=T, W=W, k=k: e.activation(T["acc"][:, 0:W], T["z"][:, 0:W], AF.Copy, scale=cw3[:, k, 1:2]),
                   r=[kp + "z", "cw"], w=[kp + "acc"])
                zv = v3(T["z"][:, 0:W], RW)
                av = v3(T["acc"][:, 0:W], RW)
                op("dve", lambda e, zv=zv, av=av, k=k, RW=RW: e.scalar_tensor_tensor(av[:, :, 1:RW], zv[:, :, 0:RW - 1], cw3[:, k, 0:1], av[:, :, 1:RW], ALU.mult, ALU.add),
                   r=[kp + "z", kp + "acc", "cw"], w=[kp + "acc"])
                op("dve", lambda e, zv=zv, av=av, k=k, RW=RW: e.scalar_tensor_tensor(av[:, :, 0:RW - 1], zv[:, :, 1:RW], cw3[:, k, 2:3], av[:, :, 0:RW - 1], ALU.mult, ALU.add),
                   r=[kp + "z", kp + "acc", "cw"], w=[kp + "acc"])
                op("dve", lambda e, T=T, b0=b0, W=W: e.tensor_tensor(T["y"][:, 0:W], bank(b0)[:, 0:W], T["acc"][:, 0:W], ALU.mult),
                   r=[PSK(b0), kp + "acc"], w=[kp + "y"])
                headnorm_store(T["y"], W, gc[:, k:k + 1], k, tok0, (T["sq"], T["ms"], T["sd"], T["yn"]), it)
                it += 1
        for cq in range(4):
            ws = wsl[cq % 2][0]
            dma("pool", ws, w_in_t[l, 36 + cq], w=[("win", cq % 2, 0)])
            for (tok0, W, RW) in tiles_for(True):
                b0 = 4 * (it % 2)
                for kk in range(KC):
                    op("pe", lambda e, kk=kk, b0=b0, ws=ws, tok0=tok0, W=W: e.matmul(
                        bank(b0)[:, 0:W], ws[:, kk, :], hT[:, kk, tok0:tok0 + W], start=(kk == 0), stop=(kk == KC - 1)),
                       r=[("win", cq % 2, 0)] + [("hT", tt) for tt in range(tok0 // P, (tok0 + W) // P)], w=[PSK(b0)])
                op("act", lambda e, b0=b0, cq=cq, tok0=tok0, W=W: e.copy(uT[:, cq, tok0:tok0 + W], bank(b0)[:, 0:W]),
                   r=[PSK(b0)], w=[("uT", cq, tok0)])
                it += 1
        S.barrier()
        A.release(m)

    def reduce_angle(dst, kd, x, kx, n, tf, ti):
        op("dve", lambda e: e.tensor_scalar(tf, x, 1.0 / TWO_PI, None, ALU.mult), r=[kx], w=["ra_tf"])
        op("dve", lambda e: e.tensor_copy(ti, tf), r=["ra_tf"], w=["ra_ti"])
        op("dve", lambda e: e.tensor_copy(tf, ti), r=["ra_ti"], w=["ra_tf"])
        op("dve", lambda e: e.scalar_tensor_tensor(dst, tf, -TWO_PI, x, ALU.mult, ALU.add), r=["ra_tf", kx], w=[kd])
        op("dve", lambda e: e.tensor_scalar(tf, dst, math.pi, -TWO_PI, ALU.is_gt, ALU.mult), r=[kd], w=["ra_tf"])
        op("dve", lambda e: e.tensor_tensor(dst, dst, tf, ALU.add), r=["ra_tf", kd], w=[kd])
        op("dve", lambda e: e.tensor_scalar(tf, dst, -math.pi, TWO_PI, ALU.is_lt, ALU.mult), r=[kd], w=["ra_tf"])
        op("dve", lambda e: e.tensor_tensor(dst, dst, tf, ALU.add), r=["ra_tf", kd], w=[kd])
        op("dve", lambda e: e.tensor_scalar(dst, dst, PI_LO, -PI_LO, ALU.min, ALU.max), r=[kd], w=[kd])

    def sincos(sin_out, ks, cos_out, kc, x, kx, n, tf, ti, xs, red, neg_sin=False):
        reduce_angle(red, "ra_red", x, kx, n, tf, ti)
        op("act", lambda e: e.activation(sin_out, red, AF.Sin, scale=(-1.0 if neg_sin else 1.0)), r=["ra_red"], w=[ks])
        op("dve", lambda e: e.tensor_scalar(xs, x, math.pi / 2, None, ALU.add), r=[kx], w=["ra_xs"])
        reduce_angle(red, "ra_red", xs, "ra_xs", n, tf, ti)
        op("act", lambda e: e.activation(cos_out, red, AF.Sin), r=["ra_red"], w=[kc])

    def phase_b2(l, ctx_out, uT):
        m = A.mark()
        ysum = v3(A.f32(4 * NTOK), NTOK)
        NT = 16 * P
        cosT = A.f32(NT); nsinT = A.f32(NT); RKre = A.f32(NT); RKim = A.f32(NT); MAG0 = A.f32(NT)
        BTre = v3(A.bf16(NT), P); BTim = v3(A.bf16(NT), P); CTre = v3(A.bf16(NT), P); CTim = v3(A.bf16(NT), P)
        small = [A.f32(16) for _ in range(26)]
        (lr, li, ldt, dt, lrdt, th, mag, s1, c1, ar, ai, nr, den, kr, ki, thr, tA, tB, Ere, Eim, th128, s128, c128, tC, tD, tE) = small
        HW = 8 * P
        QW = 4 * P
        WS = []
        big = A.f32(4 * 6 * QW)
        for q_ in range(4):
            o_ = q_ * 6 * QW
            WS.append(dict(t1=big[:, o_:o_ + QW], t2=big[:, o_ + QW:o_ + 2 * QW], t3=big[:, o_ + 2 * QW:o_ + 3 * QW],
                           t4=big[:, o_ + 3 * QW:o_ + 4 * QW], gre=big[:, o_ + 4 * QW:o_ + 5 * QW], gim=big[:, o_ + 5 * QW:o_ + 6 * QW],
                           hre=A.bf16(QW), nhim=A.bf16(QW), ytmp=A.f32(P),
                           carry=[A.f32(4), A.f32(4)], ctmp=[A.f32(4) for _ in range(4)]))
        wkA = big[:, 0:NT]; wkB = big[:, NT:2 * NT]; wkC = big[:, 2 * NT:3 * NT]; gg = big[:, 3 * NT:4 * NT]

        def sm(o, a, b, alu, keys_r, key_w):
            op("dve", lambda e: e.tensor_tensor(o, a, b, alu), r=keys_r, w=[key_w])

        for d in range(2):
            dma("sp", lr, lamre[l, d], w=["lr"])
            dma("sp", li, lamim[l, d], w=["li"])
            dma("sp", ldt, logdt[l, d], w=["ldt"])
            for (dst, src) in ((BTre, bt_re), (BTim, bt_im), (CTre, ct_re), (CTim, ct_im)):
                dma("pool", dst.rearrange("p (c j) q -> p c (j q)", c=4), src[l, d].rearrange("c p x -> p c x"), w=["BC"])
            op("act", lambda e: e.activation(dt, ldt, AF.Exp), r=["ldt"], w=["dt"])
            sm(lrdt, lr, dt, ALU.mult, ["lr", "dt"], "lrdt")
            sm(th, li, dt, ALU.mult, ["li", "dt"], "th")
            op("act", lambda e: e.activation(mag, lrdt, AF.Exp), r=["lrdt"], w=["mag"])
            tBi = tB.bitcast(I32)
            sincos(s1, "s1", c1, "c1", th, "th", 16, tA, tBi, tC, tD)
            reduce_angle(thr, "thr", th, "th", 16, tA, tBi)
            sm(ar, mag, c1, ALU.mult, ["mag", "c1"], "ar")
            sm(ai, mag, s1, ALU.mult, ["mag", "s1"], "ai")
            op("dve", lambda e: e.tensor_scalar(nr, ar, -1.0, None, ALU.add), r=["ar"], w=["nr"])
            sm(den, lr, lr, ALU.mult, ["lr"], "den")
            sm(tE, li, li, ALU.mult, ["li"], "tE")
            sm(den, den, tE, ALU.add, ["den", "tE"], "den")
            op("dve", lambda e: e.reciprocal(den, den), r=["den"], w=["den"])
            sm(kr, nr, lr, ALU.mult, ["nr", "lr"], "kr")
            sm(tE, ai, li, ALU.mult, ["ai", "li"], "tE")
            sm(kr, kr, tE, ALU.add, ["kr", "tE"], "kr")
            sm(kr, kr, den, ALU.mult, ["kr", "den"], "kr")
            sm(ki, ai, lr, ALU.mult, ["ai", "lr"], "ki")
            sm(tE, nr, li, ALU.mult, ["nr", "li"], "tE")
            sm(ki, ki, tE, ALU.subtract, ["ki", "tE"], "ki")
            sm(ki, ki, den, ALU.mult, ["ki", "den"], "ki")
            op("dve", lambda e: e.tensor_scalar(th128, thr, 128.0, None, ALU.mult), r=["thr"], w=["th128"])
            sincos(s128, "s128", c128, "c128", th128, "th128", 16, tA, tBi, tC, tD)
            sm(Ere, mag, c128, ALU.mult, ["mag", "c128"], "Ere")
            sm(Eim, mag, s128, ALU.mult, ["mag", "s128"], "Eim")
            op("dve", lambda e: e.tensor_tensor(v3(wkA, P), thr.unsqueeze(2).to_broadcast([P, 16, P]), iota_t.unsqueeze(1).to_broadcast([P, 16, P]), ALU.mult),
               r=["thr"], w=["ang"])
            sincos(nsinT, "nsinT", cosT, "cosT", wkA, "ang", NT, wkB, gg.bitcast(I32), wkC, MAG0, neg_sin=True)
            krb = kr.unsqueeze(2).to_broadcast([P, 16, P]); kib = ki.unsqueeze(2).to_broadcast([P, 16, P])
            op("dve", lambda e: e.tensor_tensor(v3(RKre, P), v3(cosT, P), krb, ALU.mult), r=["cosT", "kr"], w=["RKre"])
            op("dve", lambda e: e.tensor_tensor(v3(wkA, P), v3(nsinT, P), kib, ALU.mult), r=["nsinT", "ki", "ang"], w=["ang"])
            op("dve", lambda e: e.tensor_tensor(RKre, RKre, wkA, ALU.subtract), r=["RKre", "ang"], w=["RKre"])
            op("dve", lambda e: e.tensor_tensor(v3(RKim, P), v3(cosT, P), kib, ALU.mult), r=["cosT", "ki"], w=["RKim"])
            op("dve", lambda e: e.tensor_tensor(v3(wkA, P), v3(nsinT, P), krb, ALU.mult), r=["nsinT", "kr", "ang", "RKre"], w=["ang"])
            op("dve", lambda e: e.tensor_tensor(RKim, RKim, wkA, ALU.add), r=["RKim", "ang"], w=["RKim"])
            op("dve", lambda e: e.tensor_copy(v3(MAG0, P), mag.unsqueeze(2).to_broadcast([P, 16, P])), r=["mag", "ra_red"], w=["MAG0"])
            op("dve", lambda e: e.memset(v3(MAG0, P)[:, :, 0:1], 0.0), r=["MAG0"], w=["MAG0"])
            S.barrier()

            if d == 0:
                order = list(range(18))
            else:
                order = [1, 0] + list(range(17, 1, -1))
            pending = {}

            def KQ(n, q):
                return (n, q)

            def S1(c, q):
                tsl = slice(c * P, (c + 1) * P)
                rhs = uT[:, q, tsl] if d == 0 else uT[:, q, tsl][:, ::-1]
                for jl in range(4):
                    j = 4 * q + jl
                    op("pe", lambda e, jl=jl, j=j, rhs=rhs, q=q: e.matmul(bank(2 * q)[:, jl * P:(jl + 1) * P], BTre[:, j, :], rhs, start=True, stop=True),
                       r=["BC"], w=[PSK(2 * q)])
                    op("pe", lambda e, jl=jl, j=j, rhs=rhs, q=q: e.matmul(bank(2 * q + 1)[:, jl * P:(jl + 1) * P], BTim[:, j, :], rhs, start=True, stop=True),
                       r=["BC"], w=[PSK(2 * q + 1)])

            def S2(c, q, first):
                W_ = WS[q]
                t1 = W_["t1"]; t2 = W_["t2"]; t3 = W_["t3"]; t4 = W_["t4"]
                fs = slice(q * QW, (q + 1) * QW)
                if q in pending:
                    cq_, tsl_ = pending.pop(q)
                    op("pool", lambda e, W_=W_, cq_=cq_, tsl_=tsl_: e.tensor_tensor(ysum[:, cq_, tsl_], ysum[:, cq_, tsl_], W_["ytmp"], ALU.add),
                       r=[KQ("ytmp", q)], w=[("ys", cq_, tsl_.start)])
                op("dve", lambda e, t1=t1, fs=fs, q=q: e.tensor_tensor(t1, RKre[:, fs], bank(2 * q), ALU.mult), r=[PSK(2 * q)], w=[KQ("t1", q)])
                op("dve", lambda e, t3=t3, fs=fs, q=q: e.tensor_tensor(t3, RKre[:, fs], bank(2 * q + 1), ALU.mult), r=[PSK(2 * q + 1)], w=[KQ("t3", q)])
                op("dve", lambda e, t4=t4, fs=fs, q=q: e.tensor_tensor(t4, RKim[:, fs], bank(2 * q), ALU.mult), r=[PSK(2 * q)], w=[KQ("t4", q)])
                op("dve", lambda e, t2=t2, fs=fs, q=q: e.tensor_tensor(t2, RKim[:, fs], bank(2 * q + 1), ALU.mult), r=[PSK(2 * q + 1)], w=[KQ("t2", q)])
                op("pool", lambda e, t1=t1, t2=t2: e.tensor_tensor(t1, t1, t2, ALU.subtract), r=[KQ("t1", q), KQ("t2", q)], w=[KQ("t1", q)])
                op("dve", lambda e, t3=t3, t4=t4: e.tensor_tensor(t3, t3, t4, ALU.add), r=[KQ("t3", q), KQ("t4", q)], w=[KQ("t3", q)])
                if not first:
                    br3 = v3(t1, P); bi3 = v3(t3, P)
                    op("pool", lambda e, W_=W_, br3=br3: e.tensor_tensor(br3[:, :, 0:1], br3[:, :, 0:1], W_["carry"][0].unsqueeze(2), ALU.add),
                       r=[KQ("t1", q), KQ("carry", q)], w=[KQ("t1", q)])
                    op("pool", lambda e, W_=W_, bi3=bi3: e.tensor_tensor(bi3[:, :, 0:1], bi3[:, :, 0:1], W_["carry"][1].unsqueeze(2), ALU.add),
                       r=[KQ("t3", q), KQ("carry", q)], w=[KQ("t3", q)])

            def S3(c, q):
                W_ = WS[q]
                t1 = W_["t1"]; t3 = W_["t3"]; gre = W_["gre"]; gim = W_["gim"]
                fs = slice(q * QW, (q + 1) * QW)
                js = slice(4 * q, 4 * q + 4)
                op("dve", lambda e, fs=fs, gre=gre, t1=t1: e.tensor_tensor_scan(gre, MAG0[:, fs], t1, 0.0, ALU.mult, ALU.add), r=[KQ("t1", q)], w=[KQ("gre", q)])
                op("dve", lambda e, fs=fs, gim=gim, t3=t3: e.tensor_tensor_scan(gim, MAG0[:, fs], t3, 0.0, ALU.mult, ALU.add), r=[KQ("t3", q)], w=[KQ("gim", q)])
                glr = v3(gre, P)[:, :, P - 1]; gli = v3(gim, P)[:, :, P - 1]
                ct_ = W_["ctmp"]; cy_ = W_["carry"]
                op("pool", lambda e, js=js, glr=glr, ct_=ct_: e.tensor_tensor(ct_[0], Ere[:, js], glr, ALU.mult), r=[KQ("gre", q)], w=[KQ("c0", q)])
                op("pool", lambda e, js=js, gli=gli, ct_=ct_: e.tensor_tensor(ct_[1], Eim[:, js], gli, ALU.mult), r=[KQ("gim", q)], w=[KQ("c1", q)])
                op("pool", lambda e, cy_=cy_, ct_=ct_: e.tensor_tensor(cy_[0], ct_[0], ct_[1], ALU.subtract), r=[KQ("c0", q), KQ("c1", q)], w=[KQ("carry", q)])
                op("pool", lambda e, js=js, gli=gli, ct_=ct_: e.tensor_tensor(ct_[2], Ere[:, js], gli, ALU.mult), r=[KQ("gim", q)], w=[KQ("c2", q)])
                op("pool", lambda e, js=js, glr=glr, ct_=ct_: e.tensor_tensor(ct_[3], Eim[:, js], glr, ALU.mult), r=[KQ("gre", q)], w=[KQ("c3", q)])
                op("pool", lambda e, cy_=cy_, ct_=ct_: e.tensor_tensor(cy_[1], ct_[2], ct_[3], ALU.add), r=[KQ("c2", q), KQ("c3", q), KQ("carry", q)], w=[KQ("carry", q)])

            def S4(c, q):
                W_ = WS[q]
                t1 = W_["t1"]; t2 = W_["t2"]; t3 = W_["t3"]; t4 = W_["t4"]
                gre = W_["gre"]; gim = W_["gim"]; hre = W_["hre"]; nhim = W_["nhim"]
                fs = slice(q * QW, (q + 1) * QW)
                op("pool", lambda e, fs=fs, t1=t1, gre=gre: e.tensor_tensor(t1, cosT[:, fs], gre, ALU.mult), r=[KQ("gre", q)], w=[KQ("t1", q)])
                op("pool", lambda e, fs=fs, t2=t2, gim=gim: e.tensor_tensor(t2, nsinT[:, fs], gim, ALU.mult), r=[KQ("gim", q)], w=[KQ("t2", q)])
                op("dve", lambda e, fs=fs, t3=t3, gre=gre: e.tensor_tensor(t3, nsinT[:, fs], gre, ALU.mult), r=[KQ("gre", q)], w=[KQ("t3", q)])
                op("pool", lambda e, fs=fs, t4=t4, gim=gim: e.tensor_tensor(t4, cosT[:, fs], gim, ALU.mult), r=[KQ("gim", q)], w=[KQ("t4", q)])
                op("dve", lambda e, hre=hre, t1=t1, t2=t2: e.tensor_tensor(hre, t1, t2, ALU.add), r=[KQ("t1", q), KQ("t2", q)], w=[KQ("hre", q)])
                op("dve", lambda e, nhim=nhim, t3=t3, t4=t4: e.tensor_tensor(nhim, t3, t4, ALU.subtract), r=[KQ("t3", q), KQ("t4", q)], w=[KQ("nhim", q)])

            def S5(c, q):
                W_ = WS[q]
                tsl = slice(c * P, (c + 1) * P)
                hre3 = v3(W_["hre"], P); nhim3 = v3(W_["nhim"], P)
                pb = 2 * q
                for j4 in range(4):
                    j = q * 4 + j4
                    op("pe", lambda e, j=j, j4=j4, pb=pb, hre3=hre3, d_=d: e.matmul(bank(pb)[:, 0:P], CTre[:, j, :], (hre3[:, j4, :] if d_ == 0 else hre3[:, j4, :][:, ::-1]), start=(j4 == 0), stop=False),
                       r=["BC", KQ("hre", q)], w=[PSK(pb)])
                    op("pe", lambda e, j=j, j4=j4, pb=pb, nhim3=nhim3, d_=d: e.matmul(bank(pb)[:, 0:P], CTim[:, j, :], (nhim3[:, j4, :] if d_ == 0 else nhim3[:, j4, :][:, ::-1]), start=False, stop=(j4 == 3)),
                       r=["BC", KQ("nhim", q)], w=[PSK(pb)])
                if d == 0:
                    op("act", lambda e, q=q, pb=pb, tsl=tsl: e.copy(ysum[:, q, tsl], bank(pb)[:, 0:P]), r=[PSK(pb)], w=[("ys", q, tsl.start)])
                else:
                    op("act", lambda e, W_=W_, pb=pb: e.copy(W_["ytmp"], bank(pb)[:, 0:P]), r=[PSK(pb)], w=[KQ("ytmp", q)])
                    pending[q] = (q, tsl)

            pairs = [(0, 1), (2, 3)]
            n_ = len(order)
            for pr in pairs:
                for q in pr:
                    S1(order[0], q)
            for i, c in enumerate(order):
                need_out = ctx_out or c >= 2
                for pr in pairs:
                    for q in pr:
                        S2(c, q, i == 0)
                    for q in pr:
                        S3(c, q)
                    if need_out:
                        for q in pr:
                            S4(c, q)
                        for q in pr:
                            S5(c, q)
                    if i + 1 < n_:
                        for q in pr:
                            S1(order[i + 1], q)
            for q in list(pending.keys()):
                cq_, tsl_ = pending.pop(q)
                op("pool", lambda e, q=q, cq_=cq_, tsl_=tsl_: e.tensor_tensor(ysum[:, cq_, tsl_], ysum[:, cq_, tsl_], WS[q]["ytmp"], ALU.add),
                   r=[KQ("ytmp", q)], w=[("ys", cq_, tsl_.start)])
            S.barrier()

        tokr = slice(0, NTOK) if ctx_out else slice(NCTX, NTOK)
        NTK = NTOK if ctx_out else NLAT
        A.release(m)
        ysum = v3(A.f32(4 * NTOK), NTOK)
        dcc = A.f32(4); gsc = A.f32(4)
        dma("sp", dcc, dcol[l], w=["dc"])
        dma("sp", gsc, gssm[l], w=["gcols"])
        wg = v3(A.bf16(4 * 512), 512)
        dma("pool", wg, w_glu[l].rearrange("(k p) n -> p k n", p=P), w=["wg"])
        gl16 = v3(A.bf16(4 * NTOK), NTOK)
        w1 = A.f32(NTOK); w2 = A.f32(NTOK)
        for cq in range(4):
            yv = ysum[:, cq, tokr]
            op("dve", lambda e, cq=cq, yv=yv: e.scalar_tensor_tensor(yv, uT[:, cq, tokr], dcc[:, cq:cq + 1], yv, ALU.mult, ALU.add),
               r=["dc", "uTall"], w=[("yt", cq)])
            if "dbg_y" in dbg:
                dma("sp", dbg_y[cq], ysum[:, cq, :], r=[("yt", cq)])
            op("pool", lambda e, yv=yv: e.tensor_tensor(w1[:, 0:NTK], yv, yv, ALU.mult), r=[("yt", cq)], w=["w1"])
            op("dve", lambda e: e.tensor_scalar(w1[:, 0:NTK], w1[:, 0:NTK], 0.044715, 1.0, ALU.mult, ALU.add), r=["w1"], w=["w1"])
            op("pool", lambda e, yv=yv: e.tensor_tensor(w2[:, 0:NTK], w1[:, 0:NTK], yv, ALU.mult), r=["w1", ("yt", cq)], w=["w2"])
            op("act", lambda e: e.activation(w1[:, 0:NTK], w2[:, 0:NTK], AF.Sigmoid, scale=1.5957691216057308), r=["w2", "w1"], w=["w1"])
            op("dve", lambda e, yv=yv: e.tensor_tensor(yv, yv, w1[:, 0:NTK], ALU.mult), r=["w1", ("yt", cq)], w=[("yt", cq)])
            op("act", lambda e, cq=cq, yv=yv: e.copy(gl16[:, cq, tokr], yv), r=[("yt", cq)], w=[("gl16", cq)])
        tmp = []
        for i in range(2):
            tmp.append(dict(sg=A.f32(512), o=A.f32(512), sq=A.bf16(512), ms=A.f32(512), sd=A.f32(512), yn=A.bf16(512)))
        it = 0
        for oc in range(4):
            for (tok0, W, RW) in tiles_for(ctx_out):
                T = tmp[it % 2]
                b0 = 4 * (it % 2)
                kp = "hn%d" % (it % 2)
                for k in range(4):
                    op("pe", lambda e, k=k, oc=oc, b0=b0, tok0=tok0, W=W: e.matmul(bank(b0)[:, 0:W], wg[:, k, oc * P:(oc + 1) * P], gl16[:, k, tok0:tok0 + W], start=(k == 0), stop=(k == 3)),
                       r=["wg"] + [("gl16", kk) for kk in range(4)], w=[PSK(b0)])
                op("act", lambda e, T=T, b0=b0, W=W: e.activation(T["sg"][:, 0:W], bank(b0)[:, 0:W], AF.Sigmoid), r=[PSK(b0)], w=[kp + "sg"])
                op("dve", lambda e, T=T, oc=oc, tok0=tok0, W=W: e.tensor_tensor(T["o"][:, 0:W], ysum[:, oc, tok0:tok0 + W], T["sg"][:, 0:W], ALU.mult),
                   r=[kp + "sg", ("yt", oc)], w=[kp + "y"])
                headnorm_store(T["o"], W, gsc[:, oc:oc + 1], 12 + oc, tok0, (T["sq"], T["ms"], T["sd"], T["yn"]), it)
                it += 1
        S.barrier()
        A.release(m)

    def phase_b3(l, ctx_full):
        m = A.mark()
        wo = v3(A.bf16(KC * D), D)
        wov = w_out[l].rearrange("(k p) n -> p k n", p=P)
        for cg in range(4):
            dma("pool", wo[:, :, cg * 512:(cg + 1) * 512], wov[:, :, cg * 512:(cg + 1) * 512], w=[("wo", cg)])
        g1 = [A.f32(D) for _ in range(2)]
        for r in range(2):
            load_mod_bc(g1[r], r, 2, ("g1", r))
        mts = [v3(A.bf16(KC * 512), 512) for _ in range(2)]
        xs = [A.f32(D) for _ in range(2)]
        xo = [A.f32(D) for _ in range(2)]
        tq = A.f32(512)
        t_list = list(range(0 if ctx_full else 2, 18))
        loaded = {}
        nload = 0
        for idx_t, t in enumerate(t_list):
            s = idx_t % 2
            r = 1 if t < 2 else 0
            grp = -1 if t < 2 else (t - 2) // 4
            if grp not in loaded:
                ms_ = nload % 2
                nload += 1
                tok0, W = (0, 256) if grp < 0 else (256 + grp * 512, 512)
                dma("sp", mts[ms_][:, :, 0:W], mixn[:, :, tok0:tok0 + W].rearrange("c p t -> p c t"), w=[("mt", ms_)])
                loaded[grp] = (ms_, tok0)
            ms_, tok0 = loaded[grp]
            lo = t * P - tok0
            dma("sp", xs[s], tok_src(l, t), w=[("x", s)])
            for cg in range(4):
                pb = (idx_t * 4 + cg) % 8
                cs = slice(cg * 512, (cg + 1) * 512)
                for k in range(KC):
                    op("pe", lambda e, k=k, ms_=ms_, lo=lo, cs=cs, pb=pb: e.matmul(bank(pb), mts[ms_][:, k, lo:lo + P], wo[:, k, cs], start=(k == 0), stop=(k == KC - 1)),
                       r=[("mt", ms_), ("wo", cg)], w=[PSK(pb)])
                op("dve", lambda e, pb=pb, r=r, cs=cs: e.tensor_tensor(tq, bank(pb), g1[r][:, cs], ALU.mult), r=[PSK(pb), ("g1", r)], w=["tq"])
                op("pool", lambda e, s=s, cs=cs: e.tensor_tensor(xo[s][:, cs], tq, xs[s][:, cs], ALU.add), r=["tq", ("x", s)], w=[("xo", s)])
            dma("sp", tok_dst(t), xo[s], r=[("xo", s)])
        S.barrier()
        A.release(m)

    def phase_c0(l, ctx_full, affT):
        m = A.mark()
        gm = [A.f32(D) for _ in range(2)]
        sh = [A.f32(D) for _ in range(2)]
        gtmp = A.f32(D)
        bc_row(gtmp, norm2_g[l:l + 1, :], "g2row")
        for r in range(2):
            load_mod_bc(gm[r], r, 4, ("gm", r))
            load_mod_bc(sh[r], r, 3, ("sh", r))
            op("dve", lambda e, r=r: e.scalar_tensor_tensor(gm[r], gm[r], 1.0, gtmp, ALU.add, ALU.mult),
               r=["g2row", ("gm", r)], w=[("gm", r)])
        rw = v3(A.f32(KC * NE), NE)
        dma("sp", rw, router_w[l].rearrange("(k p) n -> p k n", p=P), w=["rw"])
        xs = [A.f32(D) for _ in range(2)]
        hn = A.f32(D)
        h2f = [A.f32(D) for _ in range(2)]
        h2b = [A.bf16(D) for _ in range(2)]
        h2T = v3(A.f32(D), P)
        junk = A.bf16(D)
        ss = A.f32(2); t1 = A.f32(2); t2 = A.f32(2); rstd = A.f32(2)
        mx = A.f32(2); se = A.f32(2); ex = A.f32(NE); aff = A.f32(NE)
        t_list = list(range(0 if ctx_full else 2, 18))
        for it, t in enumerate(t_list):
            s = it % 2
            r = 1 if t < 2 else 0
            dma("sp", xs[s], tok_dst(t), w=[("x", s)])
            op("act", lambda e, s=s: e.activation(junk, xs[s], AF.Square, accum_out=ss[:, 0:1]), r=[("x", s)], w=["junk", "c0ss"])
            rstd_from_ss(ss[:, 0:1], D, t1[:, 0:1], t2[:, 0:1], rstd[:, 0:1], "c0")
            op("dve", lambda e, s=s, r=r: e.scalar_tensor_tensor(hn, xs[s], rstd[:, 0:1], gm[r], ALU.mult, ALU.mult),
               r=[("x", s), "c0rstd", ("gm", r)], w=["hn"])
            op("pool", lambda e, s=s, r=r: e.tensor_tensor(h2f[s], hn, sh[r], ALU.add), r=["hn", ("sh", r)], w=[("h2f", s)])
            op("act", lambda e, s=s: e.copy(h2b[s], h2f[s]), r=[("h2f", s)], w=[("h2b", s)])
            dst = h2c[t * P:(t + 1) * P, :] if t < 2 else h2t[(t - 2) * P:(t - 1) * P, :]
            dma("sp", dst, h2b[s], r=[("h2b", s)])
            for k4 in range(4):
                bk = k4
                for kk in range(4):
                    k = k4 * 4 + kk
                    op("pe", lambda e, s=s, k=k, kk=kk, bk=bk: e.transpose(bank(bk)[:, kk * P:(kk + 1) * P], h2f[s][:, k * P:(k + 1) * P], ident_f),
                       r=[("h2f", s), "ident_f"], w=[PSK(bk)])
                if k4 % 2 == 0:
                    op("act", lambda e, k4=k4, bk=bk: e.copy(h2T[:, k4 * 4:k4 * 4 + 4, :], v3(bank(bk), P)), r=[PSK(bk)], w=[("h2T", k4)])
                else:
                    op("dve", lambda e, k4=k4, bk=bk: e.tensor_copy(h2T[:, k4 * 4:k4 * 4 + 4, :], v3(bank(bk), P)), r=[PSK(bk)], w=[("h2T", k4)])
            pl = 4 + (it % 2)
            for k in range(KC):
                op("pe", lambda e, k=k, pl=pl: e.matmul(bank(pl)[:, 0:NE], h2T[:, k, :], rw[:, k, :], start=(k == 0), stop=(k == KC - 1)),
                   r=[("h2T", k // 4), "rw"], w=[PSK(pl)])
            op("dve", lambda e, pl=pl: e.reduce_max(mx[:, 0:1], bank(pl)[:, 0:NE], AX.X), r=[PSK(pl)], w=["mx"])
            op("dve", lambda e: e.tensor_scalar(mx[:, 0:1], mx[:, 0:1], -1.0, None, ALU.mult), r=["mx"], w=["mx"])
            op("act", lambda e, pl=pl: e.activation(ex, bank(pl)[:, 0:NE], AF.Exp, bias=mx[:, 0:1], accum_out=se[:, 0:1]), r=[PSK(pl), "mx"], w=["ex", "se"])
            op("dve", lambda e: e.reciprocal(se[:, 0:1], se[:, 0:1]), r=["se"], w=["se"])
            op("dve", lambda e: e.tensor_scalar(aff, ex, se[:, 0:1], None, ALU.mult), r=["ex", "se"], w=["aff"])
            pt = 6 + (it % 2)
            op("pe", lambda e, pt=pt: e.transpose(bank(pt)[0:NE, 0:P], aff, ident_f), r=["aff", "ident_f"], w=[PSK(pt)])
            op("act", lambda e, pt=pt, t=t: e.copy(affT[0:NE, t * P:(t + 1) * P], bank(pt)[0:NE, 0:P]), r=[PSK(pt)], w=[("affT", t)])
        if "dbg_aff" in dbg:
            dma("sp", dbg_aff, affT[0:NE, :], r=[("affT", t) for t in t_list])
        S.barrier()
        A.release(m)

    def phase_c1(ctx_full, affT, idxT, valT):
        m = A.mark()
        work = A.f32(NLAT)
        vals = A.f32(288)
        idx = A.u32(288)
        idxf = A.f32(288)
        op("dve", lambda e: e.tensor_copy(work[0:NE, :], affT[0:NE, NCTX:NTOK]), w=["work"])
        for r_ in range(32):
            vs = slice(r_ * 8, r_ * 8 + 8)
            op("dve", lambda e, vs=vs: e.max(vals[0:NE, vs], work[0:NE, :]), r=["work"], w=["vals"])
            op("dve", lambda e, vs=vs: e.max_index(idx[0:NE, vs], vals[0:NE, vs], work[0:NE, :]), r=["work", "vals"], w=["idx"])
            op("dve", lambda e, vs=vs: e.match_replace(work[0:NE, :], vals[0:NE, vs], work[0:NE, :], -1.0), r=["work", "vals", "idx"], w=["work"])
        if ctx_full:
            wc = work[0:NE, 0:NCTX]
            op("dve", lambda e: e.tensor_copy(wc, affT[0:NE, 0:NCTX]), r=["work"], w=["work"])
            for r_ in range(4):
                vs = slice(256 + r_ * 8, 256 + r_ * 8 + 8)
                op("dve", lambda e, vs=vs: e.max(vals[0:NE, vs], wc), r=["work"], w=["vals"])
                op("dve", lambda e, vs=vs: e.max_index(idx[0:NE, vs], vals[0:NE, vs], wc), r=["work", "vals"], w=["idx"])
                op("dve", lambda e, vs=vs: e.match_replace(wc, vals[0:NE, vs], wc, -1.0), r=["work", "vals", "idx"], w=["work"])
        NS = 288 if ctx_full else 256
        op("dve", lambda e: e.tensor_copy(idxf[0:NE, 0:NS], idx[0:NE, 0:NS]), r=["idx"], w=["idxf"])
        chunks = [(0, 128), (128, 128)] + ([(256, 32)] if ctx_full else [])
        for ci, (c0, cn) in enumerate(chunks):
            op("pe", lambda e, c0=c0, cn=cn, ci=ci: e.transpose(bank(ci)[0:cn, 0:NE], idxf[0:NE, c0:c0 + cn], ident_f[0:NE, 0:NE]), r=["idxf", "ident_f"], w=[PSK(ci)])
            op("dve", lambda e, cn=cn, ci=ci: e.tensor_copy(idxT[ci][0:cn, :], bank(ci)[0:cn, 0:NE]), r=[PSK(ci)], w=[("idxT", ci)])
            op("pe", lambda e, c0=c0, cn=cn, ci=ci: e.transpose(bank(4 + ci)[0:cn, 0:NE], vals[0:NE, c0:c0 + cn], ident_f[0:NE, 0:NE]), r=["vals", "ident_f"], w=[PSK(4 + ci)])
            op("act", lambda e, cn=cn, ci=ci: e.copy(valT[ci][0:cn, :], bank(4 + ci)[0:cn, 0:NE]), r=[PSK(4 + ci)], w=[("valT", ci)])
        S.barrier()
        A.release(m)

    def phase_c2(l, ctx_full, idxT, valT):
        m = A.mark()
        g2 = [A.f32(D) for _ in range(2)]
        for r in range(2):
            load_mod_bc(g2[r], r, 5, ("g2", r))
        NGU = 3
        gus = [v3(A.bf16(KC * 512), 512) for _ in range(NGU)]
        dns = [v3(A.bf16(KC * 512), 512) for _ in range(2)]
        xs = v3(A.bf16(3 * D), D)
        NS = 288 if ctx_full else 256
        xsTs = [v3(A.bf16(KC * 288), 288) for _ in range(2)]
        aT = v3(A.bf16(KC * 288), 288)
        sg = [A.f32(288) for _ in range(2)]
        yst = v3(A.f32(3 * D), D)
        chunks = [(0, 128), (128, 128)] + ([(256, 32)] if ctx_full else [])
        gu_i = 0
        dn_i = 0

        def gather_T(e_):
            xsT = xsTs[e_ % 2]
            kx = ("xsT", e_ % 2)
            for ci, (c0, cn) in enumerate(chunks):
                srcd = h2t if ci < 2 else h2c
                op("pool", lambda e, ci=ci, cn=cn, srcd=srcd, e_=e_: e.indirect_dma_start(
                    out=xs[0:cn, ci, :], out_offset=None, in_=srcd,
                    in_offset=bass.IndirectOffsetOnAxis(ap=idxT[ci][0:cn, e_:e_ + 1], axis=0)),
                   r=[("idxT", ci)], w=[("xs", ci)], dma=True)
            for ci, (c0, cn) in enumerate(chunks):
                for k4 in range(4):
                    bk = (ci * 4 + k4) % 2
                    for kk in range(4):
                        k = k4 * 4 + kk
                        op("pe", lambda e, ci=ci, cn=cn, k=k, kk=kk, bk=bk: e.transpose(bank16(bk)[:, kk * P:kk * P + cn], xs[0:cn, ci, k * P:(k + 1) * P], ident_b[0:cn, 0:cn]),
                           r=[("xs", ci), "ident_b"], w=[PSK(bk)])
                    src = v3(bank16(bk)[:, 0:512], P)[:, :, 0:cn]
                    dstv = xsT[:, k4 * 4:k4 * 4 + 4, c0:c0 + cn]
                    if k4 % 2 == 0:
                        op("act", lambda e, src=src, dstv=dstv: e.copy(dstv, src), r=[PSK(bk)], w=[kx])
                    else:
                        op("dve", lambda e, src=src, dstv=dstv: e.tensor_copy(dstv, src), r=[PSK(bk)], w=[kx])

        gather_T(0)
        for e_ in range(NE):
            xsT = xsTs[e_ % 2]
            kx = ("xsT", e_ % 2)
            for fg in range(4):
                cs = slice(fg * 512, (fg + 1) * 512)
                sg_ = gu_i % NGU; gu_i += 1
                su_ = gu_i % NGU; gu_i += 1
                dma("pool", gus[sg_], w_gate[l, e_].rearrange("(k p) n -> p k n", p=P)[:, :, cs], w=[("gu", sg_)])
                dma("pool", gus[su_], w_up[l, e_].rearrange("(k p) n -> p k n", p=P)[:, :, cs], w=[("gu", su_)])
                for fc in range(4):
                    fcn = fg * 4 + fc
                    pg = 2 + 2 * (fcn % 2)
                    pu = pg + 1
                    for k in range(KC):
                        op("pe", lambda e, k=k, fc=fc, pg=pg, sg_=sg_, xsT=xsT: e.matmul(bank(pg)[:, 0:NS], gus[sg_][:, k, fc * P:(fc + 1) * P], xsT[:, k, 0:NS], start=(k == 0), stop=(k == KC - 1)),
                           r=[("gu", sg_), kx], w=[PSK(pg)])
                    for k in range(KC):
                        op("pe", lambda e, k=k, fc=fc, pu=pu, su_=su_, xsT=xsT: e.matmul(bank(pu)[:, 0:NS], gus[su_][:, k, fc * P:(fc + 1) * P], xsT[:, k, 0:NS], start=(k == 0), stop=(k == KC - 1)),
                           r=[("gu", su_), kx], w=[PSK(pu)])
                    sgt = sg[fcn % 2]
                    op("act", lambda e, pg=pg, sgt=sgt: e.activation(sgt[:, 0:NS], bank(pg)[:, 0:NS], AF.Silu), r=[PSK(pg)], w=[("sg", fcn % 2)])
                    op("dve", lambda e, pu=pu, sgt=sgt, fcn=fcn: e.tensor_tensor(aT[:, fcn, 0:NS], sgt[:, 0:NS], bank(pu)[:, 0:NS], ALU.mult),
                       r=[PSK(pu), ("sg", fcn % 2)], w=[("aT", fcn)])
            if e_ + 1 < NE:
                gather_T(e_ + 1)
            for cg in range(4):
                cs = slice(cg * 512, (cg + 1) * 512)
                sd_ = dn_i % 2; dn_i += 1
                dma("pool", dns[sd_], w_down[l, e_].rearrange("(k p) n -> p k n", p=P)[:, :, cs], w=[("dn", sd_)])
                for ci, (c0, cn) in enumerate(chunks):
                    pb = 6 + ((cg * 3 + ci) % 2)
                    r = 1 if ci == 2 else 0
                    for k in range(KC):
                        op("pe", lambda e, k=k, c0=c0, cn=cn, pb=pb, sd_=sd_: e.matmul(bank(pb)[0:cn, :], aT[:, k, c0:c0 + cn], dns[sd_][:, k, :], start=(k == 0), stop=(k == KC - 1)),
                           r=[("dn", sd_)] + [("aT", kk) for kk in range(KC)], w=[PSK(pb)])
                    op("dve", lambda e, ci=ci, cn=cn, pb=pb, r=r, cs=cs, e_=e_: e.scalar_tensor_tensor(yst[0:cn, ci, cs], bank(pb)[0:cn, :], valT[ci][0:cn, e_:e_ + 1], g2[r][0:cn, cs], ALU.mult, ALU.mult),
                       r=[PSK(pb), ("valT", ci), ("g2", r)], w=[("yst", ci)])
            for ci, (c0, cn) in enumerate(chunks):
                dstd = xl if ci < 2 else xc
                op("pool", lambda e, ci=ci, cn=cn, dstd=dstd, e_=e_: e.indirect_dma_start(
                    out=dstd, out_offset=bass.IndirectOffsetOnAxis(ap=idxT[ci][0:cn, e_:e_ + 1], axis=0),
                    in_=yst[0:cn, ci, :], in_offset=None, compute_op=ALU.add),
                   r=[("yst", ci), ("idxT", ci)], w=["xl_scatter" if ci < 2 else "xc_scatter"], dma=True)
        S.barrier()
        A.release(m)

    def phase_final():
        m = A.mark()
        g = A.f32(D)
        bc_row(g, final_g[0:1, :], "fg")
        xs = [A.f32(D) for _ in range(2)]
        xo = [A.f32(D) for _ in range(2)]
        junk = A.bf16(D)
        ss = A.f32(2); t1 = A.f32(2); t2 = A.f32(2); rstd = A.f32(2)
        src = xl if n_layers > 0 else x_in
        for t in range(16):
            s = t % 2
            dma("sp", xs[s], src[t * P:(t + 1) * P, :], w=[("x", s)])
            op("act", lambda e, s=s: e.activation(junk, xs[s], AF.Square, accum_out=ss[:, 0:1]), r=[("x", s)], w=["junk", "fss"])
            rstd_from_ss(ss[:, 0:1], D, t1[:, 0:1], t2[:, 0:1], rstd[:, 0:1], "f")
            op("dve", lambda e, s=s: e.scalar_tensor_tensor(xo[s], xs[s], rstd[:, 0:1], g, ALU.mult, ALU.mult), r=[("x", s), "frstd", "fg"], w=[("xo", s)])
            dma("sp", out[t * P:(t + 1) * P, :], xo[s], r=[("xo", s)])
        S.barrier()
        A.release(m)

    layer_mark = A.mark()

    def _gg(gre, gim):
        return gre
    if only == "b2":
        uT = v3(A.bf16(4 * NTOK), NTOK)
        for cq in range(4):
            op("dve", lambda e, cq=cq: e.memset(uT[:, cq, :], 0.5), w=[("uT", cq)])
        S.barrier()
        phase_b2(0, True, uT)
        n_layers = 0
        stop_after = ("x", 0)
    stages = []
    for l in range(n_layers):
        last = (l == DEPTH - 1)
        ctx_full = not last
        phase_ada(l)
        if stop_after == ("ada", l):
            break
        m0 = A.mark()
        uT = v3(A.bf16(4 * NTOK), NTOK)
        m1 = A.mark()
        hT = v3(A.bf16(KC * NTOK), NTOK)
        phase_b0(l, True, hT)
        phase_b1(l, ctx_full, hT, uT)
        A.release(m1)
        if stop_after == ("b1", l):
            break
        phase_b2(l, ctx_full, uT)
        A.release(m0)
        if stop_after == ("b2", l):
            break
        phase_b3(l, ctx_full)
        if stop_after == ("b3", l):
            break
        mc_ = A.mark()
        idxT = [A.u32(NE) for _ in range(3)]
        valT = [A.f32(NE) for _ in range(3)]
        m_aff = A.mark()
        affT = A.f32(NTOK)
        phase_c0(l, ctx_full, affT)
        phase_c1(ctx_full, affT, idxT, valT)
        A.release(m_aff)
        if stop_after == ("c1", l):
            break
        phase_c2(l, ctx_full, idxT, valT)
        A.release(mc_)
    if stop_after is None:
        phase_final()
    S.barrier()
    S.emit()
    stack.close()
    return nc


def prep_shared(inp):
    f = np.float32
    sh = {}
    sh["ada_w"] = np.ascontiguousarray(inp["ada_w"], f)
    sh["ada_b2"] = np.ascontiguousarray(np.repeat(inp["ada_b"][:, None, :], 2, axis=1), f)
    sh["norm1_g"] = np.ascontiguousarray(inp["norm1_g"], f)
    sh["norm2_g"] = np.ascontiguousarray(inp["norm2_g"], f)
    sh["final_g"] = np.ascontiguousarray(inp["final_norm_g"].reshape(1, D), f)
    w_in = inp["w_in"]
    sh["w_in_t"] = np.ascontiguousarray(w_in.reshape(DEPTH, KC, P, 40, P).transpose(0, 3, 2, 1, 4), f)
    cw = inp["conv_w"]
    sh["conv_wc"] = np.ascontiguousarray(cw.reshape(DEPTH, 3, 12, P).transpose(0, 3, 2, 1).reshape(DEPTH, P, 36), f)

    def col16(a):
        return np.ascontiguousarray(a.reshape(DEPTH, 2, 16, 2, 64).transpose(0, 1, 3, 4, 2).reshape(DEPTH, 2, P, 16), f)

    sh["lamre"] = col16(inp["ssm_lam_re"])
    sh["lamim"] = col16(inp["ssm_lam_im"])
    sh["logdt"] = col16(np.repeat(inp["ssm_log_dt"][..., None], 64, axis=-1))

    def bt(b):
        o = np.zeros((DEPTH, 2, 4, P, 4, P), f)
        bb = b.reshape(DEPTH, 2, 4, 4, 2, 64, 16)
        for jl in range(4):
            for gl in range(2):
                o[:, :, :, 32 * jl + 16 * gl:32 * jl + 16 * gl + 16, jl, gl * 64:(gl + 1) * 64] = bb[:, :, :, jl, gl].transpose(0, 1, 2, 4, 3)
        return o.reshape(DEPTH, 2, 4, P, 4 * P)

    def ct(c):
        o = np.zeros((DEPTH, 2, 4, P, 4, P), f)
        cc = c.reshape(DEPTH, 2, 4, 4, 2, 16, 64)
        for jl in range(4):
            for gl in range(2):
                o[:, :, :, gl * 64:(gl + 1) * 64, jl, 32 * jl + 16 * gl:32 * jl + 16 * gl + 16] = cc[:, :, :, jl, gl].transpose(0, 1, 2, 4, 3)
        return o.reshape(DEPTH, 2, 4, P, 4 * P)

    sh["bt_re"] = bt(inp["ssm_b_re"]); sh["bt_im"] = bt(inp["ssm_b_im"])
    sh["ct_re"] = ct(inp["ssm_c_re"]); sh["ct_im"] = ct(inp["ssm_c_im"])
    sh["dcol"] = np.ascontiguousarray(inp["ssm_d"].reshape(DEPTH, 4, P).transpose(0, 2, 1), f)
    sh["w_glu"] = np.ascontiguousarray(inp["ssm_w_glu"], f)
    sh["gconv"] = np.ascontiguousarray(inp["out_norm_conv_g"].reshape(DEPTH, 12, P).transpose(0, 2, 1), f)
    sh["gssm"] = np.ascontiguousarray(inp["out_norm_ssm_g"].reshape(DEPTH, 4, P).transpose(0, 2, 1), f)
    sh["w_out"] = np.ascontiguousarray(inp["w_out"], f)
    sh["router_w"] = np.ascontiguousarray(inp["router_w"], f)
    sh["w_gate"] = np.ascontiguousarray(inp["exp_w_gate"], f)
    sh["w_up"] = np.ascontiguousarray(inp["exp_w_up"], f)
    sh["w_down"] = np.ascontiguousarray(inp["exp_w_down"], f)
    return sh


def prep_core(inp, b):
    f = np.float32
    d = {}
    d["x"] = np.ascontiguousarray(inp["x"][b], f)
    d["ctx"] = np.ascontiguousarray(inp["ctx"][b], f)
    cc = np.zeros((P, KC, 2), f)
    cc[:, :, 0] = inp["c"][b].reshape(KC, P).T
    cc[:, :, 1] = inp["c_ctx"].reshape(KC, P).T
    d["ccol"] = cc.reshape(P, 32)
    return d


def kernel(**inputs):
    inp = {k: np.asarray(v) for k, v in inputs.items()}
    sh = prep_shared(inp)
    nc = build_program()
    in_maps = []
    for b in range(8):
        m = dict(sh)
        m.update(prep_core(inp, b))
        in_maps.append(m)
    res = run_bass_kernel_spmd(nc, in_maps, core_ids=list(range(8)))
    return np.stack([res.results[b]["out"] for b in range(8)], axis=0).astype(np.float32)
```

```python
import math
import numpy as np
import concourse.bass as bass
import concourse.mybir as mybir
from concourse.bass_utils import run_bass_kernel_spmd

F32 = mybir.dt.float32
BF16 = mybir.dt.bfloat16
U32 = mybir.dt.uint32
I32 = mybir.dt.int32
ALU = mybir.AluOpType
AF = mybir.ActivationFunctionType
AX = mybir.AxisListType

P = 128
D = 2048
KC = 16
NLAT = 2048
NCTX = 256
NTOK = NLAT + NCTX
DEPTH = 2
NE = 16
EPS = 1e-6
TWO_PI = 2.0 * math.pi
PI_LO = 3.1415925
ENGS = ["pe", "act", "dve", "pool", "sp"]
NDMA = 40


class Sched:
    def __init__(self, nc, stack):
        self.nc = nc
        self.sems = []
        self.eng_sem = {}
        for e in ENGS:
            self.eng_sem[e] = len(self.sems)
            self.sems.append(stack.enter_context(nc.semaphore("s_" + e)))
        self.dma_sem = []
        for i in range(NDMA):
            self.dma_sem.append(len(self.sems))
            self.sems.append(stack.enter_context(nc.semaphore("d_%d" % i)))
        self.dma_val = [0] * NDMA
        self.dma_next = 0
        self.cnt = {e: 0 for e in ENGS}
        self.recs = {e: [] for e in ENGS}
        self.seen = {e: {} for e in ENGS}
        self.lastw = {}
        self.rd_eng = {}
        self.rd_dma = {}

    def _need(self, eng, tok, waits):
        semi, val, src, is_dma = tok
        if self.seen[eng].get(semi, 0) >= val:
            return
        if (not is_dma) and src == eng and eng == "pe":
            return
        self.seen[eng][semi] = val
        waits.append((semi, val))

    def op(self, eng, fn, r=(), w=(), dma=False):
        waits = []
        for k in r:
            t = self.lastw.get(k)
            if t is not None:
                self._need(eng, t, waits)
        for k in w:
            t = self.lastw.get(k)
            if t is not None:
                self._need(eng, t, waits)
            for se, t in self.rd_eng.get(k, {}).items():
                self._need(eng, t, waits)
            for t in self.rd_dma.get(k, ()):
                self._need(eng, t, waits)
        if dma:
            slot = self.dma_next % NDMA
            self.dma_next += 1
            semi = self.dma_sem[slot]
            prev = self.dma_val[slot]
            if prev > self.seen[eng].get(semi, 0):
                waits.append((semi, prev))
                self.seen[eng][semi] = prev
            val = prev + 16
            self.dma_val[slot] = val
            tok = (semi, val, eng, True)
        else:
            self.cnt[eng] += 1
            tok = (self.eng_sem[eng], self.cnt[eng], eng, False)
        self.recs[eng].append((waits, fn, tok))
        for k in r:
            if dma:
                self.rd_dma.setdefault(k, []).append(tok)
            else:
                self.rd_eng.setdefault(k, {})[eng] = tok
        for k in w:
            self.lastw[k] = tok
            self.rd_eng[k] = {}
            self.rd_dma[k] = []
        return tok

    def barrier(self):
        toks = [(self.eng_sem[e], self.cnt[e], e, False) for e in ENGS if self.cnt[e] > 0]
        toks += [(self.dma_sem[i], self.dma_val[i], None, True) for i in range(NDMA) if self.dma_val[i] > 0]
        for e in ENGS:
            waits = []
            for t in toks:
                if t[2] == e and not t[3]:
                    continue
                if self.seen[e].get(t[0], 0) >= t[1]:
                    continue
                self.seen[e][t[0]] = t[1]
                waits.append((t[0], t[1]))
            if waits:
                self.recs[e].append((waits, None, None))
        self.lastw.clear()
        self.rd_eng.clear()
        self.rd_dma.clear()

    def emit(self):
        nc = self.nc
        sems = self.sems
        recs = self.recs

        def mk(name):
            def body(e):
                for waits, fn, tok in recs[name]:
                    for (s, v) in waits:
                        e.wait_ge(sems[s], v)
                    if fn is not None:
                        ins = fn(e)
                        ins.then_inc(sems[tok[0]], 16 if tok[3] else 1)
            return body

        with nc.Block() as block:
            block.tensor(mk("pe"))
            block.scalar(mk("act"))
            block.vector(mk("dve"))
            block.gpsimd(mk("pool"))
            block.sync(mk("sp"))


class Arena:
    def __init__(self, ap, ncols):
        self.ap = ap
        self.n = ncols
        self.off = 0

    def mark(self):
        return self.off

    def release(self, m):
        self.off = m

    def f32(self, cols):
        cols = (cols + 1) // 2 * 2
        assert self.off + cols <= self.n, ("SBUF arena overflow", self.off, cols, self.n)
        a = self.ap[:, self.off:self.off + cols]
        self.off += cols
        return a

    def bf16(self, cols):
        return self.f32((cols + 1) // 2).bitcast(BF16)[:, 0:cols]

    def u32(self, cols):
        return self.f32(cols).bitcast(U32)

    def i32(self, cols):
        return self.f32(cols).bitcast(I32)


def v3(ap, b):
    return ap.rearrange("p (a b) -> p a b", b=b)


def build_program(dbg=None, stop_after=None, n_layers=DEPTH, only=None):
    from contextlib import ExitStack
    dbg = dbg or []
    nc = bass.Bass("TRN2", target_bir_lowering=False)

    def din(name, shape, dt=F32):
        if only == "b2" and int(np.prod(shape)) > 4_000_000:
            shape = [2, 2]
        return nc.dram_tensor(name, list(shape), dt, kind="ExternalInput").ap()

    x_in = din("x", [NLAT, D])
    ctx_in = din("ctx", [NCTX, D])
    ccol = din("ccol", [P, 32])
    ada_w = din("ada_w", [DEPTH, D, 6 * D])
    ada_b2 = din("ada_b2", [DEPTH, 2, 6 * D])
    norm1_g = din("norm1_g", [DEPTH, D])
    norm2_g = din("norm2_g", [DEPTH, D])
    final_g = din("final_g", [1, D])
    w_in_t = din("w_in_t", [DEPTH, 40, P, KC, P])
    conv_wc = din("conv_wc", [DEPTH, P, 36])
    lamre = din("lamre", [DEPTH, 2, P, 16])
    lamim = din("lamim", [DEPTH, 2, P, 16])
    logdt = din("logdt", [DEPTH, 2, P, 16])
    bt_re = din("bt_re", [DEPTH, 2, 4, P, 4 * P])
    bt_im = din("bt_im", [DEPTH, 2, 4, P, 4 * P])
    ct_re = din("ct_re", [DEPTH, 2, 4, P, 4 * P])
    ct_im = din("ct_im", [DEPTH, 2, 4, P, 4 * P])
    dcol = din("dcol", [DEPTH, P, 4])
    w_glu = din("w_glu", [DEPTH, 512, 512])
    gconv = din("gconv", [DEPTH, P, 12])
    gssm = din("gssm", [DEPTH, P, 4])
    w_out = din("w_out", [DEPTH, D, D])
    router_w = din("router_w", [DEPTH, D, NE])
    small_exp = stop_after is not None and stop_after[0] != "c2"
    if small_exp:
        w_gate = w_up = w_down = None
    else:
        w_gate = din("w_gate", [DEPTH, NE, D, D])
        w_up = din("w_up", [DEPTH, NE, D, D])
        w_down = din("w_down", [DEPTH, NE, D, D])
    out = nc.dram_tensor("out", [NLAT, D], F32, kind="ExternalOutput").ap()

    def dscr(name, shape, dt):
        kind = "ExternalOutput" if name in dbg else "Internal"
        return nc.dram_tensor(name, list(shape), dt, kind=kind).ap()

    xl = dscr("xl", [NLAT, D], F32)
    xc = dscr("xc", [NCTX, D], F32)
    h2t = dscr("h2t", [NLAT, D], BF16)
    h2c = dscr("h2c", [NCTX, D], BF16)
    modrow = dscr("modrow", [2, 6 * D], F32)
    mixn = dscr("mixn", [16, P, NTOK], BF16)
    dbg_aff = dscr("dbg_aff", [NE, NTOK], F32)
    dbg_y = dscr("dbg_y", [4, P, NTOK], F32)

    stack = ExitStack()
    NCOLS = 44800
    arena_t = stack.enter_context(nc.sbuf_tensor("arena", [P, NCOLS], F32))
    ps_t = stack.enter_context(nc.psum_tensor("ps", [P, 4096], F32))
    S = Sched(nc, stack)
    A = Arena(arena_t, NCOLS)
    op = S.op

    def bank(i):
        return ps_t[:, i * 512:(i + 1) * 512]

    def bank16(i):
        return ps_t[:, i * 512:(i + 1) * 512].bitcast(BF16)

    def PSK(i):
        return ("ps", i)

    ident_f = A.f32(P)
    ident_b = A.bf16(P)
    ones_b = A.bf16(P)
    iota_i = A.i32(P)
    iota_t = A.f32(P)
    op("pool", lambda e: e.iota(iota_i, [[1, P]], base=0, channel_multiplier=-1), w=["iota_i"])
    op("dve", lambda e: e.tensor_single_scalar(ident_f, iota_i, 0, ALU.is_equal), r=["iota_i"], w=["ident_f"])
    op("dve", lambda e: e.tensor_copy(ident_b, ident_f), r=["ident_f"], w=["ident_b"])
    op("dve", lambda e: e.memset(ones_b, 1.0), w=["ones_b"])
    op("pool", lambda e: e.iota(iota_i, [[1, P]], base=0, channel_multiplier=0), r=["ident_f"], w=["iota_i"])
    op("dve", lambda e: e.tensor_copy(iota_t, iota_i), r=["iota_i"], w=["iota_t"])
    S.barrier()
    base_mark = A.mark()

    def dma(q, out_ap, in_ap, r=(), w=()):
        return op(q, lambda e: e.dma_start(out=out_ap, in_=in_ap), r=r, w=w, dma=True)

    def bc_row(dst, src_row, key):
        dma("sp", dst, src_row.partition_broadcast(P), w=[key])

    def rstd_from_ss(ss, n, tmp1, tmp2, rstd, keyp):
        op("dve", lambda e: e.tensor_scalar(tmp1, ss, 1.0 / n, EPS, ALU.mult, ALU.add), r=[keyp + "ss"], w=[keyp + "t1"])
        op("act", lambda e: e.activation(tmp2, tmp1, AF.Sqrt), r=[keyp + "t1"], w=[keyp + "t2"])
        op("dve", lambda e: e.reciprocal(rstd, tmp2), r=[keyp + "t2"], w=[keyp + "rstd"])

    def tok_src(l, t):
        if t < 2:
            src = ctx_in if l == 0 else xc
            return src[t * P:(t + 1) * P, :]
        src = x_in if l == 0 else xl
        return src[(t - 2) * P:(t - 1) * P, :]

    def tok_dst(t):
        if t < 2:
            return xc[t * P:(t + 1) * P, :]
        return xl[(t - 2) * P:(t - 1) * P, :]

    def phase_ada(l):
        m = A.mark()
        cc = A.f32(32)
        scT = A.bf16(32)
        wsl = [v3(A.bf16(KC * 512), 512) for _ in range(2)]
        bsl = [A.f32(512) for _ in range(2)]
        msl = [A.f32(512) for _ in range(2)]
        dma("sp", cc, ccol, w=["cc"])
        op("act", lambda e: e.activation(scT, cc, AF.Silu), r=["cc"], w=["scT"])
        scT3 = v3(scT, 2)
        awv = ada_w[l].rearrange("(k p) n -> p k n", p=P)
        for n in range(24):
            s = n % 2
            cs = slice(n * 512, (n + 1) * 512)
            dma("pool", wsl[s], awv[:, :, cs], w=[("aw", s)])
            dma("sp", bsl[s][0:2, :], ada_b2[l][:, cs], w=[("ab", s)])
            for k in range(KC):
                op("pe", lambda e, k=k, s=s: e.matmul(bank(s)[0:2, :], scT3[:, k, :], wsl[s][:, k, :], start=(k == 0), stop=(k == KC - 1)),
                   r=["scT", ("aw", s)], w=[PSK(s)])
            op("dve", lambda e, s=s: e.tensor_tensor(msl[s][0:2, :], bank(s)[0:2, :], bsl[s][0:2, :], ALU.add),
               r=[PSK(s), ("ab", s)], w=[("ms", s)])
            dma("sp", modrow[:, cs], msl[s][0:2, :], r=[("ms", s)])
        S.barrier()
        A.release(m)

    def load_mod_bc(dst, r, seg, key):
        bc_row(dst, modrow[r:r + 1, seg * D:(seg + 1) * D], key)

    def phase_b0(l, ctx_any, hT):
        m = A.mark()
        gm = [A.f32(D) for _ in range(2)]
        sh = [A.f32(D) for _ in range(2)]
        xs = [A.f32(D) for _ in range(2)]
        hn = A.f32(D)
        gtmp = hn
        bc_row(gtmp, norm1_g[l:l + 1, :], "hn")
        for r in range(2):
            load_mod_bc(gm[r], r, 1, ("gm", r))
            load_mod_bc(sh[r], r, 0, ("sh", r))
            op("dve", lambda e, r=r: e.scalar_tensor_tensor(gm[r], gm[r], 1.0, gtmp, ALU.add, ALU.mult),
               r=["hn", ("gm", r)], w=[("gm", r)])
        hb = [A.bf16(D) for _ in range(2)]
        junk = hn.bitcast(BF16)[:, 0:D]
        ss = A.f32(2); t1 = A.f32(2); t2 = A.f32(2); rstd = A.f32(2)
        for t in range(NTOK // P):
            s = t % 2
            r = 1 if t < 2 else 0
            dma("sp", xs[s], tok_src(l, t), w=[("x", s)])
            op("act", lambda e, s=s: e.activation(junk, xs[s], AF.Square, accum_out=ss[:, 0:1]), r=[("x", s)], w=["hn", "b0ss"])
            rstd_from_ss(ss[:, 0:1], D, t1[:, 0:1], t2[:, 0:1], rstd[:, 0:1], "b0")
            op("dve", lambda e, s=s, r=r: e.scalar_tensor_tensor(hn, xs[s], rstd[:, 0:1], gm[r], ALU.mult, ALU.mult),
               r=[("x", s), "b0rstd", ("gm", r)], w=["hn"])
            op("pool", lambda e, s=s, r=r: e.tensor_tensor(hb[s], hn, sh[r], ALU.add), r=["hn", ("sh", r)], w=[("hb", s)])
            for k4 in range(4):
                bk = (t * 4 + k4) % 8
                for kk in range(4):
                    k = k4 * 4 + kk
                    op("pe", lambda e, s=s, k=k, kk=kk, bk=bk: e.transpose(bank16(bk)[:, kk * P:(kk + 1) * P], hb[s][:, k * P:(k + 1) * P], ident_b),
                       r=[("hb", s), "ident_b"], w=[PSK(bk)])
                eng = "act" if k4 % 2 == 0 else "dve"
                if eng == "act":
                    op("act", lambda e, k4=k4, bk=bk, t=t: e.copy(hT[:, k4 * 4:k4 * 4 + 4, t * P:(t + 1) * P], v3(bank16(bk)[:, 0:512], P)),
                       r=[PSK(bk)], w=[("hT", t)])
                else:
                    op("dve", lambda e, k4=k4, bk=bk, t=t: e.tensor_copy(hT[:, k4 * 4:k4 * 4 + 4, t * P:(t + 1) * P], v3(bank16(bk)[:, 0:512], P)),
                       r=[PSK(bk)], w=[("hT", t)])
        S.barrier()
        A.release(m)

    def tiles_for(ctx_on):
        tl = []
        if ctx_on:
            tl.append((0, 256, 256))
        for i in range(4):
            tl.append((256 + 512 * i, 512, 64))
        return tl

    def headnorm_store(y, W, gcol, chunk, tok0, bufs, it):
        sq, ms, sd, yn = bufs
        pb = 3 + 4 * (it % 2)
        kp = "hn%d" % (it % 2)
        op("act", lambda e: e.activation(sq[:, 0:W], y[:, 0:W], AF.Square), r=[kp + "y"], w=[kp + "sq"])
        op("pe", lambda e: e.matmul(bank(pb)[:, 0:W], ones_b, sq[:, 0:W], start=True, stop=True), r=[kp + "sq", "ones_b"], w=[PSK(pb)])
        op("dve", lambda e: e.tensor_scalar(ms[:, 0:W], bank(pb)[:, 0:W], 1.0 / P, EPS, ALU.mult, ALU.add), r=[PSK(pb)], w=[kp + "ms"])
        op("act", lambda e: e.activation(sd[:, 0:W], ms[:, 0:W], AF.Sqrt), r=[kp + "ms"], w=[kp + "sd"])
        op("dve", lambda e: e.reciprocal(ms[:, 0:W], sd[:, 0:W]), r=[kp + "sd"], w=[kp + "ms"])
        op("dve", lambda e: e.scalar_tensor_tensor(yn[:, 0:W], y[:, 0:W], gcol, ms[:, 0:W], ALU.mult, ALU.mult),
           r=[kp + "y", kp + "ms", "gcols"], w=[kp + "yn"])
        dma("sp", mixn[chunk][:, tok0:tok0 + W], yn[:, 0:W], r=[kp + "yn"])

    def phase_b1(l, ctx_full, hT, uT):
        m = A.mark()
        cw = A.f32(36)
        gc = A.f32(12)
        dma("sp", cw, conv_wc[l], w=["cw"])
        dma("sp", gc, gconv[l], w=["gcols"])
        cw3 = v3(cw, 3)
        wsl = [[v3(A.bf16(KC * P), P) for _ in range(3)] for _ in range(2)]
        tmp = []
        for i in range(2):
            tmp.append(dict(c=A.f32(512), z=A.f32(512), acc=A.f32(512), y=A.f32(512),
                            sq=A.bf16(512), ms=A.f32(512), sd=A.f32(512), yn=A.bf16(512)))
        it = 0
        for k in range(12):
            ws = wsl[k % 2]
            for j in range(3):
                dma("pool", ws[j], w_in_t[l, j * 12 + k], w=[("win", k % 2, j)])
            for (tok0, W, RW) in tiles_for(ctx_full):
                T = tmp[it % 2]
                b0 = 4 * (it % 2)
                kp = "hn%d" % (it % 2)
                for j in range(3):
                    for kk in range(KC):
                        op("pe", lambda e, j=j, kk=kk, b0=b0, ws=ws, tok0=tok0, W=W: e.matmul(
                            bank(b0 + j)[:, 0:W], ws[j][:, kk, :], hT[:, kk, tok0:tok0 + W], start=(kk == 0), stop=(kk == KC - 1)),
                           r=[("win", k % 2, j)] + [("hT", tt) for tt in range(tok0 // P, (tok0 + W) // P)], w=[PSK(b0 + j)])
                op("act", lambda e, T=T, b0=b0, W=W: e.copy(T["c"][:, 0:W], bank(b0 + 1)[:, 0:W]), r=[PSK(b0 + 1)], w=[kp + "c"])
                op("dve", lambda e, T=T, b0=b0, W=W: e.tensor_tensor(T["z"][:, 0:W], bank(b0 + 2)[:, 0:W], T["c"][:, 0:W], ALU.mult),
                   r=[PSK(b0 + 2), kp + "c"], w=[kp + "z"])
                op("act", lambda e, T=T, W=W, k=k: e.activation(T["acc"][:, 0:W], T["z"][:, 0:W], AF.Copy, scale=cw3[:, k, 1:2]),
                   r=[kp + "z", "cw"], w=[kp + "acc"])
                zv = v3(T["z"][:, 0:W], RW)
                av = v3(T["acc"][:, 0:W], RW)
                op("dve", lambda e, zv=zv, av=av, k=k, RW=RW: e.scalar_tensor_tensor(av[:, :, 1:RW], zv[:, :, 0:RW - 1], cw3[:, k, 0:1], av[:, :, 1:RW], ALU.mult, ALU.add),
                   r=[kp + "z", kp + "acc", "cw"], w=[kp + "acc"])
                op("dve", lambda e, zv=zv, av=av, k=k, RW=RW: e.scalar_tensor_tensor(av[:, :, 0:RW - 1], zv[:, :, 1:RW], cw3[:, k, 2:3], av[:, :, 0:RW - 1], ALU.mult, ALU.add),
                   r=[kp + "z", kp + "acc", "cw"], w=[kp + "acc"])
                op("dve", lambda e, T=T, b0=b0, W=W: e.tensor_tensor(T["y"][:, 0:W], bank(b0)[:, 0:W], T["acc"][:, 0:W], ALU.mult),
                   r=[PSK(b0), kp + "acc"], w=[kp + "y"])
                headnorm_store(T["y"], W, gc[:, k:k + 1], k, tok0, (T["sq"], T["ms"], T["sd"], T["yn"]), it)
                it += 1
        for cq in range(4):
            ws = wsl[cq % 2][0]
            dma("pool", ws, w_in_t[l, 36 + cq], w=[("win", cq % 2, 0)])
            for (tok0, W, RW) in tiles_for(True):
                b0 = 4 * (it % 2)
                for kk in range(KC):
                    op("pe", lambda e, kk=kk, b0=b0, ws=ws, tok0=tok0, W=W: e.matmul(
                        bank(b0)[:, 0:W], ws[:, kk, :], hT[:, kk, tok0:tok0 + W], start=(kk == 0), stop=(kk == KC - 1)),
                       r=[("win", cq % 2, 0)] + [("hT", tt) for tt in range(tok0 // P, (tok0 + W) // P)], w=[PSK(b0)])
                op("act", lambda e, b0=b0, cq=cq, tok0=tok0, W=W: e.copy(uT[:, cq, tok0:tok0 + W], bank(b0)[:, 0:W]),
                   r=[PSK(b0)], w=[("uT", cq, tok0)])
                it += 1
        S.barrier()
        A.release(m)

    def reduce_angle(dst, kd, x, kx, n, tf, ti):
        op("dve", lambda e: e.tensor_scalar(tf, x, 1.0 / TWO_PI, None, ALU.mult), r=[kx], w=["ra_tf"])
        op("dve", lambda e: e.tensor_copy(ti, tf), r=["ra_tf"], w=["ra_ti"])
        op("dve", lambda e: e.tensor_copy(tf, ti), r=["ra_ti"], w=["ra_tf"])
        op("dve", lambda e: e.scalar_tensor_tensor(dst, tf, -TWO_PI, x, ALU.mult, ALU.add), r=["ra_tf", kx], w=[kd])
        op("dve", lambda e: e.tensor_scalar(tf, dst, math.pi, -TWO_PI, ALU.is_gt, ALU.mult), r=[kd], w=["ra_tf"])
        op("dve", lambda e: e.tensor_tensor(dst, dst, tf, ALU.add), r=["ra_tf", kd], w=[kd])
        op("dve", lambda e: e.tensor_scalar(tf, dst, -math.pi, TWO_PI, ALU.is_lt, ALU.mult), r=[kd], w=["ra_tf"])
        op("dve", lambda e: e.tensor_tensor(dst, dst, tf, ALU.add), r=["ra_tf", kd], w=[kd])
        op("dve", lambda e: e.tensor_scalar(dst, dst, PI_LO, -PI_LO, ALU.min, ALU.max), r=[kd], w=[kd])

    def sincos(sin_out, ks, cos_out, kc, x, kx, n, tf, ti, xs, red, neg_sin=False):
        reduce_angle(red, "ra_red", x, kx, n, tf, ti)
        op("act", lambda e: e.activation(sin_out, red, AF.Sin, scale=(-1.0 if neg_sin else 1.0)), r=["ra_red"], w=[ks])
        op("dve", lambda e: e.tensor_scalar(xs, x, math.pi / 2, None, ALU.add), r=[kx], w=["ra_xs"])
        reduce_angle(red, "ra_red", xs, "ra_xs", n, tf, ti)
        op("act", lambda e: e.activation(cos_out, red, AF.Sin), r=["ra_red"], w=[kc])

    def phase_b2(l, ctx_out, uT):
        m = A.mark()
        ysum = v3(A.f32(4 * NTOK), NTOK)
        NT = 16 * P
        cosT = A.f32(NT); nsinT = A.f32(NT); RKre = A.f32(NT); RKim = A.f32(NT); MAG0 = A.f32(NT)
        BTre = v3(A.bf16(NT), P); BTim = v3(A.bf16(NT), P); CTre = v3(A.bf16(NT), P); CTim = v3(A.bf16(NT), P)
        small = [A.f32(16) for _ in range(26)]
        (lr, li, ldt, dt, lrdt, th, mag, s1, c1, ar, ai, nr, den, kr, ki, thr, tA, tB, Ere, Eim, th128, s128, c128, tC, tD, tE) = small
        HW = 8 * P
        QW = 4 * P
        WS = []
        big = A.f32(4 * 6 * QW)
        for q_ in range(4):
            o_ = q_ * 6 * QW
            WS.append(dict(t1=big[:, o_:o_ + QW], t2=big[:, o_ + QW:o_ + 2 * QW], t3=big[:, o_ + 2 * QW:o_ + 3 * QW],
                           t4=big[:, o_ + 3 * QW:o_ + 4 * QW], gre=big[:, o_ + 4 * QW:o_ + 5 * QW], gim=big[:, o_ + 5 * QW:o_ + 6 * QW],
                           hre=A.bf16(QW), nhim=A.bf16(QW), ytmp=A.f32(P),
                           carry=[A.f32(4), A.f32(4)], ctmp=[A.f32(4) for _ in range(4)]))
        wkA = big[:, 0:NT]; wkB = big[:, NT:2 * NT]; wkC = big[:, 2 * NT:3 * NT]; gg = big[:, 3 * NT:4 * NT]

        def sm(o, a, b, alu, keys_r, key_w):
            op("dve", lambda e: e.tensor_tensor(o, a, b, alu), r=keys_r, w=[key_w])

        for d in range(2):
            dma("sp", lr, lamre[l, d], w=["lr"])
            dma("sp", li, lamim[l, d], w=["li"])
            dma("sp", ldt, logdt[l, d], w=["ldt"])
            for (dst, src) in ((BTre, bt_re), (BTim, bt_im), (CTre, ct_re), (CTim, ct_im)):
                dma("pool", dst.rearrange("p (c j) q -> p c (j q)", c=4), src[l, d].rearrange("c p x -> p c x"), w=["BC"])
            op("act", lambda e: e.activation(dt, ldt, AF.Exp), r=["ldt"], w=["dt"])
            sm(lrdt, lr, dt, ALU.mult, ["lr", "dt"], "lrdt")
            sm(th, li, dt, ALU.mult, ["li", "dt"], "th")
            op("act", lambda e: e.activation(mag, lrdt, AF.Exp), r=["lrdt"], w=["mag"])
            tBi = tB.bitcast(I32)
            sincos(s1, "s1", c1, "c1", th, "th", 16, tA, tBi, tC, tD)
            reduce_angle(thr, "thr", th, "th", 16, tA, tBi)
            sm(ar, mag, c1, ALU.mult, ["mag", "c1"], "ar")
            sm(ai, mag, s1, ALU.mult, ["mag", "s1"], "ai")
            op("dve", lambda e: e.tensor_scalar(nr, ar, -1.0, None, ALU.add), r=["ar"], w=["nr"])
            sm(den, lr, lr, ALU.mult, ["lr"], "den")
            sm(tE, li, li, ALU.mult, ["li"], "tE")
            sm(den, den, tE, ALU.add, ["den", "tE"], "den")
            op("dve", lambda e: e.reciprocal(den, den), r=["den"], w=["den"])
            sm(kr, nr, lr, ALU.mult, ["nr", "lr"], "kr")
            sm(tE, ai, li, ALU.mult, ["ai", "li"], "tE")
            sm(kr, kr, tE, ALU.add, ["kr", "tE"], "kr")
            sm(kr, kr, den, ALU.mult, ["kr", "den"], "kr")
            sm(ki, ai, lr, ALU.mult, ["ai", "lr"], "ki")
            sm(tE, nr, li, ALU.mult, ["nr", "li"], "tE")
            sm(ki, ki, tE, ALU.subtract, ["ki", "tE"], "ki")
            sm(ki, ki, den, ALU.mult, ["ki", "den"], "ki")
            op("dve", lambda e: e.tensor_scalar(th128, thr, 128.0, None, ALU.mult), r=["thr"], w=["th128"])
            sincos(s128, "s128", c128, "c128", th128, "th128", 16, tA, tBi, tC, tD)
            sm(Ere, mag, c128, ALU.mult, ["mag", "c128"], "Ere")
            sm(Eim, mag, s128, ALU.mult, ["mag", "s128"], "Eim")
            op("dve", lambda e: e.tensor_tensor(v3(wkA, P), thr.unsqueeze(2).to_broadcast([P, 16, P]), iota_t.unsqueeze(1).to_broadcast([P, 16, P]), ALU.mult),
               r=["thr"], w=["ang"])
            sincos(nsinT, "nsinT", cosT, "cosT", wkA, "ang", NT, wkB, gg.bitcast(I32), wkC, MAG0, neg_sin=True)
            krb = kr.unsqueeze(2).to_broadcast([P, 16, P]); kib = ki.unsqueeze(2).to_broadcast([P, 16, P])
            op("dve", lambda e: e.tensor_tensor(v3(RKre, P), v3(cosT, P), krb, ALU.mult), r=["cosT", "kr"], w=["RKre"])
            op("dve", lambda e: e.tensor_tensor(v3(wkA, P), v3(nsinT, P), kib, ALU.mult), r=["nsinT", "ki", "ang"], w=["ang"])
            op("dve", lambda e: e.tensor_tensor(RKre, RKre, wkA, ALU.subtract), r=["RKre", "ang"], w=["RKre"])
            op("dve", lambda e: e.tensor_tensor(v3(RKim, P), v3(cosT, P), kib, ALU.mult), r=["cosT", "ki"], w=["RKim"])
            op("dve", lambda e: e.tensor_tensor(v3(wkA, P), v3(nsinT, P), krb, ALU.mult), r=["nsinT", "kr", "ang", "RKre"], w=["ang"])
            op("dve", lambda e: e.tensor_tensor(RKim, RKim, wkA, ALU.add), r=["RKim", "ang"], w=["RKim"])
            op("dve", lambda e: e.tensor_copy(v3(MAG0, P), mag.unsqueeze(2).to_broadcast([P, 16, P])), r=["mag", "ra_red"], w=["MAG0"])
            op("dve", lambda e: e.memset(v3(MAG0, P)[:, :, 0:1], 0.0), r=["MAG0"], w=["MAG0"])
            S.barrier()

            if d == 0:
                order = list(range(18))
            else:
                order = [1, 0] + list(range(17, 1, -1))
            pending = {}

            def KQ(n, q):
                return (n, q)

            def S1(c, q):
                tsl = slice(c * P, (c + 1) * P)
                rhs = uT[:, q, tsl] if d == 0 else uT[:, q, tsl][:, ::-1]
                for jl in range(4):
                    j = 4 * q + jl
                    op("pe", lambda e, jl=jl, j=j, rhs=rhs, q=q: e.matmul(bank(2 * q)[:, jl * P:(jl + 1) * P], BTre[:, j, :], rhs, start=True, stop=True),
                       r=["BC"], w=[PSK(2 * q)])
                    op("pe", lambda e, jl=jl, j=j, rhs=rhs, q=q: e.matmul(bank(2 * q + 1)[:, jl * P:(jl + 1) * P], BTim[:, j, :], rhs, start=True, stop=True),
                       r=["BC"], w=[PSK(2 * q + 1)])

            def S2(c, q, first):
                W_ = WS[q]
                t1 = W_["t1"]; t2 = W_["t2"]; t3 = W_["t3"]; t4 = W_["t4"]
                fs = slice(q * QW, (q + 1) * QW)
                if q in pending:
                    cq_, tsl_ = pending.pop(q)
                    op("pool", lambda e, W_=W_, cq_=cq_, tsl_=tsl_: e.tensor_tensor(ysum[:, cq_, tsl_], ysum[:, cq_, tsl_], W_["ytmp"], ALU.add),
                       r=[KQ("ytmp", q)], w=[("ys", cq_, tsl_.start)])
                op("dve", lambda e, t1=t1, fs=fs, q=q: e.tensor_tensor(t1, RKre[:, fs], bank(2 * q), ALU.mult), r=[PSK(2 * q)], w=[KQ("t1", q)])
                op("dve", lambda e, t3=t3, fs=fs, q=q: e.tensor_tensor(t3, RKre[:, fs], bank(2 * q + 1), ALU.mult), r=[PSK(2 * q + 1)], w=[KQ("t3", q)])
                op("dve", lambda e, t4=t4, fs=fs, q=q: e.tensor_tensor(t4, RKim[:, fs], bank(2 * q), ALU.mult), r=[PSK(2 * q)], w=[KQ("t4", q)])
                op("dve", lambda e, t2=t2, fs=fs, q=q: e.tensor_tensor(t2, RKim[:, fs], bank(2 * q + 1), ALU.mult), r=[PSK(2 * q + 1)], w=[KQ("t2", q)])
                op("pool", lambda e, t1=t1, t2=t2: e.tensor_tensor(t1, t1, t2, ALU.subtract), r=[KQ("t1", q), KQ("t2", q)], w=[KQ("t1", q)])
                op("dve", lambda e, t3=t3, t4=t4: e.tensor_tensor(t3, t3, t4, ALU.add), r=[KQ("t3", q), KQ("t4", q)], w=[KQ("t3", q)])
                if not first:
                    br3 = v3(t1, P); bi3 = v3(t3, P)
                    op("pool", lambda e, W_=W_, br3=br3: e.tensor_tensor(br3[:, :, 0:1], br3[:, :, 0:1], W_["carry"][0].unsqueeze(2), ALU.add),
                       r=[KQ("t1", q), KQ("carry", q)], w=[KQ("t1", q)])
                    op("pool", lambda e, W_=W_, bi3=bi3: e.tensor_tensor(bi3[:, :, 0:1], bi3[:, :, 0:1], W_["carry"][1].unsqueeze(2), ALU.add),
                       r=[KQ("t3", q), KQ("carry", q)], w=[KQ("t3", q)])

            def S3(c, q):
                W_ = WS[q]
                t1 = W_["t1"]; t3 = W_["t3"]; gre = W_["gre"]; gim = W_["gim"]
                fs = slice(q * QW, (q + 1) * QW)
                js = slice(4 * q, 4 * q + 4)
                op("dve", lambda e, fs=fs, gre=gre, t1=t1: e.tensor_tensor_scan(gre, MAG0[:, fs], t1, 0.0, ALU.mult, ALU.add), r=[KQ("t1", q)], w=[KQ("gre", q)])
                op("dve", lambda e, fs=fs, gim=gim, t3=t3: e.tensor_tensor_scan(gim, MAG0[:, fs], t3, 0.0, ALU.mult, ALU.add), r=[KQ("t3", q)], w=[KQ("gim", q)])
                glr = v3(gre, P)[:, :, P - 1]; gli = v3(gim, P)[:, :, P - 1]
                ct_ = W_["ctmp"]; cy_ = W_["carry"]
                op("pool", lambda e, js=js, glr=glr, ct_=ct_: e.tensor_tensor(ct_[0], Ere[:, js], glr, ALU.mult), r=[KQ("gre", q)], w=[KQ("c0", q)])
                op("pool", lambda e, js=js, gli=gli, ct_=ct_: e.tensor_tensor(ct_[1], Eim[:, js], gli, ALU.mult), r=[KQ("gim", q)], w=[KQ("c1", q)])
                op("pool", lambda e, cy_=cy_, ct_=ct_: e.tensor_tensor(cy_[0], ct_[0], ct_[1], ALU.subtract), r=[KQ("c0", q), KQ("c1", q)], w=[KQ("carry", q)])
                op("pool", lambda e, js=js, gli=gli, ct_=ct_: e.tensor_tensor(ct_[2], Ere[:, js], gli, ALU.mult), r=[KQ("gim", q)], w=[KQ("c2", q)])
                op("pool", lambda e, js=js, glr=glr, ct_=ct_: e.tensor_tensor(ct_[3], Eim[:, js], glr, ALU.mult), r=[KQ("gre", q)], w=[KQ("c3", q)])
                op("pool", lambda e, cy_=cy_, ct_=ct_: e.tensor_tensor(cy_[1], ct_[2], ct_[3], ALU.add), r=[KQ("c2", q), KQ("c3", q), KQ("carry", q)], w=[KQ("carry", q)])

            def S4(c, q):
                W_ = WS[q]
                t1 = W_["t1"]; t2 = W_["t2"]; t3 = W_["t3"]; t4 = W_["t4"]
                gre = W_["gre"]; gim = W_["gim"]; hre = W_["hre"]; nhim = W_["nhim"]
                fs = slice(q * QW, (q + 1) * QW)
                op("pool", lambda e, fs=fs, t1=t1, gre=gre: e.tensor_tensor(t1, cosT[:, fs], gre, ALU.mult), r=[KQ("gre", q)], w=[KQ("t1", q)])
                op("pool", lambda e, fs=fs, t2=t2, gim=gim: e.tensor_tensor(t2, nsinT[:, fs], gim, ALU.mult), r=[KQ("gim", q)], w=[KQ("t2", q)])
                op("dve", lambda e, fs=fs, t3=t3, gre=gre: e.tensor_tensor(t3, nsinT[:, fs], gre, ALU.mult), r=[KQ("gre", q)], w=[KQ("t3", q)])
                op("pool", lambda e, fs=fs, t4=t4, gim=gim: e.tensor_tensor(t4, cosT[:, fs], gim, ALU.mult), r=[KQ("gim", q)], w=[KQ("t4", q)])
                op("dve", lambda e, hre=hre, t1=t1, t2=t2: e.tensor_tensor(hre, t1, t2, ALU.add), r=[KQ("t1", q), KQ("t2", q)], w=[KQ("hre", q)])
                op("dve", lambda e, nhim=nhim, t3=t3, t4=t4: e.tensor_tensor(nhim, t3, t4, ALU.subtract), r=[KQ("t3", q), KQ("t4", q)], w=[KQ("nhim", q)])

            def S5(c, q):
                W_ = WS[q]
                tsl = slice(c * P, (c + 1) * P)
                hre3 = v3(W_["hre"], P); nhim3 = v3(W_["nhim"], P)
                pb = 2 * q
                for j4 in range(4):
                    j = q * 4 + j4
                    op("pe", lambda e, j=j, j4=j4, pb=pb, hre3=hre3, d_=d: e.matmul(bank(pb)[:, 0:P], CTre[:, j, :], (hre3[:, j4, :] if d_ == 0 else hre3[:, j4, :][:, ::-1]), start=(j4 == 0), stop=False),
                       r=["BC", KQ("hre", q)], w=[PSK(pb)])
                    op("pe", lambda e, j=j, j4=j4, pb=pb, nhim3=nhim3, d_=d: e.matmul(bank(pb)[:, 0:P], CTim[:, j, :], (nhim3[:, j4, :] if d_ == 0 else nhim3[:, j4, :][:, ::-1]), start=False, stop=(j4 == 3)),
                       r=["BC", KQ("nhim", q)], w=[PSK(pb)])
                if d == 0:
                    op("act", lambda e, q=q, pb=pb, tsl=tsl: e.copy(ysum[:, q, tsl], bank(pb)[:, 0:P]), r=[PSK(pb)], w=[("ys", q, tsl.start)])
                else:
                    op("act", lambda e, W_=W_, pb=pb: e.copy(W_["ytmp"], bank(pb)[:, 0:P]), r=[PSK(pb)], w=[KQ("ytmp", q)])
                    pending[q] = (q, tsl)

            pairs = [(0, 1), (2, 3)]
            n_ = len(order)
            for pr in pairs:
                for q in pr:
                    S1(order[0], q)
            for i, c in enumerate(order):
                need_out = ctx_out or c >= 2
                for pr in pairs:
                    for q in pr:
                        S2(c, q, i == 0)
                    for q in pr:
                        S3(c, q)
                    if need_out:
                        for q in pr:
                            S4(c, q)
                        for q in pr:
                            S5(c, q)
                    if i + 1 < n_:
                        for q in pr:
                            S1(order[i + 1], q)
            for q in list(pending.keys()):
                cq_, tsl_ = pending.pop(q)
                op("pool", lambda e, q=q, cq_=cq_, tsl_=tsl_: e.tensor_tensor(ysum[:, cq_, tsl_], ysum[:, cq_, tsl_], WS[q]["ytmp"], ALU.add),
                   r=[KQ("ytmp", q)], w=[("ys", cq_, tsl_.start)])
            S.barrier()

        tokr = slice(0, NTOK) if ctx_out else slice(NCTX, NTOK)
        NTK = NTOK if ctx_out else NLAT
        A.release(m)
        ysum = v3(A.f32(4 * NTOK), NTOK)
        dcc = A.f32(4); gsc = A.f32(4)
        dma("sp", dcc, dcol[l], w=["dc"])
        dma("sp", gsc, gssm[l], w=["gcols"])
        wg = v3(A.bf16(4 * 512), 512)
        dma("pool", wg, w_glu[l].rearrange("(k p) n -> p k n", p=P), w=["wg"])
        gl16 = v3(A.bf16(4 * NTOK), NTOK)
        w1 = A.f32(NTOK); w2 = A.f32(NTOK)
        for cq in range(4):
            yv = ysum[:, cq, tokr]
            op("dve", lambda e, cq=cq, yv=yv: e.scalar_tensor_tensor(yv, uT[:, cq, tokr], dcc[:, cq:cq + 1], yv, ALU.mult, ALU.add),
               r=["dc", "uTall"], w=[("yt", cq)])
            if "dbg_y" in dbg:
                dma("sp", dbg_y[cq], ysum[:, cq, :], r=[("yt", cq)])
            op("pool", lambda e, yv=yv: e.tensor_tensor(w1[:, 0:NTK], yv, yv, ALU.mult), r=[("yt", cq)], w=["w1"])
            op("dve", lambda e: e.tensor_scalar(w1[:, 0:NTK], w1[:, 0:NTK], 0.044715, 1.0, ALU.mult, ALU.add), r=["w1"], w=["w1"])
            op("pool", lambda e, yv=yv: e.tensor_tensor(w2[:, 0:NTK], w1[:, 0:NTK], yv, ALU.mult), r=["w1", ("yt", cq)], w=["w2"])
            op("act", lambda e: e.activation(w1[:, 0:NTK], w2[:, 0:NTK], AF.Sigmoid, scale=1.5957691216057308), r=["w2", "w1"], w=["w1"])
            op("dve", lambda e, yv=yv: e.tensor_tensor(yv, yv, w1[:, 0:NTK], ALU.mult), r=["w1", ("yt", cq)], w=[("yt", cq)])
            op("act", lambda e, cq=cq, yv=yv: e.copy(gl16[:, cq, tokr], yv), r=[("yt", cq)], w=[("gl16", cq)])
        tmp = []
        for i in range(2):
            tmp.append(dict(sg=A.f32(512), o=A.f32(512), sq=A.bf16(512), ms=A.f32(512), sd=A.f32(512), yn=A.bf16(512)))
        it = 0
        for oc in range(4):
            for (tok0, W, RW) in tiles_for(ctx_out):
                T = tmp[it % 2]
                b0 = 4 * (it % 2)
                kp = "hn%d" % (it % 2)
                for k in range(4):
                    op("pe", lambda e, k=k, oc=oc, b0=b0, tok0=tok0, W=W: e.matmul(bank(b0)[:, 0:W], wg[:, k, oc * P:(oc + 1) * P], gl16[:, k, tok0:tok0 + W], start=(k == 0), stop=(k == 3)),
                       r=["wg"] + [("gl16", kk) for kk in range(4)], w=[PSK(b0)])
                op("act", lambda e, T=T, b0=b0, W=W: e.activation(T["sg"][:, 0:W], bank(b0)[:, 0:W], AF.Sigmoid), r=[PSK(b0)], w=[kp + "sg"])
                op("dve", lambda e, T=T, oc=oc, tok0=tok0, W=W: e.tensor_tensor(T["o"][:, 0:W], ysum[:, oc, tok0:tok0 + W], T["sg"][:, 0:W], ALU.mult),
                   r=[kp + "sg", ("yt", oc)], w=[kp + "y"])
                headnorm_store(T["o"], W, gsc[:, oc:oc + 1], 12 + oc, tok0, (T["sq"], T["ms"], T["sd"], T["yn"]), it)
                it += 1
        S.barrier()
        A.release(m)

    def phase_b3(l, ctx_full):
        m = A.mark()
        wo = v3(A.bf16(KC * D), D)
        wov = w_out[l].rearrange("(k p) n -> p k n", p=P)
        for cg in range(4):
            dma("pool", wo[:, :, cg * 512:(cg + 1) * 512], wov[:, :, cg * 512:(cg + 1) * 512], w=[("wo", cg)])
        g1 = [A.f32(D) for _ in range(2)]
        for r in range(2):
            load_mod_bc(g1[r], r, 2, ("g1", r))
        mts = [v3(A.bf16(KC * 512), 512) for _ in range(2)]
        xs = [A.f32(D) for _ in range(2)]
        xo = [A.f32(D) for _ in range(2)]
        tq = A.f32(512)
        t_list = list(range(0 if ctx_full else 2, 18))
        loaded = {}
        nload = 0
        for idx_t, t in enumerate(t_list):
            s = idx_t % 2
            r = 1 if t < 2 else 0
            grp = -1 if t < 2 else (t - 2) // 4
            if grp not in loaded:
                ms_ = nload % 2
                nload += 1
                tok0, W = (0, 256) if grp < 0 else (256 + grp * 512, 512)
                dma("sp", mts[ms_][:, :, 0:W], mixn[:, :, tok0:tok0 + W].rearrange("c p t -> p c t"), w=[("mt", ms_)])
                loaded[grp] = (ms_, tok0)
            ms_, tok0 = loaded[grp]
            lo = t * P - tok0
            dma("sp", xs[s], tok_src(l, t), w=[("x", s)])
            for cg in range(4):
                pb = (idx_t * 4 + cg) % 8
                cs = slice(cg * 512, (cg + 1) * 512)
                for k in range(KC):
                    op("pe", lambda e, k=k, ms_=ms_, lo=lo, cs=cs, pb=pb: e.matmul(bank(pb), mts[ms_][:, k, lo:lo + P], wo[:, k, cs], start=(k == 0), stop=(k == KC - 1)),
                       r=[("mt", ms_), ("wo", cg)], w=[PSK(pb)])
                op("dve", lambda e, pb=pb, r=r, cs=cs: e.tensor_tensor(tq, bank(pb), g1[r][:, cs], ALU.mult), r=[PSK(pb), ("g1", r)], w=["tq"])
                op("pool", lambda e, s=s, cs=cs: e.tensor_tensor(xo[s][:, cs], tq, xs[s][:, cs], ALU.add), r=["tq", ("x", s)], w=[("xo", s)])
            dma("sp", tok_dst(t), xo[s], r=[("xo", s)])
        S.barrier()
        A.release(m)

    def phase_c0(l, ctx_full, affT):
        m = A.mark()
        gm = [A.f32(D) for _ in range(2)]
        sh = [A.f32(D) for _ in range(2)]
        gtmp = A.f32(D)
        bc_row(gtmp, norm2_g[l:l + 1, :], "g2row")
        for r in range(2):
            load_mod_bc(gm[r], r, 4, ("gm", r))
            load_mod_bc(sh[r], r, 3, ("sh", r))
            op("dve", lambda e, r=r: e.scalar_tensor_tensor(gm[r], gm[r], 1.0, gtmp, ALU.add, ALU.mult),
               r=["g2row", ("gm", r)], w=[("gm", r)])
        rw = v3(A.f32(KC * NE), NE)
        dma("sp", rw, router_w[l].rearrange("(k p) n -> p k n", p=P), w=["rw"])
        xs = [A.f32(D) for _ in range(2)]
        hn = A.f32(D)
        h2f = [A.f32(D) for _ in range(2)]
        h2b = [A.bf16(D) for _ in range(2)]
        h2T = v3(A.f32(D), P)
        junk = A.bf16(D)
        ss = A.f32(2); t1 = A.f32(2); t2 = A.f32(2); rstd = A.f32(2)
        mx = A.f32(2); se = A.f32(2); ex = A.f32(NE); aff = A.f32(NE)
        t_list = list(range(0 if ctx_full else 2, 18))
        for it, t in enumerate(t_list):
            s = it % 2
            r = 1 if t < 2 else 0
            dma("sp", xs[s], tok_dst(t), w=[("x", s)])
            op("act", lambda e, s=s: e.activation(junk, xs[s], AF.Square, accum_out=ss[:, 0:1]), r=[("x", s)], w=["junk", "c0ss"])
            rstd_from_ss(ss[:, 0:1], D, t1[:, 0:1], t2[:, 0:1], rstd[:, 0:1], "c0")
            op("dve", lambda e, s=s, r=r: e.scalar_tensor_tensor(hn, xs[s], rstd[:, 0:1], gm[r], ALU.mult, ALU.mult),
               r=[("x", s), "c0rstd", ("gm", r)], w=["hn"])
            op("pool", lambda e, s=s, r=r: e.tensor_tensor(h2f[s], hn, sh[r], ALU.add), r=["hn", ("sh", r)], w=[("h2f", s)])
            op("act", lambda e, s=s: e.copy(h2b[s], h2f[s]), r=[("h2f", s)], w=[("h2b", s)])
            dst = h2c[t * P:(t + 1) * P, :] if t < 2 else h2t[(t - 2) * P:(t - 1) * P, :]
            dma("sp", dst, h2b[s], r=[("h2b", s)])
            for k4 in range(4):
                bk = k4
                for kk in range(4):
                    k = k4 * 4 + kk
                    op("pe", lambda e, s=s, k=k, kk=kk, bk=bk: e.transpose(bank(bk)[:, kk * P:(kk + 1) * P], h2f[s][:, k * P:(k + 1) * P], ident_f),
                       r=[("h2f", s), "ident_f"], w=[PSK(bk)])
                if k4 % 2 == 0:
                    op("act", lambda e, k4=k4, bk=bk: e.copy(h2T[:, k4 * 4:k4 * 4 + 4, :], v3(bank(bk), P)), r=[PSK(bk)], w=[("h2T", k4)])
                else:
                    op("dve", lambda e, k4=k4, bk=bk: e.tensor_copy(h2T[:, k4 * 4:k4 * 4 + 4, :], v3(bank(bk), P)), r=[PSK(bk)], w=[("h2T", k4)])
            pl = 4 + (it % 2)
            for k in range(KC):
                op("pe", lambda e, k=k, pl=pl: e.matmul(bank(pl)[:, 0:NE], h2T[:, k, :], rw[:, k, :], start=(k == 0), stop=(k == KC - 1)),
                   r=[("h2T", k // 4), "rw"], w=[PSK(pl)])
            op("dve", lambda e, pl=pl: e.reduce_max(mx[:, 0:1], bank(pl)[:, 0:NE], AX.X), r=[PSK(pl)], w=["mx"])
            op("dve", lambda e: e.tensor_scalar(mx[:, 0:1], mx[:, 0:1], -1.0, None, ALU.mult), r=["mx"], w=["mx"])
            op("act", lambda e, pl=pl: e.activation(ex, bank(pl)[:, 0:NE], AF.Exp, bias=mx[:, 0:1], accum_out=se[:, 0:1]), r=[PSK(pl), "mx"], w=["ex", "se"])
            op("dve", lambda e: e.reciprocal(se[:, 0:1], se[:, 0:1]), r=["se"], w=["se"])
            op("dve", lambda e: e.tensor_scalar(aff, ex, se[:, 0:1], None, ALU.mult), r=["ex", "se"], w=["aff"])
            pt = 6 + (it % 2)
            op("pe", lambda e, pt=pt: e.transpose(bank(pt)[0:NE, 0:P], aff, ident_f), r=["aff", "ident_f"], w=[PSK(pt)])
            op("act", lambda e, pt=pt, t=t: e.copy(affT[0:NE, t * P:(t + 1) * P], bank(pt)[0:NE, 0:P]), r=[PSK(pt)], w=[("affT", t)])
        if "dbg_aff" in dbg:
            dma("sp", dbg_aff, affT[0:NE, :], r=[("affT", t) for t in t_list])
        S.barrier()
        A.release(m)

    def phase_c1(ctx_full, affT, idxT, valT):
        m = A.mark()
        work = A.f32(NLAT)
        vals = A.f32(288)
        idx = A.u32(288)
        idxf = A.f32(288)
        op("dve", lambda e: e.tensor_copy(work[0:NE, :], affT[0:NE, NCTX:NTOK]), w=["work"])
        for r_ in range(32):
            vs = slice(r_ * 8, r_ * 8 + 8)
            op("dve", lambda e, vs=vs: e.max(vals[0:NE, vs], work[0:NE, :]), r=["work"], w=["vals"])
            op("dve", lambda e, vs=vs: e.max_index(idx[0:NE, vs], vals[0:NE, vs], work[0:NE, :]), r=["work", "vals"], w=["idx"])
            op("dve", lambda e, vs=vs: e.match_replace(work[0:NE, :], vals[0:NE, vs], work[0:NE, :], -1.0), r=["work", "vals", "idx"], w=["work"])
        if ctx_full:
            wc = work[0:NE, 0:NCTX]
            op("dve", lambda e: e.tensor_copy(wc, affT[0:NE, 0:NCTX]), r=["work"], w=["work"])
            for r_ in range(4):
                vs = slice(256 + r_ * 8, 256 + r_ * 8 + 8)
                op("dve", lambda e, vs=vs: e.max(vals[0:NE, vs], wc), r=["work"], w=["vals"])
                op("dve", lambda e, vs=vs: e.max_index(idx[0:NE, vs], vals[0:NE, vs], wc), r=["work", "vals"], w=["idx"])
                op("dve", lambda e, vs=vs: e.match_replace(wc, vals[0:NE, vs], wc, -1.0), r=["work", "vals", "idx"], w=["work"])
        NS = 288 if ctx_full else 256
        op("dve", lambda e: e.tensor_copy(idxf[0:NE, 0:NS], idx[0:NE, 0:NS]), r=["idx"], w=["idxf"])
        chunks = [(0, 128), (128, 128)] + ([(256, 32)] if ctx_full else [])
        for ci, (c0, cn) in enumerate(chunks):
            op("pe", lambda e, c0=c0, cn=cn, ci=ci: e.transpose(bank(ci)[0:cn, 0:NE], idxf[0:NE, c0:c0 + cn], ident_f[0:NE, 0:NE]), r=["idxf", "ident_f"], w=[PSK(ci)])
            op("dve", lambda e, cn=cn, ci=ci: e.tensor_copy(idxT[ci][0:cn, :], bank(ci)[0:cn, 0:NE]), r=[PSK(ci)], w=[("idxT", ci)])
            op("pe", lambda e, c0=c0, cn=cn, ci=ci: e.transpose(bank(4 + ci)[0:cn, 0:NE], vals[0:NE, c0:c0 + cn], ident_f[0:NE, 0:NE]), r=["vals", "ident_f"], w=[PSK(4 + ci)])
            op("act", lambda e, cn=cn, ci=ci: e.copy(valT[ci][0:cn, :], bank(4 + ci)[0:cn, 0:NE]), r=[PSK(4 + ci)], w=[("valT", ci)])
        S.barrier()
        A.release(m)

    def phase_c2(l, ctx_full, idxT, valT):
        m = A.mark()
        g2 = [A.f32(D) for _ in range(2)]
        for r in range(2):
            load_mod_bc(g2[r], r, 5, ("g2", r))
        NGU = 4
        NDN = 2
        gus = [v3(A.bf16(KC * 512), 512) for _ in range(NGU)]
        dns = [v3(A.bf16(KC * 512), 512) for _ in range(NDN)]
        xs = v3(A.bf16(3 * D), D)
        NS = 288 if ctx_full else 256
        xsT = v3(A.bf16(KC * 288), 288)
        aT = v3(A.bf16(KC * 288), 288)
        sg = [A.f32(288) for _ in range(2)]
        yst = v3(A.f32(3 * D), D)
        chunks = [(0, 128), (128, 128)] + ([(256, 32)] if ctx_full else [])
        jobs_gu = [(e_, fg, wh) for e_ in range(NE) for fg in range(4) for wh in range(2)]
        jobs_dn = [(e_, cg) for e_ in range(NE) for cg in range(4)]
        st = dict(gu_issued=0, gu_done=0, dn_issued=0, dn_done=0)

        def pump_gu():
            while st["gu_issued"] < len(jobs_gu) and st["gu_issued"] < st["gu_done"] + NGU:
                j = st["gu_issued"]
                e_, fg, wh = jobs_gu[j]
                src = (w_gate if wh == 0 else w_up)[l, e_].rearrange("(k p) n -> p k n", p=P)[:, :, fg * 512:(fg + 1) * 512]
                dma("pool", gus[j % NGU], src, w=[("gu", j % NGU)])
                st["gu_issued"] += 1

        def pump_dn():
            while st["dn_issued"] < len(jobs_dn) and st["dn_issued"] < st["dn_done"] + NDN:
                j = st["dn_issued"]
                e_, cg = jobs_dn[j]
                src = w_down[l, e_].rearrange("(k p) n -> p k n", p=P)[:, :, cg * 512:(cg + 1) * 512]
                dma("pool", dns[j % NDN], src, w=[("dn", j % NDN)])
                st["dn_issued"] += 1

        def gather(e_):
            for ci, (c0, cn) in enumerate(chunks):
                srcd = h2t if ci < 2 else h2c
                op("pool", lambda e, ci=ci, cn=cn, srcd=srcd, e_=e_: e.indirect_dma_start(
                    out=xs[0:cn, ci, :], out_offset=None, in_=srcd,
                    in_offset=bass.IndirectOffsetOnAxis(ap=idxT[ci][0:cn, e_:e_ + 1], axis=0)),
                   r=[("idxT", ci)], w=[("xs", ci)], dma=True)

        def transposeT(e_):
            for ci, (c0, cn) in enumerate(chunks):
                for k4 in range(4):
                    bk = (ci * 4 + k4) % 2
                    for kk in range(4):
                        k = k4 * 4 + kk
                        op("pe", lambda e, ci=ci, cn=cn, k=k, kk=kk, bk=bk: e.transpose(bank16(bk)[:, kk * P:kk * P + cn], xs[0:cn, ci, k * P:(k + 1) * P], ident_b[0:cn, 0:cn]),
                           r=[("xs", ci), "ident_b"], w=[PSK(bk)])
                    src = v3(bank16(bk)[:, 0:512], P)[:, :, 0:cn]
                    dstv = xsT[:, k4 * 4:k4 * 4 + 4, c0:c0 + cn]
                    if k4 % 2 == 0:
                        op("act", lambda e, src=src, dstv=dstv: e.copy(dstv, src), r=[PSK(bk)], w=["xsT"])
                    else:
                        op("dve", lambda e, src=src, dstv=dstv: e.tensor_copy(dstv, src), r=[PSK(bk)], w=["xsT"])

        pump_gu()
        pump_dn()
        gather(0)
        transposeT(0)
        for e_ in range(NE):
            if e_ + 1 < NE:
                gather(e_ + 1)
            for fg in range(4):
                jg = (e_ * 4 + fg) * 2
                sg_ = jg % NGU
                su_ = (jg + 1) % NGU
                for fc in range(4):
                    fcn = fg * 4 + fc
                    pg = 2 + 2 * (fcn % 2)
                    pu = pg + 1
                    for k in range(KC):
                        op("pe", lambda e, k=k, fc=fc, pg=pg, sg_=sg_: e.matmul(bank(pg)[:, 0:NS], gus[sg_][:, k, fc * P:(fc + 1) * P], xsT[:, k, 0:NS], start=(k == 0), stop=(k == KC - 1)),
                           r=[("gu", sg_), "xsT"], w=[PSK(pg)])
                    for k in range(KC):
                        op("pe", lambda e, k=k, fc=fc, pu=pu, su_=su_: e.matmul(bank(pu)[:, 0:NS], gus[su_][:, k, fc * P:(fc + 1) * P], xsT[:, k, 0:NS], start=(k == 0), stop=(k == KC - 1)),
                           r=[("gu", su_), "xsT"], w=[PSK(pu)])
                    sgt = sg[fcn % 2]
                    op("act", lambda e, pg=pg, sgt=sgt: e.activation(sgt[:, 0:NS], bank(pg)[:, 0:NS], AF.Silu), r=[PSK(pg)], w=[("sg", fcn % 2)])
                    op("dve", lambda e, pu=pu, sgt=sgt, fcn=fcn: e.tensor_tensor(aT[:, fcn, 0:NS], sgt[:, 0:NS], bank(pu)[:, 0:NS], ALU.mult),
                       r=[PSK(pu), ("sg", fcn % 2)], w=[("aT", fcn)])
                st["gu_done"] += 2
                pump_gu()
            if e_ + 1 < NE:
                transposeT(e_ + 1)
            for cg in range(4):
                cs = slice(cg * 512, (cg + 1) * 512)
                sd_ = (e_ * 4 + cg) % NDN
                for ci, (c0, cn) in enumerate(chunks):
                    pb = 6 + ((cg * 3 + ci) % 2)
                    r = 1 if ci == 2 else 0
                    for k in range(KC):
                        op("pe", lambda e, k=k, c0=c0, cn=cn, pb=pb, sd_=sd_: e.matmul(bank(pb)[0:cn, :], aT[:, k, c0:c0 + cn], dns[sd_][:, k, :], start=(k == 0), stop=(k == KC - 1)),
                           r=[("dn", sd_)] + [("aT", kk) for kk in range(KC)], w=[PSK(pb)])
                    op("dve", lambda e, ci=ci, cn=cn, pb=pb, r=r, cs=cs, e_=e_: e.scalar_tensor_tensor(yst[0:cn, ci, cs], bank(pb)[0:cn, :], valT[ci][0:cn, e_:e_ + 1], g2[r][0:cn, cs], ALU.mult, ALU.mult),
                       r=[PSK(pb), ("valT", ci), ("g2", r)], w=[("yst", ci)])
                st["dn_done"] += 1
                pump_dn()
            for ci, (c0, cn) in enumerate(chunks):
                dstd = xl if ci < 2 else xc
                op("pool", lambda e, ci=ci, cn=cn, dstd=dstd, e_=e_: e.indirect_dma_start(
                    out=dstd, out_offset=bass.IndirectOffsetOnAxis(ap=idxT[ci][0:cn, e_:e_ + 1], axis=0),
                    in_=yst[0:cn, ci, :], in_offset=None, compute_op=ALU.add),
                   r=[("yst", ci), ("idxT", ci)], w=["xl_scatter" if ci < 2 else "xc_scatter"], dma=True)
        S.barrier()
        A.release(m)

    def phase_final():
        m = A.mark()
        g = A.f32(D)
        bc_row(g, final_g[0:1, :], "fg")
        xs = [A.f32(D) for _ in range(2)]
        xo = [A.f32(D) for _ in range(2)]
        junk = A.bf16(D)
        ss = A.f32(2); t1 = A.f32(2); t2 = A.f32(2); rstd = A.f32(2)
        src = xl if n_layers > 0 else x_in
        for t in range(16):
            s = t % 2
            dma("sp", xs[s], src[t * P:(t + 1) * P, :], w=[("x", s)])
            op("act", lambda e, s=s: e.activation(junk, xs[s], AF.Square, accum_out=ss[:, 0:1]), r=[("x", s)], w=["junk", "fss"])
            rstd_from_ss(ss[:, 0:1], D, t1[:, 0:1], t2[:, 0:1], rstd[:, 0:1], "f")
            op("dve", lambda e, s=s: e.scalar_tensor_tensor(xo[s], xs[s], rstd[:, 0:1], g, ALU.mult, ALU.mult), r=[("x", s), "frstd", "fg"], w=[("xo", s)])
            dma("sp", out[t * P:(t + 1) * P, :], xo[s], r=[("xo", s)])
        S.barrier()
        A.release(m)

    layer_mark = A.mark()

    def _gg(gre, gim):
        return gre
    if only == "b2":
        uT = v3(A.bf16(4 * NTOK), NTOK)
        for cq in range(4):
            op("dve", lambda e, cq=cq: e.memset(uT[:, cq, :], 0.5), w=[("uT", cq)])
        S.barrier()
        phase_b2(0, True, uT)
        n_layers = 0
        stop_after = ("x", 0)
    stages = []
    for l in range(n_layers):
        last = (l == DEPTH - 1)
        ctx_full = not last
        phase_ada(l)
        if stop_after == ("ada", l):
            break
        m0 = A.mark()
        uT = v3(A.bf16(4 * NTOK), NTOK)
        m1 = A.mark()
        hT = v3(A.bf16(KC * NTOK), NTOK)
        phase_b0(l, True, hT)
        phase_b1(l, ctx_full, hT, uT)
        A.release(m1)
        if stop_after == ("b1", l):
            break
        phase_b2(l, ctx_full, uT)
        A.release(m0)
        if stop_after == ("b2", l):
            break
        phase_b3(l, ctx_full)
        if stop_after == ("b3", l):
            break
        mc_ = A.mark()
        idxT = [A.u32(NE) for _ in range(3)]
        valT = [A.f32(NE) for _ in range(3)]
        m_aff = A.mark()
        affT = A.f32(NTOK)
        phase_c0(l, ctx_full, affT)
        phase_c1(ctx_full, affT, idxT, valT)
        A.release(m_aff)
        if stop_after == ("c1", l):
            break
        phase_c2(l, ctx_full, idxT, valT)
        A.release(mc_)
    if stop_after is None:
        phase_final()
    S.barrier()
    S.emit()
    stack.close()
    return nc


def prep_shared(inp):
    f = np.float32
    sh = {}
    sh["ada_w"] = np.ascontiguousarray(inp["ada_w"], f)
    sh["ada_b2"] = np.ascontiguousarray(np.repeat(inp["ada_b"][:, None, :], 2, axis=1), f)
    sh["norm1_g"] = np.ascontiguousarray(inp["norm1_g"], f)
    sh["norm2_g"] = np.ascontiguousarray(inp["norm2_g"], f)
    sh["final_g"] = np.ascontiguousarray(inp["final_norm_g"].reshape(1, D), f)
    w_in = inp["w_in"]
    sh["w_in_t"] = np.ascontiguousarray(w_in.reshape(DEPTH, KC, P, 40, P).transpose(0, 3, 2, 1, 4), f)
    cw = inp["conv_w"]
    sh["conv_wc"] = np.ascontiguousarray(cw.reshape(DEPTH, 3, 12, P).transpose(0, 3, 2, 1).reshape(DEPTH, P, 36), f)

    def col16(a):
        return np.ascontiguousarray(a.reshape(DEPTH, 2, 16, 2, 64).transpose(0, 1, 3, 4, 2).reshape(DEPTH, 2, P, 16), f)

    sh["lamre"] = col16(inp["ssm_lam_re"])
    sh["lamim"] = col16(inp["ssm_lam_im"])
    sh["logdt"] = col16(np.repeat(inp["ssm_log_dt"][..., None], 64, axis=-1))

    def bt(b):
        o = np.zeros((DEPTH, 2, 4, P, 4, P), f)
        bb = b.reshape(DEPTH, 2, 4, 4, 2, 64, 16)
        for jl in range(4):
            for gl in range(2):
                o[:, :, :, 32 * jl + 16 * gl:32 * jl + 16 * gl + 16, jl, gl * 64:(gl + 1) * 64] = bb[:, :, :, jl, gl].transpose(0, 1, 2, 4, 3)
        return o.reshape(DEPTH, 2, 4, P, 4 * P)

    def ct(c):
        o = np.zeros((DEPTH, 2, 4, P, 4, P), f)
        cc = c.reshape(DEPTH, 2, 4, 4, 2, 16, 64)
        for jl in range(4):
            for gl in range(2):
                o[:, :, :, gl * 64:(gl + 1) * 64, jl, 32 * jl + 16 * gl:32 * jl + 16 * gl + 16] = cc[:, :, :, jl, gl].transpose(0, 1, 2, 4, 3)
        return o.reshape(DEPTH, 2, 4, P, 4 * P)

    sh["bt_re"] = bt(inp["ssm_b_re"]); sh["bt_im"] = bt(inp["ssm_b_im"])
    sh["ct_re"] = ct(inp["ssm_c_re"]); sh["ct_im"] = ct(inp["ssm_c_im"])
    sh["dcol"] = np.ascontiguousarray(inp["ssm_d"].reshape(DEPTH, 4, P).transpose(0, 2, 1), f)
    sh["w_glu"] = np.ascontiguousarray(inp["ssm_w_glu"], f)
    sh["gconv"] = np.ascontiguousarray(inp["out_norm_conv_g"].reshape(DEPTH, 12, P).transpose(0, 2, 1), f)
    sh["gssm"] = np.ascontiguousarray(inp["out_norm_ssm_g"].reshape(DEPTH, 4, P).transpose(0, 2, 1), f)
    sh["w_out"] = np.ascontiguousarray(inp["w_out"], f)
    sh["router_w"] = np.ascontiguousarray(inp["router_w"], f)
    sh["w_gate"] = np.ascontiguousarray(inp["exp_w_gate"], f)
    sh["w_up"] = np.ascontiguousarray(inp["exp_w_up"], f)
    sh["w_down"] = np.ascontiguousarray(inp["exp_w_down"], f)
    return sh


def prep_core(inp, b):
    f = np.float32
    d = {}
    d["x"] = np.ascontiguousarray(inp["x"][b], f)
    d["ctx"] = np.ascontiguousarray(inp["ctx"][b], f)
    cc = np.zeros((P, KC, 2), f)
    cc[:, :, 0] = inp["c"][b].reshape(KC, P).T
    cc[:, :, 1] = inp["c_ctx"].reshape(KC, P).T
    d["ccol"] = cc.reshape(P, 32)
    return d


def kernel(**inputs):
    inp = {k: np.asarray(v) for k, v in inputs.items()}
    sh = prep_shared(inp)
    nc = build_program()
    in_maps = []
    for b in range(8):
        m = dict(sh)
        m.update(prep_core(inp, b))
        in_maps.append(m)
    res = run_bass_kernel_spmd(nc, in_maps, core_ids=list(range(8)))
    return np.stack([res.results[b]["out"] for b in range(8)], axis=0).astype(np.float32)
```

```python
import math
import numpy as np
import concourse.bass as bass
import concourse.mybir as mybir
from concourse.bass_utils import run_bass_kernel_spmd

F32 = mybir.dt.float32
BF16 = mybir.dt.bfloat16
U32 = mybir.dt.uint32
I32 = mybir.dt.int32
ALU = mybir.AluOpType
AF = mybir.ActivationFunctionType
AX = mybir.AxisListType

P = 128
D = 2048
KC = 16
NLAT = 2048
NCTX = 256
NTOK = NLAT + NCTX
DEPTH = 2
NE = 16
EPS = 1e-6
TWO_PI = 2.0 * math.pi
PI_LO = 3.1415925
ENGS = ["pe", "act", "dve", "pool", "sp"]
NDMA = 40


class Sched:
    def __init__(self, nc, stack):
        self.nc = nc
        self.sems = []
        self.eng_sem = {}
        for e in ENGS:
            self.eng_sem[e] = len(self.sems)
            self.sems.append(stack.enter_context(nc.semaphore("s_" + e)))
        self.dma_sem = []
        for i in range(NDMA):
            self.dma_sem.append(len(self.sems))
            self.sems.append(stack.enter_context(nc.semaphore("d_%d" % i)))
        self.dma_val = [0] * NDMA
        self.dma_next = 0
        self.cnt = {e: 0 for e in ENGS}
        self.recs = {e: [] for e in ENGS}
        self.seen = {e: {} for e in ENGS}
        self.lastw = {}
        self.rd_eng = {}
        self.rd_dma = {}

    def _need(self, eng, tok, waits):
        semi, val, src, is_dma = tok
        if self.seen[eng].get(semi, 0) >= val:
            return
        if (not is_dma) and src == eng and eng == "pe":
            return
        self.seen[eng][semi] = val
        waits.append((semi, val))

    def op(self, eng, fn, r=(), w=(), dma=False):
        waits = []
        for k in r:
            t = self.lastw.get(k)
            if t is not None:
                self._need(eng, t, waits)
        for k in w:
            t = self.lastw.get(k)
            if t is not None:
                self._need(eng, t, waits)
            for se, t in self.rd_eng.get(k, {}).items():
                self._need(eng, t, waits)
            for t in self.rd_dma.get(k, ()):
                self._need(eng, t, waits)
        if dma:
            slot = self.dma_next % NDMA
            self.dma_next += 1
            semi = self.dma_sem[slot]
            prev = self.dma_val[slot]
            if prev > self.seen[eng].get(semi, 0):
                waits.append((semi, prev))
                self.seen[eng][semi] = prev
            val = prev + 16
            self.dma_val[slot] = val
            tok = (semi, val, eng, True)
        else:
            self.cnt[eng] += 1
            tok = (self.eng_sem[eng], self.cnt[eng], eng, False)
        self.recs[eng].append((waits, fn, tok))
        for k in r:
            if dma:
                self.rd_dma.setdefault(k, []).append(tok)
            else:
                self.rd_eng.setdefault(k, {})[eng] = tok
        for k in w:
            self.lastw[k] = tok
            self.rd_eng[k] = {}
            self.rd_dma[k] = []
        return tok

    def barrier(self):
        toks = [(self.eng_sem[e], self.cnt[e], e, False) for e in ENGS if self.cnt[e] > 0]
        toks += [(self.dma_sem[i], self.dma_val[i], None, True) for i in range(NDMA) if self.dma_val[i] > 0]
        for e in ENGS:
            waits = []
            for t in toks:
                if t[2] == e and not t[3]:
                    continue
                if self.seen[e].get(t[0], 0) >= t[1]:
                    continue
                self.seen[e][t[0]] = t[1]
                waits.append((t[0], t[1]))
            if waits:
                self.recs[e].append((waits, None, None))
        self.lastw.clear()
        self.rd_eng.clear()
        self.rd_dma.clear()

    def emit(self):
        nc = self.nc
        sems = self.sems
        recs = self.recs

        def mk(name):
            def body(e):
                for waits, fn, tok in recs[name]:
                    for (s, v) in waits:
                        e.wait_ge(sems[s], v)
                    if fn is not None:
                        ins = fn(e)
                        ins.then_inc(sems[tok[0]], 16 if tok[3] else 1)
            return body

        with nc.Block() as block:
            block.tensor(mk("pe"))
            block.scalar(mk("act"))
            block.vector(mk("dve"))
            block.gpsimd(mk("pool"))
            block.sync(mk("sp"))


class Arena:
    def __init__(self, ap, ncols):
        self.ap = ap
        self.n = ncols
        self.off = 0

    def mark(self):
        return self.off

    def release(self, m):
        self.off = m

    def f32(self, cols):
        cols = (cols + 1) // 2 * 2
        assert self.off + cols <= self.n, ("SBUF arena overflow", self.off, cols, self.n)
        a = self.ap[:, self.off:self.off + cols]
        self.off += cols
        return a

    def bf16(self, cols):
        return self.f32((cols + 1) // 2).bitcast(BF16)[:, 0:cols]

    def u32(self, cols):
        return self.f32(cols).bitcast(U32)

    def i32(self, cols):
        return self.f32(cols).bitcast(I32)


def v3(ap, b):
    return ap.rearrange("p (a b) -> p a b", b=b)


def build_program(dbg=None, stop_after=None, n_layers=DEPTH, only=None):
    from contextlib import ExitStack
    dbg = dbg or []
    nc = bass.Bass("TRN2", target_bir_lowering=False)

    def din(name, shape, dt=F32):
        if only == "b2" and int(np.prod(shape)) > 4_000_000:
            shape = [2, 2]
        return nc.dram_tensor(name, list(shape), dt, kind="ExternalInput").ap()

    x_in = din("x", [NLAT, D])
    ctx_in = din("ctx", [NCTX, D])
    ccol = din("ccol", [P, 32])
    ada_w = din("ada_w", [DEPTH, 24, P, KC * 512])
    ada_b2 = din("ada_b2", [DEPTH, 2, 6 * D])
    norm1_g = din("norm1_g", [DEPTH, D])
    norm2_g = din("norm2_g", [DEPTH, D])
    final_g = din("final_g", [1, D])
    w_in_t = din("w_in_t", [DEPTH, 40, P, KC, P])
    conv_wc = din("conv_wc", [DEPTH, P, 36])
    lamre = din("lamre", [DEPTH, 2, P, 16])
    lamim = din("lamim", [DEPTH, 2, P, 16])
    logdt = din("logdt", [DEPTH, 2, P, 16])
    bt_re = din("bt_re", [DEPTH, 2, 4, P, 4 * P])
    bt_im = din("bt_im", [DEPTH, 2, 4, P, 4 * P])
    ct_re = din("ct_re", [DEPTH, 2, 4, P, 4 * P])
    ct_im = din("ct_im", [DEPTH, 2, 4, P, 4 * P])
    dcol = din("dcol", [DEPTH, P, 4])
    w_glu = din("w_glu", [DEPTH, 512, 512])
    gconv = din("gconv", [DEPTH, P, 12])
    gssm = din("gssm", [DEPTH, P, 4])
    w_out = din("w_out", [DEPTH, 4, P, KC * 512])
    router_w = din("router_w", [DEPTH, D, NE])
    small_exp = stop_after is not None and stop_after[0] != "c2"
    if small_exp:
        w_gate = w_up = w_down = None
    else:
        w_gate = din("w_gate", [DEPTH, NE, 4, P, KC * 512])
        w_up = din("w_up", [DEPTH, NE, 4, P, KC * 512])
        w_down = din("w_down", [DEPTH, NE, 4, P, KC * 512])
    out = nc.dram_tensor("out", [NLAT, D], F32, kind="ExternalOutput").ap()

    def dscr(name, shape, dt):
        kind = "ExternalOutput" if name in dbg else "Internal"
        return nc.dram_tensor(name, list(shape), dt, kind=kind).ap()

    xl = dscr("xl", [NLAT, D], F32)
    xc = dscr("xc", [NCTX, D], F32)
    h2t = dscr("h2t", [NLAT, D], BF16)
    h2c = dscr("h2c", [NCTX, D], BF16)
    modrow = dscr("modrow", [2, 6 * D], F32)
    mixn = dscr("mixn", [16, P, NTOK], BF16)
    dbg_aff = dscr("dbg_aff", [NE, NTOK], F32)
    dbg_y = dscr("dbg_y", [4, P, NTOK], F32)

    stack = ExitStack()
    NCOLS = 44800
    arena_t = stack.enter_context(nc.sbuf_tensor("arena", [P, NCOLS], F32))
    ps_t = stack.enter_context(nc.psum_tensor("ps", [P, 4096], F32))
    S = Sched(nc, stack)
    A = Arena(arena_t, NCOLS)
    op = S.op

    def bank(i):
        return ps_t[:, i * 512:(i + 1) * 512]

    def bank16(i):
        return ps_t[:, i * 512:(i + 1) * 512].bitcast(BF16)

    def PSK(i):
        return ("ps", i)

    ident_f = A.f32(P)
    ident_b = A.bf16(P)
    ones_b = A.bf16(P)
    iota_i = A.i32(P)
    iota_t = A.f32(P)
    op("pool", lambda e: e.iota(iota_i, [[1, P]], base=0, channel_multiplier=-1), w=["iota_i"])
    op("dve", lambda e: e.tensor_single_scalar(ident_f, iota_i, 0, ALU.is_equal), r=["iota_i"], w=["ident_f"])
    op("dve", lambda e: e.tensor_copy(ident_b, ident_f), r=["ident_f"], w=["ident_b"])
    op("dve", lambda e: e.memset(ones_b, 1.0), w=["ones_b"])
    op("pool", lambda e: e.iota(iota_i, [[1, P]], base=0, channel_multiplier=0), r=["ident_f"], w=["iota_i"])
    op("dve", lambda e: e.tensor_copy(iota_t, iota_i), r=["iota_i"], w=["iota_t"])
    S.barrier()
    base_mark = A.mark()

    def dma(q, out_ap, in_ap, r=(), w=()):
        if q == "pool":
            return op(q, lambda e: e.dma_start(out=out_ap, in_=in_ap, max_dma_last_dim=8192), r=r, w=w, dma=True)
        return op(q, lambda e: e.dma_start(out=out_ap, in_=in_ap), r=r, w=w, dma=True)

    def bc_row(dst, src_row, key):
        dma("sp", dst, src_row.partition_broadcast(P), w=[key])

    def rstd_from_ss(ss, n, tmp1, tmp2, rstd, keyp):
        op("dve", lambda e: e.tensor_scalar(tmp1, ss, 1.0 / n, EPS, ALU.mult, ALU.add), r=[keyp + "ss"], w=[keyp + "t1"])
        op("act", lambda e: e.activation(tmp2, tmp1, AF.Sqrt), r=[keyp + "t1"], w=[keyp + "t2"])
        op("dve", lambda e: e.reciprocal(rstd, tmp2), r=[keyp + "t2"], w=[keyp + "rstd"])

    def tok_src(l, t):
        if t < 2:
            src = ctx_in if l == 0 else xc
            return src[t * P:(t + 1) * P, :]
        src = x_in if l == 0 else xl
        return src[(t - 2) * P:(t - 1) * P, :]

    def tok_dst(t):
        if t < 2:
            return xc[t * P:(t + 1) * P, :]
        return xl[(t - 2) * P:(t - 1) * P, :]

    def phase_ada(l):
        m = A.mark()
        cc = A.f32(32)
        scT = A.bf16(32)
        wsl = [v3(A.bf16(KC * 512), 512) for _ in range(2)]
        bsl = [A.f32(512) for _ in range(2)]
        msl = [A.f32(512) for _ in range(2)]
        dma("sp", cc, ccol, w=["cc"])
        op("act", lambda e: e.activation(scT, cc, AF.Silu), r=["cc"], w=["scT"])
        scT3 = v3(scT, 2)
        for n in range(24):
            s = n % 2
            cs = slice(n * 512, (n + 1) * 512)
            dma("pool", wsl[s], ada_w[l, n].rearrange("p (k n) -> p k n", n=512), w=[("aw", s)])
            dma("sp", bsl[s][0:2, :], ada_b2[l][:, cs], w=[("ab", s)])
            for k in range(KC):
                op("pe", lambda e, k=k, s=s: e.matmul(bank(s)[0:2, :], scT3[:, k, :], wsl[s][:, k, :], start=(k == 0), stop=(k == KC - 1)),
                   r=["scT", ("aw", s)], w=[PSK(s)])
            op("dve", lambda e, s=s: e.tensor_tensor(msl[s][0:2, :], bank(s)[0:2, :], bsl[s][0:2, :], ALU.add),
               r=[PSK(s), ("ab", s)], w=[("ms", s)])
            dma("sp", modrow[:, cs], msl[s][0:2, :], r=[("ms", s)])
        S.barrier()
        A.release(m)

    def load_mod_bc(dst, r, seg, key):
        bc_row(dst, modrow[r:r + 1, seg * D:(seg + 1) * D], key)

    def phase_b0(l, ctx_any, hT):
        m = A.mark()
        gm = [A.f32(D) for _ in range(2)]
        sh = [A.f32(D) for _ in range(2)]
        xs = [A.f32(D) for _ in range(2)]
        hn = A.f32(D)
        gtmp = hn
        bc_row(gtmp, norm1_g[l:l + 1, :], "hn")
        for r in range(2):
            load_mod_bc(gm[r], r, 1, ("gm", r))
            load_mod_bc(sh[r], r, 0, ("sh", r))
            op("dve", lambda e, r=r: e.scalar_tensor_tensor(gm[r], gm[r], 1.0, gtmp, ALU.add, ALU.mult),
               r=["hn", ("gm", r)], w=[("gm", r)])
        hb = [A.bf16(D) for _ in range(2)]
        junk = hn.bitcast(BF16)[:, 0:D]
        ss = A.f32(2); t1 = A.f32(2); t2 = A.f32(2); rstd = A.f32(2)
        for t in range(NTOK // P):
            s = t % 2
            r = 1 if t < 2 else 0
            dma("sp", xs[s], tok_src(l, t), w=[("x", s)])
            op("act", lambda e, s=s: e.activation(junk, xs[s], AF.Square, accum_out=ss[:, 0:1]), r=[("x", s)], w=["hn", "b0ss"])
            rstd_from_ss(ss[:, 0:1], D, t1[:, 0:1], t2[:, 0:1], rstd[:, 0:1], "b0")
            op("dve", lambda e, s=s, r=r: e.scalar_tensor_tensor(hn, xs[s], rstd[:, 0:1], gm[r], ALU.mult, ALU.mult),
               r=[("x", s), "b0rstd", ("gm", r)], w=["hn"])
            op("pool", lambda e, s=s, r=r: e.tensor_tensor(hb[s], hn, sh[r], ALU.add), r=["hn", ("sh", r)], w=[("hb", s)])
            for k4 in range(4):
                bk = (t * 4 + k4) % 8
                for kk in range(4):
                    k = k4 * 4 + kk
                    op("pe", lambda e, s=s, k=k, kk=kk, bk=bk: e.transpose(bank16(bk)[:, kk * P:(kk + 1) * P], hb[s][:, k * P:(k + 1) * P], ident_b),
                       r=[("hb", s), "ident_b"], w=[PSK(bk)])
                eng = "act" if k4 % 2 == 0 else "dve"
                if eng == "act":
                    op("act", lambda e, k4=k4, bk=bk, t=t: e.copy(hT[:, k4 * 4:k4 * 4 + 4, t * P:(t + 1) * P], v3(bank16(bk)[:, 0:512], P)),
                       r=[PSK(bk)], w=[("hT", t)])
                else:
                    op("dve", lambda e, k4=k4, bk=bk, t=t: e.tensor_copy(hT[:, k4 * 4:k4 * 4 + 4, t * P:(t + 1) * P], v3(bank16(bk)[:, 0:512], P)),
                       r=[PSK(bk)], w=[("hT", t)])
        S.barrier()
        A.release(m)

    def tiles_for(ctx_on):
        tl = []
        if ctx_on:
            tl.append((0, 256, 256))
        for i in range(4):
            tl.append((256 + 512 * i, 512, 64))
        return tl

    def headnorm_store(y, W, gcol, chunk, tok0, bufs, it):
        sq, ms, sd, yn = bufs
        pb = 3 + 4 * (it % 2)
        kp = "hn%d" % (it % 2)
        op("act", lambda e: e.activation(sq[:, 0:W], y[:, 0:W], AF.Square), r=[kp + "y"], w=[kp + "sq"])
        op("pe", lambda e: e.matmul(bank(pb)[:, 0:W], ones_b, sq[:, 0:W], start=True, stop=True), r=[kp + "sq", "ones_b"], w=[PSK(pb)])
        op("dve", lambda e: e.tensor_scalar(ms[:, 0:W], bank(pb)[:, 0:W], 1.0 / P, EPS, ALU.mult, ALU.add), r=[PSK(pb)], w=[kp + "ms"])
        op("act", lambda e: e.activation(sd[:, 0:W], ms[:, 0:W], AF.Sqrt), r=[kp + "ms"], w=[kp + "sd"])
        op("dve", lambda e: e.reciprocal(ms[:, 0:W], sd[:, 0:W]), r=[kp + "sd"], w=[kp + "ms"])
        op("dve", lambda e: e.scalar_tensor_tensor(yn[:, 0:W], y[:, 0:W], gcol, ms[:, 0:W], ALU.mult, ALU.mult),
           r=[kp + "y", kp + "ms", "gcols"], w=[kp + "yn"])
        dma("sp", mixn[chunk][:, tok0:tok0 + W], yn[:, 0:W], r=[kp + "yn"])

    def phase_b1(l, ctx_full, hT, uT):
        m = A.mark()
        cw = A.f32(36)
        gc = A.f32(12)
        dma("sp", cw, conv_wc[l], w=["cw"])
        dma("sp", gc, gconv[l], w=["gcols"])
        cw3 = v3(cw, 3)
        wsl = [[v3(A.bf16(KC * P), P) for _ in range(3)] for _ in range(2)]
        tmp = []
        for i in range(2):
            tmp.append(dict(c=A.f32(512), z=A.f32(512), acc=A.f32(512), y=A.f32(512),
                            sq=A.bf16(512), ms=A.f32(512), sd=A.f32(512), yn=A.bf16(512)))
        it = 0
        for k in range(12):
            ws = wsl[k % 2]
            for j in range(3):
                dma("pool", ws[j], w_in_t[l, j * 12 + k], w=[("win", k % 2, j)])
            for (tok0, W, RW) in tiles_for(ctx_full):
                T = tmp[it % 2]
                b0 = 4 * (it % 2)
                kp = "hn%d" % (it % 2)
                for j in range(3):
                    for kk in range(KC):
                        op("pe", lambda e, j=j, kk=kk, b0=b0, ws=ws, tok0=tok0, W=W: e.matmul(
                            bank(b0 + j)[:, 0:W], ws[j][:, kk, :], hT[:, kk, tok0:tok0 + W], start=(kk == 0), stop=(kk == KC - 1)),
                           r=[("win", k % 2, j)] + [("hT", tt) for tt in range(tok0 // P, (tok0 + W) // P)], w=[PSK(b0 + j)])
                op("act", lambda e, T=T, b0=b0, W=W: e.copy(T["c"][:, 0:W], bank(b0 + 1)[:, 0:W]), r=[PSK(b0 + 1)], w=[kp + "c"])
                op("dve", lambda e, T=T, b0=b0, W=W: e.tensor_tensor(T["z"][:, 0:W], bank(b0 + 2)[:, 0:W], T["c"][:, 0:W], ALU.mult),
                   r=[PSK(b0 + 2), kp + "c"], w=[kp + "z"])
                op("act", lambda e, T=T, W=W, k=k: e.activation(T["acc"][:, 0:W], T["z"][:, 0:W], AF.Copy, scale=cw3[:, k, 1:2]),
                   r=[kp + "z", "cw"], w=[kp + "acc"])
                zv = v3(T["z"][:, 0:W], RW)
                av = v3(T["acc"][:, 0:W], RW)
                op("dve", lambda e, zv=zv, av=av, k=k, RW=RW: e.scalar_tensor_tensor(av[:, :, 1:RW], zv[:, :, 0:RW - 1], cw3[:, k, 0:1], av[:, :, 1:RW], ALU.mult, ALU.add),
                   r=[kp + "z", kp + "acc", "cw"], w=[kp + "acc"])
                op("dve", lambda e, zv=zv, av=av, k=k, RW=RW: e.scalar_tensor_tensor(av[:, :, 0:RW - 1], zv[:, :, 1:RW], cw3[:, k, 2:3], av[:, :, 0:RW - 1], ALU.mult, ALU.add),
                   r=[kp + "z", kp + "acc", "cw"], w=[kp + "acc"])
                op("dve", lambda e, T=T, b0=b0, W=W: e.tensor_tensor(T["y"][:, 0:W], bank(b0)[:, 0:W], T["acc"][:, 0:W], ALU.mult),
                   r=[PSK(b0), kp + "acc"], w=[kp + "y"])
                headnorm_store(T["y"], W, gc[:, k:k + 1], k, tok0, (T["sq"], T["ms"], T["sd"], T["yn"]), it)
                it += 1
        for cq in range(4):
            ws = wsl[cq % 2][0]
            dma("pool", ws, w_in_t[l, 36 + cq], w=[("win", cq % 2, 0)])
            for (tok0, W, RW) in tiles_for(True):
                b0 = 4 * (it % 2)
                for kk in range(KC):
                    op("pe", lambda e, kk=kk, b0=b0, ws=ws, tok0=tok0, W=W: e.matmul(
                        bank(b0)[:, 0:W], ws[:, kk, :], hT[:, kk, tok0:tok0 + W], start=(kk == 0), stop=(kk == KC - 1)),
                       r=[("win", cq % 2, 0)] + [("hT", tt) for tt in range(tok0 // P, (tok0 + W) // P)], w=[PSK(b0)])
                op("act", lambda e, b0=b0, cq=cq, tok0=tok0, W=W: e.copy(uT[:, cq, tok0:tok0 + W], bank(b0)[:, 0:W]),
                   r=[PSK(b0)], w=[("uT", cq, tok0)])
                it += 1
        S.barrier()
        A.release(m)

    def reduce_angle(dst, kd, x, kx, n, tf, ti):
        op("dve", lambda e: e.tensor_scalar(tf, x, 1.0 / TWO_PI, None, ALU.mult), r=[kx], w=["ra_tf"])
        op("dve", lambda e: e.tensor_copy(ti, tf), r=["ra_tf"], w=["ra_ti"])
        op("dve", lambda e: e.tensor_copy(tf, ti), r=["ra_ti"], w=["ra_tf"])
        op("dve", lambda e: e.scalar_tensor_tensor(dst, tf, -TWO_PI, x, ALU.mult, ALU.add), r=["ra_tf", kx], w=[kd])
        op("dve", lambda e: e.tensor_scalar(tf, dst, math.pi, -TWO_PI, ALU.is_gt, ALU.mult), r=[kd], w=["ra_tf"])
        op("dve", lambda e: e.tensor_tensor(dst, dst, tf, ALU.add), r=["ra_tf", kd], w=[kd])
        op("dve", lambda e: e.tensor_scalar(tf, dst, -math.pi, TWO_PI, ALU.is_lt, ALU.mult), r=[kd], w=["ra_tf"])
        op("dve", lambda e: e.tensor_tensor(dst, dst, tf, ALU.add), r=["ra_tf", kd], w=[kd])
        op("dve", lambda e: e.tensor_scalar(dst, dst, PI_LO, -PI_LO, ALU.min, ALU.max), r=[kd], w=[kd])

    def sincos(sin_out, ks, cos_out, kc, x, kx, n, tf, ti, xs, red, neg_sin=False):
        reduce_angle(red, "ra_red", x, kx, n, tf, ti)
        op("act", lambda e: e.activation(sin_out, red, AF.Sin, scale=(-1.0 if neg_sin else 1.0)), r=["ra_red"], w=[ks])
        op("dve", lambda e: e.tensor_scalar(xs, x, math.pi / 2, None, ALU.add), r=[kx], w=["ra_xs"])
        reduce_angle(red, "ra_red", xs, "ra_xs", n, tf, ti)
        op("act", lambda e: e.activation(cos_out, red, AF.Sin), r=["ra_red"], w=[kc])

    def phase_b2(l, ctx_out, uT):
        m = A.mark()
        ysum = v3(A.f32(4 * NTOK), NTOK)
        NT = 16 * P
        cosT = A.f32(NT); nsinT = A.f32(NT); RKre = A.f32(NT); RKim = A.f32(NT); MAG0 = A.f32(NT)
        BTre = v3(A.bf16(NT), P); BTim = v3(A.bf16(NT), P); CTre = v3(A.bf16(NT), P); CTim = v3(A.bf16(NT), P)
        small = [A.f32(16) for _ in range(26)]
        (lr, li, ldt, dt, lrdt, th, mag, s1, c1, ar, ai, nr, den, kr, ki, thr, tA, tB, Ere, Eim, th128, s128, c128, tC, tD, tE) = small
        HW = 8 * P
        QW = 4 * P
        WS = []
        big = A.f32(4 * 6 * QW)
        for q_ in range(4):
            o_ = q_ * 6 * QW
            WS.append(dict(t1=big[:, o_:o_ + QW], t2=big[:, o_ + QW:o_ + 2 * QW], t3=big[:, o_ + 2 * QW:o_ + 3 * QW],
                           t4=big[:, o_ + 3 * QW:o_ + 4 * QW], gre=big[:, o_ + 4 * QW:o_ + 5 * QW], gim=big[:, o_ + 5 * QW:o_ + 6 * QW],
                           hre=A.bf16(QW), nhim=A.bf16(QW), ytmp=A.f32(P),
                           carry=[A.f32(4), A.f32(4)], ctmp=[A.f32(4) for _ in range(4)]))
        wkA = big[:, 0:NT]; wkB = big[:, NT:2 * NT]; wkC = big[:, 2 * NT:3 * NT]; gg = big[:, 3 * NT:4 * NT]

        def sm(o, a, b, alu, keys_r, key_w):
            op("dve", lambda e: e.tensor_tensor(o, a, b, alu), r=keys_r, w=[key_w])

        for d in range(2):
            dma("sp", lr, lamre[l, d], w=["lr"])
            dma("sp", li, lamim[l, d], w=["li"])
            dma("sp", ldt, logdt[l, d], w=["ldt"])
            for (dst, src) in ((BTre, bt_re), (BTim, bt_im), (CTre, ct_re), (CTim, ct_im)):
                dma("pool", dst.rearrange("p (c j) q -> p c (j q)", c=4), src[l, d].rearrange("c p x -> p c x"), w=["BC"])
            op("act", lambda e: e.activation(dt, ldt, AF.Exp), r=["ldt"], w=["dt"])
            sm(lrdt, lr, dt, ALU.mult, ["lr", "dt"], "lrdt")
            sm(th, li, dt, ALU.mult, ["li", "dt"], "th")
            op("act", lambda e: e.activation(mag, lrdt, AF.Exp), r=["lrdt"], w=["mag"])
            tBi = tB.bitcast(I32)
            sincos(s1, "s1", c1, "c1", th, "th", 16, tA, tBi, tC, tD)
            reduce_angle(thr, "thr", th, "th", 16, tA, tBi)
            sm(ar, mag, c1, ALU.mult, ["mag", "c1"], "ar")
            sm(ai, mag, s1, ALU.mult, ["mag", "s1"], "ai")
            op("dve", lambda e: e.tensor_scalar(nr, ar, -1.0, None, ALU.add), r=["ar"], w=["nr"])
            sm(den, lr, lr, ALU.mult, ["lr"], "den")
            sm(tE, li, li, ALU.mult, ["li"], "tE")
            sm(den, den, tE, ALU.add, ["den", "tE"], "den")
            op("dve", lambda e: e.reciprocal(den, den), r=["den"], w=["den"])
            sm(kr, nr, lr, ALU.mult, ["nr", "lr"], "kr")
            sm(tE, ai, li, ALU.mult, ["ai", "li"], "tE")
            sm(kr, kr, tE, ALU.add, ["kr", "tE"], "kr")
            sm(kr, kr, den, ALU.mult, ["kr", "den"], "kr")
            sm(ki, ai, lr, ALU.mult, ["ai", "lr"], "ki")
            sm(tE, nr, li, ALU.mult, ["nr", "li"], "tE")
            sm(ki, ki, tE, ALU.subtract, ["ki", "tE"], "ki")
            sm(ki, ki, den, ALU.mult, ["ki", "den"], "ki")
            op("dve", lambda e: e.tensor_scalar(th128, thr, 128.0, None, ALU.mult), r=["thr"], w=["th128"])
            sincos(s128, "s128", c128, "c128", th128, "th128", 16, tA, tBi, tC, tD)
            sm(Ere, mag, c128, ALU.mult, ["mag", "c128"], "Ere")
            sm(Eim, mag, s128, ALU.mult, ["mag", "s128"], "Eim")
            op("dve", lambda e: e.tensor_tensor(v3(wkA, P), thr.unsqueeze(2).to_broadcast([P, 16, P]), iota_t.unsqueeze(1).to_broadcast([P, 16, P]), ALU.mult),
               r=["thr"], w=["ang"])
            sincos(nsinT, "nsinT", cosT, "cosT", wkA, "ang", NT, wkB, gg.bitcast(I32), wkC, MAG0, neg_sin=True)
            krb = kr.unsqueeze(2).to_broadcast([P, 16, P]); kib = ki.unsqueeze(2).to_broadcast([P, 16, P])
            op("dve", lambda e: e.tensor_tensor(v3(RKre, P), v3(cosT, P), krb, ALU.mult), r=["cosT", "kr"], w=["RKre"])
            op("dve", lambda e: e.tensor_tensor(v3(wkA, P), v3(nsinT, P), kib, ALU.mult), r=["nsinT", "ki", "ang"], w=["ang"])
            op("dve", lambda e: e.tensor_tensor(RKre, RKre, wkA, ALU.subtract), r=["RKre", "ang"], w=["RKre"])
            op("dve", lambda e: e.tensor_tensor(v3(RKim, P), v3(cosT, P), kib, ALU.mult), r=["cosT", "ki"], w=["RKim"])
            op("dve", lambda e: e.tensor_tensor(v3(wkA, P), v3(nsinT, P), krb, ALU.mult), r=["nsinT", "kr", "ang", "RKre"], w=["ang"])
            op("dve", lambda e: e.tensor_tensor(RKim, RKim, wkA, ALU.add), r=["RKim", "ang"], w=["RKim"])
            op("dve", lambda e: e.tensor_copy(v3(MAG0, P), mag.unsqueeze(2).to_broadcast([P, 16, P])), r=["mag", "ra_red"], w=["MAG0"])
            op("dve", lambda e: e.memset(v3(MAG0, P)[:, :, 0:1], 0.0), r=["MAG0"], w=["MAG0"])
            S.barrier()

            if d == 0:
                order = list(range(18))
            else:
                order = [1, 0] + list(range(17, 1, -1))
            pending = {}

            def KQ(n, q):
                return (n, q)

            def S1(c, q):
                tsl = slice(c * P, (c + 1) * P)
                rhs = uT[:, q, tsl] if d == 0 else uT[:, q, tsl][:, ::-1]
                for jl in range(4):
                    j = 4 * q + jl
                    op("pe", lambda e, jl=jl, j=j, rhs=rhs, q=q: e.matmul(bank(2 * q)[:, jl * P:(jl + 1) * P], BTre[:, j, :], rhs, start=True, stop=True),
                       r=["BC"], w=[PSK(2 * q)])
                    op("pe", lambda e, jl=jl, j=j, rhs=rhs, q=q: e.matmul(bank(2 * q + 1)[:, jl * P:(jl + 1) * P], BTim[:, j, :], rhs, start=True, stop=True),
                       r=["BC"], w=[PSK(2 * q + 1)])

            def S2(c, q, first):
                W_ = WS[q]
                t1 = W_["t1"]; t2 = W_["t2"]; t3 = W_["t3"]; t4 = W_["t4"]
                fs = slice(q * QW, (q + 1) * QW)
                if q in pending:
                    cq_, tsl_ = pending.pop(q)
                    op("pool", lambda e, W_=W_, cq_=cq_, tsl_=tsl_: e.tensor_tensor(ysum[:, cq_, tsl_], ysum[:, cq_, tsl_], W_["ytmp"], ALU.add),
                       r=[KQ("ytmp", q)], w=[("ys", cq_, tsl_.start)])
                op("dve", lambda e, t1=t1, fs=fs, q=q: e.tensor_tensor(t1, RKre[:, fs], bank(2 * q), ALU.mult), r=[PSK(2 * q)], w=[KQ("t1", q)])
                op("dve", lambda e, t3=t3, fs=fs, q=q: e.tensor_tensor(t3, RKre[:, fs], bank(2 * q + 1), ALU.mult), r=[PSK(2 * q + 1)], w=[KQ("t3", q)])
                op("dve", lambda e, t4=t4, fs=fs, q=q: e.tensor_tensor(t4, RKim[:, fs], bank(2 * q), ALU.mult), r=[PSK(2 * q)], w=[KQ("t4", q)])
                op("dve", lambda e, t2=t2, fs=fs, q=q: e.tensor_tensor(t2, RKim[:, fs], bank(2 * q + 1), ALU.mult), r=[PSK(2 * q + 1)], w=[KQ("t2", q)])
                op("pool", lambda e, t1=t1, t2=t2: e.tensor_tensor(t1, t1, t2, ALU.subtract), r=[KQ("t1", q), KQ("t2", q)], w=[KQ("t1", q)])
                op("dve", lambda e, t3=t3, t4=t4: e.tensor_tensor(t3, t3, t4, ALU.add), r=[KQ("t3", q), KQ("t4", q)], w=[KQ("t3", q)])
                if not first:
                    br3 = v3(t1, P); bi3 = v3(t3, P)
                    op("pool", lambda e, W_=W_, br3=br3: e.tensor_tensor(br3[:, :, 0:1], br3[:, :, 0:1], W_["carry"][0].unsqueeze(2), ALU.add),
                       r=[KQ("t1", q), KQ("carry", q)], w=[KQ("t1", q)])
                    op("pool", lambda e, W_=W_, bi3=bi3: e.tensor_tensor(bi3[:, :, 0:1], bi3[:, :, 0:1], W_["carry"][1].unsqueeze(2), ALU.add),
                       r=[KQ("t3", q), KQ("carry", q)], w=[KQ("t3", q)])

            def S3(c, q):
                W_ = WS[q]
                t1 = W_["t1"]; t3 = W_["t3"]; gre = W_["gre"]; gim = W_["gim"]
                fs = slice(q * QW, (q + 1) * QW)
                js = slice(4 * q, 4 * q + 4)
                op("dve", lambda e, fs=fs, gre=gre, t1=t1: e.tensor_tensor_scan(gre, MAG0[:, fs], t1, 0.0, ALU.mult, ALU.add), r=[KQ("t1", q)], w=[KQ("gre", q)])
                op("dve", lambda e, fs=fs, gim=gim, t3=t3: e.tensor_tensor_scan(gim, MAG0[:, fs], t3, 0.0, ALU.mult, ALU.add), r=[KQ("t3", q)], w=[KQ("gim", q)])
                glr = v3(gre, P)[:, :, P - 1]; gli = v3(gim, P)[:, :, P - 1]
                ct_ = W_["ctmp"]; cy_ = W_["carry"]
                op("pool", lambda e, js=js, glr=glr, ct_=ct_: e.tensor_tensor(ct_[0], Ere[:, js], glr, ALU.mult), r=[KQ("gre", q)], w=[KQ("c0", q)])
                op("pool", lambda e, js=js, gli=gli, ct_=ct_: e.tensor_tensor(ct_[1], Eim[:, js], gli, ALU.mult), r=[KQ("gim", q)], w=[KQ("c1", q)])
                op("pool", lambda e, cy_=cy_, ct_=ct_: e.tensor_tensor(cy_[0], ct_[0], ct_[1], ALU.subtract), r=[KQ("c0", q), KQ("c1", q)], w=[KQ("carry", q)])
                op("pool", lambda e, js=js, gli=gli, ct_=ct_: e.tensor_tensor(ct_[2], Ere[:, js], gli, ALU.mult), r=[KQ("gim", q)], w=[KQ("c2", q)])
                op("pool", lambda e, js=js, glr=glr, ct_=ct_: e.tensor_tensor(ct_[3], Eim[:, js], glr, ALU.mult), r=[KQ("gre", q)], w=[KQ("c3", q)])
                op("pool", lambda e, cy_=cy_, ct_=ct_: e.tensor_tensor(cy_[1], ct_[2], ct_[3], ALU.add), r=[KQ("c2", q), KQ("c3", q), KQ("carry", q)], w=[KQ("carry", q)])

            def S4(c, q):
                W_ = WS[q]
                t1 = W_["t1"]; t2 = W_["t2"]; t3 = W_["t3"]; t4 = W_["t4"]
                gre = W_["gre"]; gim = W_["gim"]; hre = W_["hre"]; nhim = W_["nhim"]
                fs = slice(q * QW, (q + 1) * QW)
                op("pool", lambda e, fs=fs, t1=t1, gre=gre: e.tensor_tensor(t1, cosT[:, fs], gre, ALU.mult), r=[KQ("gre", q)], w=[KQ("t1", q)])
                op("pool", lambda e, fs=fs, t2=t2, gim=gim: e.tensor_tensor(t2, nsinT[:, fs], gim, ALU.mult), r=[KQ("gim", q)], w=[KQ("t2", q)])
                op("dve", lambda e, fs=fs, t3=t3, gre=gre: e.tensor_tensor(t3, nsinT[:, fs], gre, ALU.mult), r=[KQ("gre", q)], w=[KQ("t3", q)])
                op("pool", lambda e, fs=fs, t4=t4, gim=gim: e.tensor_tensor(t4, cosT[:, fs], gim, ALU.mult), r=[KQ("gim", q)], w=[KQ("t4", q)])
                op("dve", lambda e, hre=hre, t1=t1, t2=t2: e.tensor_tensor(hre, t1, t2, ALU.add), r=[KQ("t1", q), KQ("t2", q)], w=[KQ("hre", q)])
                op("dve", lambda e, nhim=nhim, t3=t3, t4=t4: e.tensor_tensor(nhim, t3, t4, ALU.subtract), r=[KQ("t3", q), KQ("t4", q)], w=[KQ("nhim", q)])

            def S5(c, q):
                W_ = WS[q]
                tsl = slice(c * P, (c + 1) * P)
                hre3 = v3(W_["hre"], P); nhim3 = v3(W_["nhim"], P)
                pb = 2 * q
                for j4 in range(4):
                    j = q * 4 + j4
                    op("pe", lambda e, j=j, j4=j4, pb=pb, hre3=hre3, d_=d: e.matmul(bank(pb)[:, 0:P], CTre[:, j, :], (hre3[:, j4, :] if d_ == 0 else hre3[:, j4, :][:, ::-1]), start=(j4 == 0), stop=False),
                       r=["BC", KQ("hre", q)], w=[PSK(pb)])
                    op("pe", lambda e, j=j, j4=j4, pb=pb, nhim3=nhim3, d_=d: e.matmul(bank(pb)[:, 0:P], CTim[:, j, :], (nhim3[:, j4, :] if d_ == 0 else nhim3[:, j4, :][:, ::-1]), start=False, stop=(j4 == 3)),
                       r=["BC", KQ("nhim", q)], w=[PSK(pb)])
                if d == 0:
                    op("act", lambda e, q=q, pb=pb, tsl=tsl: e.copy(ysum[:, q, tsl], bank(pb)[:, 0:P]), r=[PSK(pb)], w=[("ys", q, tsl.start)])
                else:
                    op("act", lambda e, W_=W_, pb=pb: e.copy(W_["ytmp"], bank(pb)[:, 0:P]), r=[PSK(pb)], w=[KQ("ytmp", q)])
                    pending[q] = (q, tsl)

            pairs = [(0, 1), (2, 3)]
            n_ = len(order)
            for pr in pairs:
                for q in pr:
                    S1(order[0], q)
            for i, c in enumerate(order):
                need_out = ctx_out or c >= 2
                for pr in pairs:
                    for q in pr:
                        S2(c, q, i == 0)
                    for q in pr:
                        S3(c, q)
                    if need_out:
                        for q in pr:
                            S4(c, q)
                        for q in pr:
                            S5(c, q)
                    if i + 1 < n_:
                        for q in pr:
                            S1(order[i + 1], q)
            for q in list(pending.keys()):
                cq_, tsl_ = pending.pop(q)
                op("pool", lambda e, q=q, cq_=cq_, tsl_=tsl_: e.tensor_tensor(ysum[:, cq_, tsl_], ysum[:, cq_, tsl_], WS[q]["ytmp"], ALU.add),
                   r=[KQ("ytmp", q)], w=[("ys", cq_, tsl_.start)])
            S.barrier()

        tokr = slice(0, NTOK) if ctx_out else slice(NCTX, NTOK)
        NTK = NTOK if ctx_out else NLAT
        A.release(m)
        ysum = v3(A.f32(4 * NTOK), NTOK)
        dcc = A.f32(4); gsc = A.f32(4)
        dma("sp", dcc, dcol[l], w=["dc"])
        dma("sp", gsc, gssm[l], w=["gcols"])
        wg = v3(A.bf16(4 * 512), 512)
        dma("pool", wg, w_glu[l].rearrange("(k p) n -> p k n", p=P), w=["wg"])
        gl16 = v3(A.bf16(4 * NTOK), NTOK)
        w1 = A.f32(NTOK); w2 = A.f32(NTOK)
        for cq in range(4):
            yv = ysum[:, cq, tokr]
            op("dve", lambda e, cq=cq, yv=yv: e.scalar_tensor_tensor(yv, uT[:, cq, tokr], dcc[:, cq:cq + 1], yv, ALU.mult, ALU.add),
               r=["dc", "uTall"], w=[("yt", cq)])
            if "dbg_y" in dbg:
                dma("sp", dbg_y[cq], ysum[:, cq, :], r=[("yt", cq)])
            op("pool", lambda e, yv=yv: e.tensor_tensor(w1[:, 0:NTK], yv, yv, ALU.mult), r=[("yt", cq)], w=["w1"])
            op("dve", lambda e: e.tensor_scalar(w1[:, 0:NTK], w1[:, 0:NTK], 0.044715, 1.0, ALU.mult, ALU.add), r=["w1"], w=["w1"])
            op("pool", lambda e, yv=yv: e.tensor_tensor(w2[:, 0:NTK], w1[:, 0:NTK], yv, ALU.mult), r=["w1", ("yt", cq)], w=["w2"])
            op("act", lambda e: e.activation(w1[:, 0:NTK], w2[:, 0:NTK], AF.Sigmoid, scale=1.5957691216057308), r=["w2", "w1"], w=["w1"])
            op("dve", lambda e, yv=yv: e.tensor_tensor(yv, yv, w1[:, 0:NTK], ALU.mult), r=["w1", ("yt", cq)], w=[("yt", cq)])
            op("act", lambda e, cq=cq, yv=yv: e.copy(gl16[:, cq, tokr], yv), r=[("yt", cq)], w=[("gl16", cq)])
        tmp = []
        for i in range(2):
            tmp.append(dict(sg=A.f32(512), o=A.f32(512), sq=A.bf16(512), ms=A.f32(512), sd=A.f32(512), yn=A.bf16(512)))
        it = 0
        for oc in range(4):
            for (tok0, W, RW) in tiles_for(ctx_out):
                T = tmp[it % 2]
                b0 = 4 * (it % 2)
                kp = "hn%d" % (it % 2)
                for k in range(4):
                    op("pe", lambda e, k=k, oc=oc, b0=b0, tok0=tok0, W=W: e.matmul(bank(b0)[:, 0:W], wg[:, k, oc * P:(oc + 1) * P], gl16[:, k, tok0:tok0 + W], start=(k == 0), stop=(k == 3)),
                       r=["wg"] + [("gl16", kk) for kk in range(4)], w=[PSK(b0)])
                op("act", lambda e, T=T, b0=b0, W=W: e.activation(T["sg"][:, 0:W], bank(b0)[:, 0:W], AF.Sigmoid), r=[PSK(b0)], w=[kp + "sg"])
                op("dve", lambda e, T=T, oc=oc, tok0=tok0, W=W: e.tensor_tensor(T["o"][:, 0:W], ysum[:, oc, tok0:tok0 + W], T["sg"][:, 0:W], ALU.mult),
                   r=[kp + "sg", ("yt", oc)], w=[kp + "y"])
                headnorm_store(T["o"], W, gsc[:, oc:oc + 1], 12 + oc, tok0, (T["sq"], T["ms"], T["sd"], T["yn"]), it)
                it += 1
        S.barrier()
        A.release(m)

    def phase_b3(l, ctx_full):
        m = A.mark()
        wo = v3(A.bf16(KC * D), D)
        for cg in range(4):
            dma("pool", wo[:, :, cg * 512:(cg + 1) * 512], w_out[l, cg].rearrange("p (k n) -> p k n", n=512), w=[("wo", cg)])
        g1 = [A.f32(D) for _ in range(2)]
        for r in range(2):
            load_mod_bc(g1[r], r, 2, ("g1", r))
        mts = [v3(A.bf16(KC * 512), 512) for _ in range(2)]
        xs = [A.f32(D) for _ in range(2)]
        xo = [A.f32(D) for _ in range(2)]
        tq = A.f32(512)
        t_list = list(range(0 if ctx_full else 2, 18))
        loaded = {}
        nload = 0
        for idx_t, t in enumerate(t_list):
            s = idx_t % 2
            r = 1 if t < 2 else 0
            grp = -1 if t < 2 else (t - 2) // 4
            if grp not in loaded:
                ms_ = nload % 2
                nload += 1
                tok0, W = (0, 256) if grp < 0 else (256 + grp * 512, 512)
                dma("sp", mts[ms_][:, :, 0:W], mixn[:, :, tok0:tok0 + W].rearrange("c p t -> p c t"), w=[("mt", ms_)])
                loaded[grp] = (ms_, tok0)
            ms_, tok0 = loaded[grp]
            lo = t * P - tok0
            dma("sp", xs[s], tok_src(l, t), w=[("x", s)])
            for cg in range(4):
                pb = (idx_t * 4 + cg) % 8
                cs = slice(cg * 512, (cg + 1) * 512)
                for k in range(KC):
                    op("pe", lambda e, k=k, ms_=ms_, lo=lo, cs=cs, pb=pb: e.matmul(bank(pb), mts[ms_][:, k, lo:lo + P], wo[:, k, cs], start=(k == 0), stop=(k == KC - 1)),
                       r=[("mt", ms_), ("wo", cg)], w=[PSK(pb)])
                op("dve", lambda e, pb=pb, r=r, cs=cs: e.tensor_tensor(tq, bank(pb), g1[r][:, cs], ALU.mult), r=[PSK(pb), ("g1", r)], w=["tq"])
                op("pool", lambda e, s=s, cs=cs: e.tensor_tensor(xo[s][:, cs], tq, xs[s][:, cs], ALU.add), r=["tq", ("x", s)], w=[("xo", s)])
            dma("sp", tok_dst(t), xo[s], r=[("xo", s)])
        S.barrier()
        A.release(m)

    def phase_c0(l, ctx_full, affT):
        m = A.mark()
        gm = [A.f32(D) for _ in range(2)]
        sh = [A.f32(D) for _ in range(2)]
        gtmp = A.f32(D)
        bc_row(gtmp, norm2_g[l:l + 1, :], "g2row")
        for r in range(2):
            load_mod_bc(gm[r], r, 4, ("gm", r))
            load_mod_bc(sh[r], r, 3, ("sh", r))
            op("dve", lambda e, r=r: e.scalar_tensor_tensor(gm[r], gm[r], 1.0, gtmp, ALU.add, ALU.mult),
               r=["g2row", ("gm", r)], w=[("gm", r)])
        rw = v3(A.f32(KC * NE), NE)
        dma("sp", rw, router_w[l].rearrange("(k p) n -> p k n", p=P), w=["rw"])
        xs = [A.f32(D) for _ in range(2)]
        hn = A.f32(D)
        h2f = [A.f32(D) for _ in range(2)]
        h2b = [A.bf16(D) for _ in range(2)]
        h2T = v3(A.f32(D), P)
        junk = A.bf16(D)
        ss = A.f32(2); t1 = A.f32(2); t2 = A.f32(2); rstd = A.f32(2)
        mx = A.f32(2); se = A.f32(2); ex = A.f32(NE); aff = A.f32(NE)
        t_list = list(range(0 if ctx_full else 2, 18))
        for it, t in enumerate(t_list):
            s = it % 2
            r = 1 if t < 2 else 0
            dma("sp", xs[s], tok_dst(t), w=[("x", s)])
            op("act", lambda e, s=s: e.activation(junk, xs[s], AF.Square, accum_out=ss[:, 0:1]), r=[("x", s)], w=["junk", "c0ss"])
            rstd_from_ss(ss[:, 0:1], D, t1[:, 0:1], t2[:, 0:1], rstd[:, 0:1], "c0")
            op("dve", lambda e, s=s, r=r: e.scalar_tensor_tensor(hn, xs[s], rstd[:, 0:1], gm[r], ALU.mult, ALU.mult),
               r=[("x", s), "c0rstd", ("gm", r)], w=["hn"])
            op("pool", lambda e, s=s, r=r: e.tensor_tensor(h2f[s], hn, sh[r], ALU.add), r=["hn", ("sh", r)], w=[("h2f", s)])
            op("act", lambda e, s=s: e.copy(h2b[s], h2f[s]), r=[("h2f", s)], w=[("h2b", s)])
            dst = h2c[t * P:(t + 1) * P, :] if t < 2 else h2t[(t - 2) * P:(t - 1) * P, :]
            dma("sp", dst, h2b[s], r=[("h2b", s)])
            for k4 in range(4):
                bk = k4
                for kk in range(4):
                    k = k4 * 4 + kk
                    op("pe", lambda e, s=s, k=k, kk=kk, bk=bk: e.transpose(bank(bk)[:, kk * P:(kk + 1) * P], h2f[s][:, k * P:(k + 1) * P], ident_f),
                       r=[("h2f", s), "ident_f"], w=[PSK(bk)])
                if k4 % 2 == 0:
                    op("act", lambda e, k4=k4, bk=bk: e.copy(h2T[:, k4 * 4:k4 * 4 + 4, :], v3(bank(bk), P)), r=[PSK(bk)], w=[("h2T", k4)])
                else:
                    op("dve", lambda e, k4=k4, bk=bk: e.tensor_copy(h2T[:, k4 * 4:k4 * 4 + 4, :], v3(bank(bk), P)), r=[PSK(bk)], w=[("h2T", k4)])
            pl = 4 + (it % 2)
            for k in range(KC):
                op("pe", lambda e, k=k, pl=pl: e.matmul(bank(pl)[:, 0:NE], h2T[:, k, :], rw[:, k, :], start=(k == 0), stop=(k == KC - 1)),
                   r=[("h2T", k // 4), "rw"], w=[PSK(pl)])
            op("dve", lambda e, pl=pl: e.reduce_max(mx[:, 0:1], bank(pl)[:, 0:NE], AX.X), r=[PSK(pl)], w=["mx"])
            op("dve", lambda e: e.tensor_scalar(mx[:, 0:1], mx[:, 0:1], -1.0, None, ALU.mult), r=["mx"], w=["mx"])
            op("act", lambda e, pl=pl: e.activation(ex, bank(pl)[:, 0:NE], AF.Exp, bias=mx[:, 0:1], accum_out=se[:, 0:1]), r=[PSK(pl), "mx"], w=["ex", "se"])
            op("dve", lambda e: e.reciprocal(se[:, 0:1], se[:, 0:1]), r=["se"], w=["se"])
            op("dve", lambda e: e.tensor_scalar(aff, ex, se[:, 0:1], None, ALU.mult), r=["ex", "se"], w=["aff"])
            pt = 6 + (it % 2)
            op("pe", lambda e, pt=pt: e.transpose(bank(pt)[0:NE, 0:P], aff, ident_f), r=["aff", "ident_f"], w=[PSK(pt)])
            op("act", lambda e, pt=pt, t=t: e.copy(affT[0:NE, t * P:(t + 1) * P], bank(pt)[0:NE, 0:P]), r=[PSK(pt)], w=[("affT", t)])
        if "dbg_aff" in dbg:
            dma("sp", dbg_aff, affT[0:NE, :], r=[("affT", t) for t in t_list])
        S.barrier()
        A.release(m)

    def phase_c1(ctx_full, affT, idxT, valT):
        m = A.mark()
        work = A.f32(NLAT)
        vals = A.f32(288)
        idx = A.u32(288)
        idxf = A.f32(288)
        op("dve", lambda e: e.tensor_copy(work[0:NE, :], affT[0:NE, NCTX:NTOK]), w=["work"])
        for r_ in range(32):
            vs = slice(r_ * 8, r_ * 8 + 8)
            op("dve", lambda e, vs=vs: e.max(vals[0:NE, vs], work[0:NE, :]), r=["work"], w=["vals"])
            op("dve", lambda e, vs=vs: e.max_index(idx[0:NE, vs], vals[0:NE, vs], work[0:NE, :]), r=["work", "vals"], w=["idx"])
            op("dve", lambda e, vs=vs: e.match_replace(work[0:NE, :], vals[0:NE, vs], work[0:NE, :], -1.0), r=["work", "vals", "idx"], w=["work"])
        if ctx_full:
            wc = work[0:NE, 0:NCTX]
            op("dve", lambda e: e.tensor_copy(wc, affT[0:NE, 0:NCTX]), r=["work"], w=["work"])
            for r_ in range(4):
                vs = slice(256 + r_ * 8, 256 + r_ * 8 + 8)
                op("dve", lambda e, vs=vs: e.max(vals[0:NE, vs], wc), r=["work"], w=["vals"])
                op("dve", lambda e, vs=vs: e.max_index(idx[0:NE, vs], vals[0:NE, vs], wc), r=["work", "vals"], w=["idx"])
                op("dve", lambda e, vs=vs: e.match_replace(wc, vals[0:NE, vs], wc, -1.0), r=["work", "vals", "idx"], w=["work"])
        NS = 288 if ctx_full else 256
        op("dve", lambda e: e.tensor_copy(idxf[0:NE, 0:NS], idx[0:NE, 0:NS]), r=["idx"], w=["idxf"])
        chunks = [(0, 128), (128, 128)] + ([(256, 32)] if ctx_full else [])
        for ci, (c0, cn) in enumerate(chunks):
            op("pe", lambda e, c0=c0, cn=cn, ci=ci: e.transpose(bank(ci)[0:cn, 0:NE], idxf[0:NE, c0:c0 + cn], ident_f[0:NE, 0:NE]), r=["idxf", "ident_f"], w=[PSK(ci)])
            op("dve", lambda e, cn=cn, ci=ci: e.tensor_copy(idxT[ci][0:cn, :], bank(ci)[0:cn, 0:NE]), r=[PSK(ci)], w=[("idxT", ci)])
            op("pe", lambda e, c0=c0, cn=cn, ci=ci: e.transpose(bank(4 + ci)[0:cn, 0:NE], vals[0:NE, c0:c0 + cn], ident_f[0:NE, 0:NE]), r=["vals", "ident_f"], w=[PSK(4 + ci)])
            op("act", lambda e, cn=cn, ci=ci: e.copy(valT[ci][0:cn, :], bank(4 + ci)[0:cn, 0:NE]), r=[PSK(4 + ci)], w=[("valT", ci)])
        S.barrier()
        A.release(m)

    def phase_c2(l, ctx_full, idxT, valT):
        m = A.mark()
        g2 = [A.f32(D) for _ in range(2)]
        for r in range(2):
            load_mod_bc(g2[r], r, 5, ("g2", r))
        NGU = 4
        NDN = 2
        gus = [v3(A.bf16(KC * 512), 512) for _ in range(NGU)]
        dns = [v3(A.bf16(KC * 512), 512) for _ in range(NDN)]
        xs = v3(A.bf16(3 * D), D)
        NS = 288 if ctx_full else 256
        xsT = v3(A.bf16(KC * 288), 288)
        aT = v3(A.bf16(KC * 288), 288)
        sg = [A.f32(288) for _ in range(2)]
        yst = v3(A.f32(3 * D), D)
        chunks = [(0, 128), (128, 128)] + ([(256, 32)] if ctx_full else [])
        jobs_gu = [(e_, fg, wh) for e_ in range(NE) for fg in range(4) for wh in range(2)]
        jobs_dn = [(e_, cg) for e_ in range(NE) for cg in range(4)]
        st = dict(gu_issued=0, gu_done=0, dn_issued=0, dn_done=0)

        def pump_gu():
            while st["gu_issued"] < len(jobs_gu) and st["gu_issued"] < st["gu_done"] + NGU:
                j = st["gu_issued"]
                e_, fg, wh = jobs_gu[j]
                src = (w_gate if wh == 0 else w_up)[l, e_, fg].rearrange("p (k n) -> p k n", n=512)
                dma("pool", gus[j % NGU], src, w=[("gu", j % NGU)])
                st["gu_issued"] += 1

        def pump_dn():
            while st["dn_issued"] < len(jobs_dn) and st["dn_issued"] < st["dn_done"] + NDN:
                j = st["dn_issued"]
                e_, cg = jobs_dn[j]
                src = w_down[l, e_, cg].rearrange("p (k n) -> p k n", n=512)
                dma("pool", dns[j % NDN], src, w=[("dn", j % NDN)])
                st["dn_issued"] += 1

        def gather(e_):
            for ci, (c0, cn) in enumerate(chunks):
                srcd = h2t if ci < 2 else h2c
                op("pool", lambda e, ci=ci, cn=cn, srcd=srcd, e_=e_: e.indirect_dma_start(
                    out=xs[0:cn, ci, :], out_offset=None, in_=srcd,
                    in_offset=bass.IndirectOffsetOnAxis(ap=idxT[ci][0:cn, e_:e_ + 1], axis=0)),
                   r=[("idxT", ci)], w=[("xs", ci)], dma=True)

        def transposeT(e_):
            for ci, (c0, cn) in enumerate(chunks):
                for k4 in range(4):
                    bk = (ci * 4 + k4) % 2
                    for kk in range(4):
                        k = k4 * 4 + kk
                        op("pe", lambda e, ci=ci, cn=cn, k=k, kk=kk, bk=bk: e.transpose(bank16(bk)[:, kk * P:kk * P + cn], xs[0:cn, ci, k * P:(k + 1) * P], ident_b[0:cn, 0:cn]),
                           r=[("xs", ci), "ident_b"], w=[PSK(bk)])
                    src = v3(bank16(bk)[:, 0:512], P)[:, :, 0:cn]
                    dstv = xsT[:, k4 * 4:k4 * 4 + 4, c0:c0 + cn]
                    if k4 % 2 == 0:
                        op("act", lambda e, src=src, dstv=dstv: e.copy(dstv, src), r=[PSK(bk)], w=["xsT"])
                    else:
                        op("dve", lambda e, src=src, dstv=dstv: e.tensor_copy(dstv, src), r=[PSK(bk)], w=["xsT"])

        pump_gu()
        pump_dn()
        gather(0)
        transposeT(0)
        for e_ in range(NE):
            if e_ + 1 < NE:
                gather(e_ + 1)
            for fg in range(4):
                jg = (e_ * 4 + fg) * 2
                sg_ = jg % NGU
                su_ = (jg + 1) % NGU
                for fc in range(4):
                    fcn = fg * 4 + fc
                    pg = 2 + 2 * (fcn % 2)
                    pu = pg + 1
                    for k in range(KC):
                        op("pe", lambda e, k=k, fc=fc, pg=pg, sg_=sg_: e.matmul(bank(pg)[:, 0:NS], gus[sg_][:, k, fc * P:(fc + 1) * P], xsT[:, k, 0:NS], start=(k == 0), stop=(k == KC - 1)),
                           r=[("gu", sg_), "xsT"], w=[PSK(pg)])
                    for k in range(KC):
                        op("pe", lambda e, k=k, fc=fc, pu=pu, su_=su_: e.matmul(bank(pu)[:, 0:NS], gus[su_][:, k, fc * P:(fc + 1) * P], xsT[:, k, 0:NS], start=(k == 0), stop=(k == KC - 1)),
                           r=[("gu", su_), "xsT"], w=[PSK(pu)])
                    sgt = sg[fcn % 2]
                    op("act", lambda e, pg=pg, sgt=sgt: e.activation(sgt[:, 0:NS], bank(pg)[:, 0:NS], AF.Silu), r=[PSK(pg)], w=[("sg", fcn % 2)])
                    op("dve", lambda e, pu=pu, sgt=sgt, fcn=fcn: e.tensor_tensor(aT[:, fcn, 0:NS], sgt[:, 0:NS], bank(pu)[:, 0:NS], ALU.mult),
                       r=[PSK(pu), ("sg", fcn % 2)], w=[("aT", fcn)])
                st["gu_done"] += 2
                pump_gu()
            if e_ + 1 < NE:
                transposeT(e_ + 1)
            for cg in range(4):
                cs = slice(cg * 512, (cg + 1) * 512)
                sd_ = (e_ * 4 + cg) % NDN
                for ci, (c0, cn) in enumerate(chunks):
                    pb = 6 + ((cg * 3 + ci) % 2)
                    r = 1 if ci == 2 else 0
                    for k in range(KC):
                        op("pe", lambda e, k=k, c0=c0, cn=cn, pb=pb, sd_=sd_: e.matmul(bank(pb)[0:cn, :], aT[:, k, c0:c0 + cn], dns[sd_][:, k, :], start=(k == 0), stop=(k == KC - 1)),
                           r=[("dn", sd_)] + [("aT", kk) for kk in range(KC)], w=[PSK(pb)])
                    op("dve", lambda e, ci=ci, cn=cn, pb=pb, r=r, cs=cs, e_=e_: e.scalar_tensor_tensor(yst[0:cn, ci, cs], bank(pb)[0:cn, :], valT[ci][0:cn, e_:e_ + 1], g2[r][0:cn, cs], ALU.mult, ALU.mult),
                       r=[PSK(pb), ("valT", ci), ("g2", r)], w=[("yst", ci)])
                st["dn_done"] += 1
                pump_dn()
            for ci, (c0, cn) in enumerate(chunks):
                dstd = xl if ci < 2 else xc
                op("pool", lambda e, ci=ci, cn=cn, dstd=dstd, e_=e_: e.indirect_dma_start(
                    out=dstd, out_offset=bass.IndirectOffsetOnAxis(ap=idxT[ci][0:cn, e_:e_ + 1], axis=0),
                    in_=yst[0:cn, ci, :], in_offset=None, compute_op=ALU.add),
                   r=[("yst", ci), ("idxT", ci)], w=["xl_scatter" if ci < 2 else "xc_scatter"], dma=True)
        S.barrier()
        A.release(m)

    def phase_final():
        m = A.mark()
        g = A.f32(D)
        bc_row(g, final_g[0:1, :], "fg")
        xs = [A.f32(D) for _ in range(2)]
        xo = [A.f32(D) for _ in range(2)]
        junk = A.bf16(D)
        ss = A.f32(2); t1 = A.f32(2); t2 = A.f32(2); rstd = A.f32(2)
        src = xl if n_layers > 0 else x_in
        for t in range(16):
            s = t % 2
            dma("sp", xs[s], src[t * P:(t + 1) * P, :], w=[("x", s)])
            op("act", lambda e, s=s: e.activation(junk, xs[s], AF.Square, accum_out=ss[:, 0:1]), r=[("x", s)], w=["junk", "fss"])
            rstd_from_ss(ss[:, 0:1], D, t1[:, 0:1], t2[:, 0:1], rstd[:, 0:1], "f")
            op("dve", lambda e, s=s: e.scalar_tensor_tensor(xo[s], xs[s], rstd[:, 0:1], g, ALU.mult, ALU.mult), r=[("x", s), "frstd", "fg"], w=[("xo", s)])
            dma("sp", out[t * P:(t + 1) * P, :], xo[s], r=[("xo", s)])
        S.barrier()
        A.release(m)

    layer_mark = A.mark()

    def _gg(gre, gim):
        return gre
    if only == "b2":
        uT = v3(A.bf16(4 * NTOK), NTOK)
        for cq in range(4):
            op("dve", lambda e, cq=cq: e.memset(uT[:, cq, :], 0.5), w=[("uT", cq)])
        S.barrier()
        phase_b2(0, True, uT)
        n_layers = 0
        stop_after = ("x", 0)
    stages = []
    for l in range(n_layers):
        last = (l == DEPTH - 1)
        ctx_full = not last
        phase_ada(l)
        if stop_after == ("ada", l):
            break
        m0 = A.mark()
        uT = v3(A.bf16(4 * NTOK), NTOK)
        m1 = A.mark()
        hT = v3(A.bf16(KC * NTOK), NTOK)
        phase_b0(l, True, hT)
        phase_b1(l, ctx_full, hT, uT)
        A.release(m1)
        if stop_after == ("b1", l):
            break
        phase_b2(l, ctx_full, uT)
        A.release(m0)
        if stop_after == ("b2", l):
            break
        phase_b3(l, ctx_full)
        if stop_after == ("b3", l):
            break
        mc_ = A.mark()
        idxT = [A.u32(NE) for _ in range(3)]
        valT = [A.f32(NE) for _ in range(3)]
        m_aff = A.mark()
        affT = A.f32(NTOK)
        phase_c0(l, ctx_full, affT)
        phase_c1(ctx_full, affT, idxT, valT)
        A.release(m_aff)
        if stop_after == ("c1", l):
            break
        phase_c2(l, ctx_full, idxT, valT)
        A.release(mc_)
    if stop_after is None:
        phase_final()
    S.barrier()
    S.emit()
    stack.close()
    return nc


def prep_shared(inp):
    f = np.float32
    sh = {}
    def tile_w(w, ncg):
        lead = w.shape[:-2]
        nl = len(lead)
        t = w.reshape(*lead, KC, P, ncg, 512)
        t = t.transpose(*range(nl), nl + 2, nl + 1, nl, nl + 3)
        return np.ascontiguousarray(t, f).reshape(*lead, ncg, P, KC * 512)
    sh["ada_w"] = tile_w(inp["ada_w"], 24)
    sh["ada_b2"] = np.ascontiguousarray(np.repeat(inp["ada_b"][:, None, :], 2, axis=1), f)
    sh["norm1_g"] = np.ascontiguousarray(inp["norm1_g"], f)
    sh["norm2_g"] = np.ascontiguousarray(inp["norm2_g"], f)
    sh["final_g"] = np.ascontiguousarray(inp["final_norm_g"].reshape(1, D), f)
    w_in = inp["w_in"]
    sh["w_in_t"] = np.ascontiguousarray(w_in.reshape(DEPTH, KC, P, 40, P).transpose(0, 3, 2, 1, 4), f)
    cw = inp["conv_w"]
    sh["conv_wc"] = np.ascontiguousarray(cw.reshape(DEPTH, 3, 12, P).transpose(0, 3, 2, 1).reshape(DEPTH, P, 36), f)

    def col16(a):
        return np.ascontiguousarray(a.reshape(DEPTH, 2, 16, 2, 64).transpose(0, 1, 3, 4, 2).reshape(DEPTH, 2, P, 16), f)

    sh["lamre"] = col16(inp["ssm_lam_re"])
    sh["lamim"] = col16(inp["ssm_lam_im"])
    sh["logdt"] = col16(np.repeat(inp["ssm_log_dt"][..., None], 64, axis=-1))

    def bt(b):
        o = np.zeros((DEPTH, 2, 4, P, 4, P), f)
        bb = b.reshape(DEPTH, 2, 4, 4, 2, 64, 16)
        for jl in range(4):
            for gl in range(2):
                o[:, :, :, 32 * jl + 16 * gl:32 * jl + 16 * gl + 16, jl, gl * 64:(gl + 1) * 64] = bb[:, :, :, jl, gl].transpose(0, 1, 2, 4, 3)
        return o.reshape(DEPTH, 2, 4, P, 4 * P)

    def ct(c):
        o = np.zeros((DEPTH, 2, 4, P, 4, P), f)
        cc = c.reshape(DEPTH, 2, 4, 4, 2, 16, 64)
        for jl in range(4):
            for gl in range(2):
                o[:, :, :, gl * 64:(gl + 1) * 64, jl, 32 * jl + 16 * gl:32 * jl + 16 * gl + 16] = cc[:, :, :, jl, gl].transpose(0, 1, 2, 4, 3)
        return o.reshape(DEPTH, 2, 4, P, 4 * P)

    sh["bt_re"] = bt(inp["ssm_b_re"]); sh["bt_im"] = bt(inp["ssm_b_im"])
    sh["ct_re"] = ct(inp["ssm_c_re"]); sh["ct_im"] = ct(inp["ssm_c_im"])
    sh["dcol"] = np.ascontiguousarray(inp["ssm_d"].reshape(DEPTH, 4, P).transpose(0, 2, 1), f)
    sh["w_glu"] = np.ascontiguousarray(inp["ssm_w_glu"], f)
    sh["gconv"] = np.ascontiguousarray(inp["out_norm_conv_g"].reshape(DEPTH, 12, P).transpose(0, 2, 1), f)
    sh["gssm"] = np.ascontiguousarray(inp["out_norm_ssm_g"].reshape(DEPTH, 4, P).transpose(0, 2, 1), f)
    sh["w_out"] = tile_w(inp["w_out"], 4)
    sh["router_w"] = np.ascontiguousarray(inp["router_w"], f)
    sh["w_gate"] = tile_w(inp["exp_w_gate"], 4)
    sh["w_up"] = tile_w(inp["exp_w_up"], 4)
    sh["w_down"] = tile_w(inp["exp_w_down"], 4)
    return sh


def prep_core(inp, b):
    f = np.float32
    d = {}
    d["x"] = np.ascontiguousarray(inp["x"][b], f)
    d["ctx"] = np.ascontiguousarray(inp["ctx"][b], f)
    cc = np.zeros((P, KC, 2), f)
    cc[:, :, 0] = inp["c"][b].reshape(KC, P).T
    cc[:, :, 1] = inp["c_ctx"].reshape(KC, P).T
    d["ccol"] = cc.reshape(P, 32)
    return d


def kernel(**inputs):
    inp = {k: np.asarray(v) for k, v in inputs.items()}
    sh = prep_shared(inp)
    nc = build_program()
    in_maps = []
    for b in range(8):
        m = dict(sh)
        m.update(prep_core(inp, b))
        in_maps.append(m)
    res = run_bass_kernel_spmd(nc, in_maps, core_ids=list(range(8)))
    return np.stack([res.results[b]["out"] for b in range(8)], axis=0).astype(np.float32)
```

```python
import math
import numpy as np
import concourse.bass as bass
import concourse.mybir as mybir
from concourse.bass_utils import run_bass_kernel_spmd

F32 = mybir.dt.float32
BF16 = mybir.dt.bfloat16
U32 = mybir.dt.uint32
I32 = mybir.dt.int32
ALU = mybir.AluOpType
AF = mybir.ActivationFunctionType
AX = mybir.AxisListType

P = 128
D = 2048
KC = 16
NLAT = 2048
NCTX = 256
NTOK = NLAT + NCTX
DEPTH = 2
NE = 16
EPS = 1e-6
TWO_PI = 2.0 * math.pi
PI_LO = 3.1415925
ENGS = ["pe", "act", "dve", "pool", "sp"]
NDMA = 40


class Sched:
    def __init__(self, nc, stack):
        self.nc = nc
        self.sems = []
        self.eng_sem = {}
        for e in ENGS:
            self.eng_sem[e] = len(self.sems)
            self.sems.append(stack.enter_context(nc.semaphore("s_" + e)))
        self.dma_sem = []
        for i in range(NDMA):
            self.dma_sem.append(len(self.sems))
            self.sems.append(stack.enter_context(nc.semaphore("d_%d" % i)))
        self.dma_val = [0] * NDMA
        self.dma_next = 0
        self.cnt = {e: 0 for e in ENGS}
        self.recs = {e: [] for e in ENGS}
        self.seen = {e: {} for e in ENGS}
        self.lastw = {}
        self.rd_eng = {}
        self.rd_dma = {}

    def _need(self, eng, tok, waits):
        semi, val, src, is_dma = tok
        if self.seen[eng].get(semi, 0) >= val:
            return
        if (not is_dma) and src == eng and eng == "pe":
            return
        self.seen[eng][semi] = val
        waits.append((semi, val))

    def op(self, eng, fn, r=(), w=(), dma=False):
        waits = []
        for k in r:
            t = self.lastw.get(k)
            if t is not None:
                self._need(eng, t, waits)
        for k in w:
            t = self.lastw.get(k)
            if t is not None:
                self._need(eng, t, waits)
            for se, t in self.rd_eng.get(k, {}).items():
                self._need(eng, t, waits)
            for t in self.rd_dma.get(k, ()):
                self._need(eng, t, waits)
        if dma:
            slot = self.dma_next % NDMA
            self.dma_next += 1
            semi = self.dma_sem[slot]
            prev = self.dma_val[slot]
            if prev > self.seen[eng].get(semi, 0):
                waits.append((semi, prev))
                self.seen[eng][semi] = prev
            val = prev + 16
            self.dma_val[slot] = val
            tok = (semi, val, eng, True)
        else:
            self.cnt[eng] += 1
            tok = (self.eng_sem[eng], self.cnt[eng], eng, False)
        self.recs[eng].append((waits, fn, tok))
        for k in r:
            if dma:
                self.rd_dma.setdefault(k, []).append(tok)
            else:
                self.rd_eng.setdefault(k, {})[eng] = tok
        for k in w:
            self.lastw[k] = tok
            self.rd_eng[k] = {}
            self.rd_dma[k] = []
        return tok

    def barrier(self):
        toks = [(self.eng_sem[e], self.cnt[e], e, False) for e in ENGS if self.cnt[e] > 0]
        toks += [(self.dma_sem[i], self.dma_val[i], None, True) for i in range(NDMA) if self.dma_val[i] > 0]
        for e in ENGS:
            waits = []
            for t in toks:
                if t[2] == e and not t[3]:
                    continue
                if self.seen[e].get(t[0], 0) >= t[1]:
                    continue
                self.seen[e][t[0]] = t[1]
                waits.append((t[0], t[1]))
            if waits:
                self.recs[e].append((waits, None, None))
        self.lastw.clear()
        self.rd_eng.clear()
        self.rd_dma.clear()

    def emit(self):
        nc = self.nc
        sems = self.sems
        recs = self.recs

        def mk(name):
            def body(e):
                for waits, fn, tok in recs[name]:
                    for (s, v) in waits:
                        e.wait_ge(sems[s], v)
                    if fn is not None:
                        ins = fn(e)
                        ins.then_inc(sems[tok[0]], 16 if tok[3] else 1)
            return body

        with nc.Block() as block:
            block.tensor(mk("pe"))
            block.scalar(mk("act"))
            block.vector(mk("dve"))
            block.gpsimd(mk("pool"))
            block.sync(mk("sp"))


class Arena:
    def __init__(self, ap, ncols):
        self.ap = ap
        self.n = ncols
        self.off = 0

    def mark(self):
        return self.off

    def release(self, m):
        self.off = m

    def f32(self, cols):
        cols = (cols + 1) // 2 * 2
        assert self.off + cols <= self.n, ("SBUF arena overflow", self.off, cols, self.n)
        a = self.ap[:, self.off:self.off + cols]
        self.off += cols
        return a

    def bf16(self, cols):
        return self.f32((cols + 1) // 2).bitcast(BF16)[:, 0:cols]

    def u32(self, cols):
        return self.f32(cols).bitcast(U32)

    def i32(self, cols):
        return self.f32(cols).bitcast(I32)


def v3(ap, b):
    return ap.rearrange("p (a b) -> p a b", b=b)


def build_program(dbg=None, stop_after=None, n_layers=DEPTH, only=None):
    from contextlib import ExitStack
    dbg = dbg or []
    nc = bass.Bass("TRN2", target_bir_lowering=False)

    def din(name, shape, dt=F32):
        if only == "b2" and int(np.prod(shape)) > 4_000_000:
            shape = [2, 2]
        return nc.dram_tensor(name, list(shape), dt, kind="ExternalInput").ap()

    x_in = din("x", [NLAT, D])
    ctx_in = din("ctx", [NCTX, D])
    ccol = din("ccol", [P, 32])
    ada_w = din("ada_w", [DEPTH, 24, P, KC * 512])
    ada_b2 = din("ada_b2", [DEPTH, 2, 6 * D])
    norm1_g = din("norm1_g", [DEPTH, D])
    norm2_g = din("norm2_g", [DEPTH, D])
    final_g = din("final_g", [1, D])
    w_in_t = din("w_in_t", [DEPTH, 40, P, KC, P])
    conv_wc = din("conv_wc", [DEPTH, P, 36])
    lamre = din("lamre", [DEPTH, 2, P, 16])
    lamim = din("lamim", [DEPTH, 2, P, 16])
    logdt = din("logdt", [DEPTH, 2, P, 16])
    bt_re = din("bt_re", [DEPTH, 2, 4, P, 4 * P])
    bt_im = din("bt_im", [DEPTH, 2, 4, P, 4 * P])
    ct_re = din("ct_re", [DEPTH, 2, 4, P, 4 * P])
    ct_im = din("ct_im", [DEPTH, 2, 4, P, 4 * P])
    dcol = din("dcol", [DEPTH, P, 4])
    w_glu = din("w_glu", [DEPTH, 512, 512])
    gconv = din("gconv", [DEPTH, P, 12])
    gssm = din("gssm", [DEPTH, P, 4])
    w_out = din("w_out", [DEPTH, 4, P, KC * 512])
    router_w = din("router_w", [DEPTH, D, NE])
    small_exp = stop_after is not None and stop_after[0] != "c2"
    if small_exp:
        w_gate = w_up = w_down = None
    else:
        w_gate = din("w_gate", [DEPTH, NE, 4, P, KC * 512])
        w_up = din("w_up", [DEPTH, NE, 4, P, KC * 512])
        w_down = din("w_down", [DEPTH, NE, 4, P, KC * 512])
    out = nc.dram_tensor("out", [NLAT, D], F32, kind="ExternalOutput").ap()

    def dscr(name, shape, dt):
        kind = "ExternalOutput" if name in dbg else "Internal"
        return nc.dram_tensor(name, list(shape), dt, kind=kind).ap()

    xl = dscr("xl", [NLAT, D], F32)
    xc = dscr("xc", [NCTX, D], F32)
    h2t = dscr("h2t", [NLAT, D], BF16)
    h2c = dscr("h2c", [NCTX, D], BF16)
    modrow = dscr("modrow", [2, 6 * D], F32)
    mixn = dscr("mixn", [16, P, NTOK], BF16)
    dbg_aff = dscr("dbg_aff", [NE, NTOK], F32)
    dbg_y = dscr("dbg_y", [4, P, NTOK], F32)

    stack = ExitStack()
    NCOLS = 44800
    arena_t = stack.enter_context(nc.sbuf_tensor("arena", [P, NCOLS], F32))
    ps_t = stack.enter_context(nc.psum_tensor("ps", [P, 4096], F32))
    S = Sched(nc, stack)
    A = Arena(arena_t, NCOLS)
    op = S.op

    def bank(i):
        return ps_t[:, i * 512:(i + 1) * 512]

    def bank16(i):
        return ps_t[:, i * 512:(i + 1) * 512].bitcast(BF16)

    def PSK(i):
        return ("ps", i)

    ident_f = A.f32(P)
    ident_b = A.bf16(P)
    ones_b = A.bf16(P)
    iota_i = A.i32(P)
    iota_t = A.f32(P)
    op("pool", lambda e: e.iota(iota_i, [[1, P]], base=0, channel_multiplier=-1), w=["iota_i"])
    op("dve", lambda e: e.tensor_single_scalar(ident_f, iota_i, 0, ALU.is_equal), r=["iota_i"], w=["ident_f"])
    op("dve", lambda e: e.tensor_copy(ident_b, ident_f), r=["ident_f"], w=["ident_b"])
    op("dve", lambda e: e.memset(ones_b, 1.0), w=["ones_b"])
    op("pool", lambda e: e.iota(iota_i, [[1, P]], base=0, channel_multiplier=0), r=["ident_f"], w=["iota_i"])
    op("dve", lambda e: e.tensor_copy(iota_t, iota_i), r=["iota_i"], w=["iota_t"])
    S.barrier()
    base_mark = A.mark()

    def dma(q, out_ap, in_ap, r=(), w=()):
        if q == "pool":
            return op(q, lambda e: e.dma_start(out=out_ap, in_=in_ap, max_dma_last_dim=8192), r=r, w=w, dma=True)
        return op(q, lambda e: e.dma_start(out=out_ap, in_=in_ap), r=r, w=w, dma=True)

    def bc_row(dst, src_row, key):
        dma("sp", dst, src_row.partition_broadcast(P), w=[key])

    def rstd_from_ss(ss, n, tmp1, tmp2, rstd, keyp):
        op("dve", lambda e: e.tensor_scalar(tmp1, ss, 1.0 / n, EPS, ALU.mult, ALU.add), r=[keyp + "ss"], w=[keyp + "t1"])
        op("act", lambda e: e.activation(tmp2, tmp1, AF.Sqrt), r=[keyp + "t1"], w=[keyp + "t2"])
        op("dve", lambda e: e.reciprocal(rstd, tmp2), r=[keyp + "t2"], w=[keyp + "rstd"])

    def tok_src(l, t):
        if t < 2:
            src = ctx_in if l == 0 else xc
            return src[t * P:(t + 1) * P, :]
        src = x_in if l == 0 else xl
        return src[(t - 2) * P:(t - 1) * P, :]

    def tok_dst(t):
        if t < 2:
            return xc[t * P:(t + 1) * P, :]
        return xl[(t - 2) * P:(t - 1) * P, :]

    def phase_ada(l):
        m = A.mark()
        cc = A.f32(32)
        scT = A.bf16(32)
        wsl = [v3(A.bf16(KC * 512), 512) for _ in range(2)]
        bsl = [A.f32(512) for _ in range(2)]
        msl = [A.f32(512) for _ in range(2)]
        dma("sp", cc, ccol, w=["cc"])
        op("act", lambda e: e.activation(scT, cc, AF.Silu), r=["cc"], w=["scT"])
        scT3 = v3(scT, 2)
        for n in range(24):
            s = n % 2
            cs = slice(n * 512, (n + 1) * 512)
            dma("pool", wsl[s], ada_w[l, n].rearrange("p (k n) -> p k n", n=512), w=[("aw", s)])
            dma("sp", bsl[s][0:2, :], ada_b2[l][:, cs], w=[("ab", s)])
            for k in range(KC):
                op("pe", lambda e, k=k, s=s: e.matmul(bank(s)[0:2, :], scT3[:, k, :], wsl[s][:, k, :], start=(k == 0), stop=(k == KC - 1)),
                   r=["scT", ("aw", s)], w=[PSK(s)])
            op("dve", lambda e, s=s: e.tensor_tensor(msl[s][0:2, :], bank(s)[0:2, :], bsl[s][0:2, :], ALU.add),
               r=[PSK(s), ("ab", s)], w=[("ms", s)])
            dma("sp", modrow[:, cs], msl[s][0:2, :], r=[("ms", s)])
        S.barrier()
        A.release(m)

    def load_mod_bc(dst, r, seg, key):
        bc_row(dst, modrow[r:r + 1, seg * D:(seg + 1) * D], key)

    def phase_b0(l, ctx_any, hT):
        m = A.mark()
        gm = [A.f32(D) for _ in range(2)]
        sh = [A.f32(D) for _ in range(2)]
        xs = [A.f32(D) for _ in range(2)]
        hn = A.f32(D)
        gtmp = hn
        bc_row(gtmp, norm1_g[l:l + 1, :], "hn")
        for r in range(2):
            load_mod_bc(gm[r], r, 1, ("gm", r))
            load_mod_bc(sh[r], r, 0, ("sh", r))
            op("dve", lambda e, r=r: e.scalar_tensor_tensor(gm[r], gm[r], 1.0, gtmp, ALU.add, ALU.mult),
               r=["hn", ("gm", r)], w=[("gm", r)])
        hb = [A.bf16(D) for _ in range(2)]
        junk = hn.bitcast(BF16)[:, 0:D]
        ss = A.f32(2); t1 = A.f32(2); t2 = A.f32(2); rstd = A.f32(2)
        for t in range(NTOK // P):
            s = t % 2
            r = 1 if t < 2 else 0
            dma("sp", xs[s], tok_src(l, t), w=[("x", s)])
            op("act", lambda e, s=s: e.activation(junk, xs[s], AF.Square, accum_out=ss[:, 0:1]), r=[("x", s)], w=["hn", "b0ss"])
            rstd_from_ss(ss[:, 0:1], D, t1[:, 0:1], t2[:, 0:1], rstd[:, 0:1], "b0")
            op("dve", lambda e, s=s, r=r: e.scalar_tensor_tensor(hn, xs[s], rstd[:, 0:1], gm[r], ALU.mult, ALU.mult),
               r=[("x", s), "b0rstd", ("gm", r)], w=["hn"])
            op("pool", lambda e, s=s, r=r: e.tensor_tensor(hb[s], hn, sh[r], ALU.add), r=["hn", ("sh", r)], w=[("hb", s)])
            for k4 in range(4):
                bk = (t * 4 + k4) % 8
                for kk in range(4):
                    k = k4 * 4 + kk
                    op("pe", lambda e, s=s, k=k, kk=kk, bk=bk: e.transpose(bank16(bk)[:, kk * P:(kk + 1) * P], hb[s][:, k * P:(k + 1) * P], ident_b),
                       r=[("hb", s), "ident_b"], w=[PSK(bk)])
                eng = "act" if k4 % 2 == 0 else "dve"
                if eng == "act":
                    op("act", lambda e, k4=k4, bk=bk, t=t: e.copy(hT[:, k4 * 4:k4 * 4 + 4, t * P:(t + 1) * P], v3(bank16(bk)[:, 0:512], P)),
                       r=[PSK(bk)], w=[("hT", t)])
                else:
                    op("dve", lambda e, k4=k4, bk=bk, t=t: e.tensor_copy(hT[:, k4 * 4:k4 * 4 + 4, t * P:(t + 1) * P], v3(bank16(bk)[:, 0:512], P)),
                       r=[PSK(bk)], w=[("hT", t)])
        S.barrier()
        A.release(m)

    def tiles_for(ctx_on):
        tl = []
        if ctx_on:
            tl.append((0, 256, 256))
        for i in range(4):
            tl.append((256 + 512 * i, 512, 64))
        return tl

    def headnorm_store(y, W, gcol, chunk, tok0, bufs, it):
        sq, ms, sd, yn = bufs
        pb = 3 + 4 * (it % 2)
        kp = "hn%d" % (it % 2)
        op("act", lambda e: e.activation(sq[:, 0:W], y[:, 0:W], AF.Square), r=[kp + "y"], w=[kp + "sq"])
        op("pe", lambda e: e.matmul(bank(pb)[:, 0:W], ones_b, sq[:, 0:W], start=True, stop=True), r=[kp + "sq", "ones_b"], w=[PSK(pb)])
        op("dve", lambda e: e.tensor_scalar(ms[:, 0:W], bank(pb)[:, 0:W], 1.0 / P, EPS, ALU.mult, ALU.add), r=[PSK(pb)], w=[kp + "ms"])
        op("act", lambda e: e.activation(sd[:, 0:W], ms[:, 0:W], AF.Sqrt), r=[kp + "ms"], w=[kp + "sd"])
        op("dve", lambda e: e.reciprocal(ms[:, 0:W], sd[:, 0:W]), r=[kp + "sd"], w=[kp + "ms"])
        op("dve", lambda e: e.scalar_tensor_tensor(yn[:, 0:W], y[:, 0:W], gcol, ms[:, 0:W], ALU.mult, ALU.mult),
           r=[kp + "y", kp + "ms", "gcols"], w=[kp + "yn"])
        dma("sp", mixn[chunk][:, tok0:tok0 + W], yn[:, 0:W], r=[kp + "yn"])

    def phase_b1(l, ctx_full, hT, uT):
        m = A.mark()
        cw = A.f32(36)
        gc = A.f32(12)
        dma("sp", cw, conv_wc[l], w=["cw"])
        dma("sp", gc, gconv[l], w=["gcols"])
        cw3 = v3(cw, 3)
        wsl = [[v3(A.bf16(KC * P), P) for _ in range(3)] for _ in range(2)]
        tmp = []
        for i in range(2):
            tmp.append(dict(c=A.f32(512), z=A.f32(512), acc=A.f32(512), y=A.f32(512),
                            sq=A.bf16(512), ms=A.f32(512), sd=A.f32(512), yn=A.bf16(512)))
        it = 0
        for k in range(12):
            ws = wsl[k % 2]
            for j in range(3):
                dma("pool", ws[j], w_in_t[l, j * 12 + k], w=[("win", k % 2, j)])
            for (tok0, W, RW) in tiles_for(ctx_full):
                T = tmp[it % 2]
                b0 = 4 * (it % 2)
                kp = "hn%d" % (it % 2)
                for j in range(3):
                    for kk in range(KC):
                        op("pe", lambda e, j=j, kk=kk, b0=b0, ws=ws, tok0=tok0, W=W: e.matmul(
                            bank(b0 + j)[:, 0:W], ws[j][:, kk, :], hT[:, kk, tok0:tok0 + W], start=(kk == 0), stop=(kk == KC - 1)),
                           r=[("win", k % 2, j)] + [("hT", tt) for tt in range(tok0 // P, (tok0 + W) // P)], w=[PSK(b0 + j)])
                op("act", lambda e, T=T, b0=b0, W=W: e.copy(T["c"][:, 0:W], bank(b0 + 1)[:, 0:W]), r=[PSK(b0 + 1)], w=[kp + "c"])
                op("dve", lambda e, T=T, b0=b0, W=W: e.tensor_tensor(T["z"][:, 0:W], bank(b0 + 2)[:, 0:W], T["c"][:, 0:W], ALU.mult),
                   r=[PSK(b0 + 2), kp + "c"], w=[kp + "z"])
                op("act", lambda e, T=T, W=W, k=k: e.activation(T["acc"][:, 0:W], T["z"][:, 0:W], AF.Copy, scale=cw3[:, k, 1:2]),
                   r=[kp + "z", "cw"], w=[kp + "acc"])
                zv = v3(T["z"][:, 0:W], RW)
                av = v3(T["acc"][:, 0:W], RW)
                op("dve", lambda e, zv=zv, av=av, k=k, RW=RW: e.scalar_tensor_tensor(av[:, :, 1:RW], zv[:, :, 0:RW - 1], cw3[:, k, 0:1], av[:, :, 1:RW], ALU.mult, ALU.add),
                   r=[kp + "z", kp + "acc", "cw"], w=[kp + "acc"])
                op("dve", lambda e, zv=zv, av=av, k=k, RW=RW: e.scalar_tensor_tensor(av[:, :, 0:RW - 1], zv[:, :, 1:RW], cw3[:, k, 2:3], av[:, :, 0:RW - 1], ALU.mult, ALU.add),
                   r=[kp + "z", kp + "acc", "cw"], w=[kp + "acc"])
                op("dve", lambda e, T=T, b0=b0, W=W: e.tensor_tensor(T["y"][:, 0:W], bank(b0)[:, 0:W], T["acc"][:, 0:W], ALU.mult),
                   r=[PSK(b0), kp + "acc"], w=[kp + "y"])
                headnorm_store(T["y"], W, gc[:, k:k + 1], k, tok0, (T["sq"], T["ms"], T["sd"], T["yn"]), it)
                it += 1
        for cq in range(4):
            ws = wsl[cq % 2][0]
            dma("pool", ws, w_in_t[l, 36 + cq], w=[("win", cq % 2, 0)])
            for (tok0, W, RW) in tiles_for(True):
                b0 = 4 * (it % 2)
                for kk in range(KC):
                    op("pe", lambda e, kk=kk, b0=b0, ws=ws, tok0=tok0, W=W: e.matmul(
                        bank(b0)[:, 0:W], ws[:, kk, :], hT[:, kk, tok0:tok0 + W], start=(kk == 0), stop=(kk == KC - 1)),
                       r=[("win", cq % 2, 0)] + [("hT", tt) for tt in range(tok0 // P, (tok0 + W) // P)], w=[PSK(b0)])
                op("act", lambda e, b0=b0, cq=cq, tok0=tok0, W=W: e.copy(uT[:, cq, tok0:tok0 + W], bank(b0)[:, 0:W]),
                   r=[PSK(b0)], w=[("uT", cq, tok0)])
                it += 1
        S.barrier()
        A.release(m)

    def reduce_angle(dst, kd, x, kx, n, tf, ti):
        op("dve", lambda e: e.tensor_scalar(tf, x, 1.0 / TWO_PI, None, ALU.mult), r=[kx], w=["ra_tf"])
        op("dve", lambda e: e.tensor_copy(ti, tf), r=["ra_tf"], w=["ra_ti"])
        op("dve", lambda e: e.tensor_copy(tf, ti), r=["ra_ti"], w=["ra_tf"])
        op("dve", lambda e: e.scalar_tensor_tensor(dst, tf, -TWO_PI, x, ALU.mult, ALU.add), r=["ra_tf", kx], w=[kd])
        op("dve", lambda e: e.tensor_scalar(tf, dst, math.pi, -TWO_PI, ALU.is_gt, ALU.mult), r=[kd], w=["ra_tf"])
        op("dve", lambda e: e.tensor_tensor(dst, dst, tf, ALU.add), r=["ra_tf", kd], w=[kd])
        op("dve", lambda e: e.tensor_scalar(tf, dst, -math.pi, TWO_PI, ALU.is_lt, ALU.mult), r=[kd], w=["ra_tf"])
        op("dve", lambda e: e.tensor_tensor(dst, dst, tf, ALU.add), r=["ra_tf", kd], w=[kd])
        op("dve", lambda e: e.tensor_scalar(dst, dst, PI_LO, -PI_LO, ALU.min, ALU.max), r=[kd], w=[kd])

    def sincos(sin_out, ks, cos_out, kc, x, kx, n, tf, ti, xs, red, neg_sin=False):
        reduce_angle(red, "ra_red", x, kx, n, tf, ti)
        op("act", lambda e: e.activation(sin_out, red, AF.Sin, scale=(-1.0 if neg_sin else 1.0)), r=["ra_red"], w=[ks])
        op("dve", lambda e: e.tensor_scalar(xs, x, math.pi / 2, None, ALU.add), r=[kx], w=["ra_xs"])
        reduce_angle(red, "ra_red", xs, "ra_xs", n, tf, ti)
        op("act", lambda e: e.activation(cos_out, red, AF.Sin), r=["ra_red"], w=[kc])

    def phase_b2(l, ctx_out, uT):
        m = A.mark()
        ysum = v3(A.f32(4 * NTOK), NTOK)
        NT = 16 * P
        cosT = A.f32(NT); nsinT = A.f32(NT); RKre = A.f32(NT); RKim = A.f32(NT); MAG0 = A.f32(NT)
        BTre = v3(A.bf16(NT), P); BTim = v3(A.bf16(NT), P); CTre = v3(A.bf16(NT), P); CTim = v3(A.bf16(NT), P)
        small = [A.f32(16) for _ in range(26)]
        (lr, li, ldt, dt, lrdt, th, mag, s1, c1, ar, ai, nr, den, kr, ki, thr, tA, tB, Ere, Eim, th128, s128, c128, tC, tD, tE) = small
        HW = 8 * P
        QW = 4 * P
        WS = []
        big = A.f32(4 * 6 * QW)
        for q_ in range(4):
            o_ = q_ * 6 * QW
            WS.append(dict(t1=big[:, o_:o_ + QW], t2=big[:, o_ + QW:o_ + 2 * QW], t3=big[:, o_ + 2 * QW:o_ + 3 * QW],
                           t4=big[:, o_ + 3 * QW:o_ + 4 * QW], gre=big[:, o_ + 4 * QW:o_ + 5 * QW], gim=big[:, o_ + 5 * QW:o_ + 6 * QW],
                           hre=A.bf16(QW), nhim=A.bf16(QW), ytmp=A.f32(P),
                           carry=[A.f32(4), A.f32(4)], ctmp=[A.f32(4) for _ in range(4)]))
        wkA = big[:, 0:NT]; wkB = big[:, NT:2 * NT]; wkC = big[:, 2 * NT:3 * NT]; gg = big[:, 3 * NT:4 * NT]

        def sm(o, a, b, alu, keys_r, key_w):
            op("dve", lambda e: e.tensor_tensor(o, a, b, alu), r=keys_r, w=[key_w])

        for d in range(2):
            dma("sp", lr, lamre[l, d], w=["lr"])
            dma("sp", li, lamim[l, d], w=["li"])
            dma("sp", ldt, logdt[l, d], w=["ldt"])
            for (dst, src) in ((BTre, bt_re), (BTim, bt_im), (CTre, ct_re), (CTim, ct_im)):
                dma("pool", dst.rearrange("p (c j) q -> p c (j q)", c=4), src[l, d].rearrange("c p x -> p c x"), w=["BC"])
            op("act", lambda e: e.activation(dt, ldt, AF.Exp), r=["ldt"], w=["dt"])
            sm(lrdt, lr, dt, ALU.mult, ["lr", "dt"], "lrdt")
            sm(th, li, dt, ALU.mult, ["li", "dt"], "th")
            op("act", lambda e: e.activation(mag, lrdt, AF.Exp), r=["lrdt"], w=["mag"])
            tBi = tB.bitcast(I32)
            sincos(s1, "s1", c1, "c1", th, "th", 16, tA, tBi, tC, tD)
            reduce_angle(thr, "thr", th, "th", 16, tA, tBi)
            sm(ar, mag, c1, ALU.mult, ["mag", "c1"], "ar")
            sm(ai, mag, s1, ALU.mult, ["mag", "s1"], "ai")
            op("dve", lambda e: e.tensor_scalar(nr, ar, -1.0, None, ALU.add), r=["ar"], w=["nr"])
            sm(den, lr, lr, ALU.mult, ["lr"], "den")
            sm(tE, li, li, ALU.mult, ["li"], "tE")
            sm(den, den, tE, ALU.add, ["den", "tE"], "den")
            op("dve", lambda e: e.reciprocal(den, den), r=["den"], w=["den"])
            sm(kr, nr, lr, ALU.mult, ["nr", "lr"], "kr")
            sm(tE, ai, li, ALU.mult, ["ai", "li"], "tE")
            sm(kr, kr, tE, ALU.add, ["kr", "tE"], "kr")
            sm(kr, kr, den, ALU.mult, ["kr", "den"], "kr")
            sm(ki, ai, lr, ALU.mult, ["ai", "lr"], "ki")
            sm(tE, nr, li, ALU.mult, ["nr", "li"], "tE")
            sm(ki, ki, tE, ALU.subtract, ["ki", "tE"], "ki")
            sm(ki, ki, den, ALU.mult, ["ki", "den"], "ki")
            op("dve", lambda e: e.tensor_scalar(th128, thr, 128.0, None, ALU.mult), r=["thr"], w=["th128"])
            sincos(s128, "s128", c128, "c128", th128, "th128", 16, tA, tBi, tC, tD)
            sm(Ere, mag, c128, ALU.mult, ["mag", "c128"], "Ere")
            sm(Eim, mag, s128, ALU.mult, ["mag", "s128"], "Eim")
            op("dve", lambda e: e.tensor_tensor(v3(wkA, P), thr.unsqueeze(2).to_broadcast([P, 16, P]), iota_t.unsqueeze(1).to_broadcast([P, 16, P]), ALU.mult),
               r=["thr"], w=["ang"])
            sincos(nsinT, "nsinT", cosT, "cosT", wkA, "ang", NT, wkB, gg.bitcast(I32), wkC, MAG0, neg_sin=True)
            krb = kr.unsqueeze(2).to_broadcast([P, 16, P]); kib = ki.unsqueeze(2).to_broadcast([P, 16, P])
            op("dve", lambda e: e.tensor_tensor(v3(RKre, P), v3(cosT, P), krb, ALU.mult), r=["cosT", "kr"], w=["RKre"])
            op("dve", lambda e: e.tensor_tensor(v3(wkA, P), v3(nsinT, P), kib, ALU.mult), r=["nsinT", "ki", "ang"], w=["ang"])
            op("dve", lambda e: e.tensor_tensor(RKre, RKre, wkA, ALU.subtract), r=["RKre", "ang"], w=["RKre"])
            op("dve", lambda e: e.tensor_tensor(v3(RKim, P), v3(cosT, P), kib, ALU.mult), r=["cosT", "ki"], w=["RKim"])
            op("dve", lambda e: e.tensor_tensor(v3(wkA, P), v3(nsinT, P), krb, ALU.mult), r=["nsinT", "kr", "ang", "RKre"], w=["ang"])
            op("dve", lambda e: e.tensor_tensor(RKim, RKim, wkA, ALU.add), r=["RKim", "ang"], w=["RKim"])
            op("dve", lambda e: e.tensor_copy(v3(MAG0, P), mag.unsqueeze(2).to_broadcast([P, 16, P])), r=["mag", "ra_red"], w=["MAG0"])
            op("dve", lambda e: e.memset(v3(MAG0, P)[:, :, 0:1], 0.0), r=["MAG0"], w=["MAG0"])
            S.barrier()

            if d == 0:
                order = list(range(18))
            else:
                order = [1, 0] + list(range(17, 1, -1))
            pending = {}

            def KQ(n, q):
                return (n, q)

            _dv = {"u1", "u2", "u4"}

            def EN(nm):
                return "dve" if nm in _dv else "pool"

            def S1(c, q):
                tsl = slice(c * P, (c + 1) * P)
                rhs = uT[:, q, tsl] if d == 0 else uT[:, q, tsl][:, ::-1]
                for jl in range(4):
                    j = 4 * q + jl
                    op("pe", lambda e, jl=jl, j=j, rhs=rhs, q=q: e.matmul(bank(2 * q)[:, jl * P:(jl + 1) * P], BTre[:, j, :], rhs, start=True, stop=True),
                       r=["BC"], w=[PSK(2 * q)])
                    op("pe", lambda e, jl=jl, j=j, rhs=rhs, q=q: e.matmul(bank(2 * q + 1)[:, jl * P:(jl + 1) * P], BTim[:, j, :], rhs, start=True, stop=True),
                       r=["BC"], w=[PSK(2 * q + 1)])

            def S2(c, q, first):
                W_ = WS[q]
                t1 = W_["t1"]; t2 = W_["t2"]; t3 = W_["t3"]; t4 = W_["t4"]
                fs = slice(q * QW, (q + 1) * QW)
                if q in pending:
                    cq_, tsl_ = pending.pop(q)
                    op("pool", lambda e, W_=W_, cq_=cq_, tsl_=tsl_: e.tensor_tensor(ysum[:, cq_, tsl_], ysum[:, cq_, tsl_], W_["ytmp"], ALU.add),
                       r=[KQ("ytmp", q)], w=[("ys", cq_, tsl_.start)])
                op("dve", lambda e, t1=t1, fs=fs, q=q: e.tensor_tensor(t1, RKre[:, fs], bank(2 * q), ALU.mult), r=[PSK(2 * q)], w=[KQ("t1", q)])
                op("dve", lambda e, t3=t3, fs=fs, q=q: e.tensor_tensor(t3, RKre[:, fs], bank(2 * q + 1), ALU.mult), r=[PSK(2 * q + 1)], w=[KQ("t3", q)])
                op("dve", lambda e, t4=t4, fs=fs, q=q: e.tensor_tensor(t4, RKim[:, fs], bank(2 * q), ALU.mult), r=[PSK(2 * q)], w=[KQ("t4", q)])
                op("dve", lambda e, t2=t2, fs=fs, q=q: e.tensor_tensor(t2, RKim[:, fs], bank(2 * q + 1), ALU.mult), r=[PSK(2 * q + 1)], w=[KQ("t2", q)])
                op(EN("br"), lambda e, t1=t1, t2=t2: e.tensor_tensor(t1, t1, t2, ALU.subtract), r=[KQ("t1", q), KQ("t2", q)], w=[KQ("t1", q)])
                op("dve", lambda e, t3=t3, t4=t4: e.tensor_tensor(t3, t3, t4, ALU.add), r=[KQ("t3", q), KQ("t4", q)], w=[KQ("t3", q)])
                if not first:
                    br3 = v3(t1, P); bi3 = v3(t3, P)
                    op("pool", lambda e, W_=W_, br3=br3: e.tensor_tensor(br3[:, :, 0:1], br3[:, :, 0:1], W_["carry"][0].unsqueeze(2), ALU.add),
                       r=[KQ("t1", q), KQ("carry", q)], w=[KQ("t1", q)])
                    op("pool", lambda e, W_=W_, bi3=bi3: e.tensor_tensor(bi3[:, :, 0:1], bi3[:, :, 0:1], W_["carry"][1].unsqueeze(2), ALU.add),
                       r=[KQ("t3", q), KQ("carry", q)], w=[KQ("t3", q)])

            def S3(c, q):
                W_ = WS[q]
                t1 = W_["t1"]; t3 = W_["t3"]; gre = W_["gre"]; gim = W_["gim"]
                fs = slice(q * QW, (q + 1) * QW)
                js = slice(4 * q, 4 * q + 4)
                op("dve", lambda e, fs=fs, gre=gre, t1=t1: e.tensor_tensor_scan(gre, MAG0[:, fs], t1, 0.0, ALU.mult, ALU.add), r=[KQ("t1", q)], w=[KQ("gre", q)])
                op("dve", lambda e, fs=fs, gim=gim, t3=t3: e.tensor_tensor_scan(gim, MAG0[:, fs], t3, 0.0, ALU.mult, ALU.add), r=[KQ("t3", q)], w=[KQ("gim", q)])
                glr = v3(gre, P)[:, :, P - 1]; gli = v3(gim, P)[:, :, P - 1]
                ct_ = W_["ctmp"]; cy_ = W_["carry"]
                op("pool", lambda e, js=js, glr=glr, ct_=ct_: e.tensor_tensor(ct_[0], Ere[:, js], glr, ALU.mult), r=[KQ("gre", q)], w=[KQ("c0", q)])
                op("pool", lambda e, js=js, gli=gli, ct_=ct_: e.tensor_tensor(ct_[1], Eim[:, js], gli, ALU.mult), r=[KQ("gim", q)], w=[KQ("c1", q)])
                op("pool", lambda e, cy_=cy_, ct_=ct_: e.tensor_tensor(cy_[0], ct_[0], ct_[1], ALU.subtract), r=[KQ("c0", q), KQ("c1", q)], w=[KQ("carry", q)])
                op("pool", lambda e, js=js, gli=gli, ct_=ct_: e.tensor_tensor(ct_[2], Ere[:, js], gli, ALU.mult), r=[KQ("gim", q)], w=[KQ("c2", q)])
                op("pool", lambda e, js=js, glr=glr, ct_=ct_: e.tensor_tensor(ct_[3], Eim[:, js], glr, ALU.mult), r=[KQ("gre", q)], w=[KQ("c3", q)])
                op("pool", lambda e, cy_=cy_, ct_=ct_: e.tensor_tensor(cy_[1], ct_[2], ct_[3], ALU.add), r=[KQ("c2", q), KQ("c3", q), KQ("carry", q)], w=[KQ("carry", q)])

            def S4(c, q):
                W_ = WS[q]
                t1 = W_["t1"]; t2 = W_["t2"]; t3 = W_["t3"]; t4 = W_["t4"]
                gre = W_["gre"]; gim = W_["gim"]; hre = W_["hre"]; nhim = W_["nhim"]
                fs = slice(q * QW, (q + 1) * QW)
                op(EN("u1"), lambda e, fs=fs, t1=t1, gre=gre: e.tensor_tensor(t1, cosT[:, fs], gre, ALU.mult), r=[KQ("gre", q)], w=[KQ("t1", q)])
                op(EN("u2"), lambda e, fs=fs, t2=t2, gim=gim: e.tensor_tensor(t2, nsinT[:, fs], gim, ALU.mult), r=[KQ("gim", q)], w=[KQ("t2", q)])
                op("dve", lambda e, fs=fs, t3=t3, gre=gre: e.tensor_tensor(t3, nsinT[:, fs], gre, ALU.mult), r=[KQ("gre", q)], w=[KQ("t3", q)])
                op(EN("u4"), lambda e, fs=fs, t4=t4, gim=gim: e.tensor_tensor(t4, cosT[:, fs], gim, ALU.mult), r=[KQ("gim", q)], w=[KQ("t4", q)])
                op("dve", lambda e, hre=hre, t1=t1, t2=t2: e.tensor_tensor(hre, t1, t2, ALU.add), r=[KQ("t1", q), KQ("t2", q)], w=[KQ("hre", q)])
                op("dve", lambda e, nhim=nhim, t3=t3, t4=t4: e.tensor_tensor(nhim, t3, t4, ALU.subtract), r=[KQ("t3", q), KQ("t4", q)], w=[KQ("nhim", q)])

            def S5(c, q):
                W_ = WS[q]
                tsl = slice(c * P, (c + 1) * P)
                hre3 = v3(W_["hre"], P); nhim3 = v3(W_["nhim"], P)
                pb = 2 * q
                for j4 in range(4):
                    j = q * 4 + j4
                    op("pe", lambda e, j=j, j4=j4, pb=pb, hre3=hre3, d_=d: e.matmul(bank(pb)[:, 0:P], CTre[:, j, :], (hre3[:, j4, :] if d_ == 0 else hre3[:, j4, :][:, ::-1]), start=(j4 == 0), stop=False),
                       r=["BC", KQ("hre", q)], w=[PSK(pb)])
                    op("pe", lambda e, j=j, j4=j4, pb=pb, nhim3=nhim3, d_=d: e.matmul(bank(pb)[:, 0:P], CTim[:, j, :], (nhim3[:, j4, :] if d_ == 0 else nhim3[:, j4, :][:, ::-1]), start=False, stop=(j4 == 3)),
                       r=["BC", KQ("nhim", q)], w=[PSK(pb)])
                if d == 0:
                    op("act", lambda e, q=q, pb=pb, tsl=tsl: e.copy(ysum[:, q, tsl], bank(pb)[:, 0:P]), r=[PSK(pb)], w=[("ys", q, tsl.start)])
                else:
                    op("act", lambda e, W_=W_, pb=pb: e.copy(W_["ytmp"], bank(pb)[:, 0:P]), r=[PSK(pb)], w=[KQ("ytmp", q)])
                    pending[q] = (q, tsl)

            pairs = [(0, 1), (2, 3)]
            n_ = len(order)
            for pr in pairs:
                for q in pr:
                    S1(order[0], q)
            for i, c in enumerate(order):
                need_out = ctx_out or c >= 2
                for pr in pairs:
                    for q in pr:
                        S2(c, q, i == 0)
                    for q in pr:
                        S3(c, q)
                    if need_out:
                        for q in pr:
                            S4(c, q)
                        for q in pr:
                            S5(c, q)
                    if i + 1 < n_:
                        for q in pr:
                            S1(order[i + 1], q)
            for q in list(pending.keys()):
                cq_, tsl_ = pending.pop(q)
                op("pool", lambda e, q=q, cq_=cq_, tsl_=tsl_: e.tensor_tensor(ysum[:, cq_, tsl_], ysum[:, cq_, tsl_], WS[q]["ytmp"], ALU.add),
                   r=[KQ("ytmp", q)], w=[("ys", cq_, tsl_.start)])
            S.barrier()

        tokr = slice(0, NTOK) if ctx_out else slice(NCTX, NTOK)
        NTK = NTOK if ctx_out else NLAT
        A.release(m)
        ysum = v3(A.f32(4 * NTOK), NTOK)
        dcc = A.f32(4); gsc = A.f32(4)
        dma("sp", dcc, dcol[l], w=["dc"])
        dma("sp", gsc, gssm[l], w=["gcols"])
        wg = v3(A.bf16(4 * 512), 512)
        dma("pool", wg, w_glu[l].rearrange("(k p) n -> p k n", p=P), w=["wg"])
        gl16 = v3(A.bf16(4 * NTOK), NTOK)
        w1 = A.f32(NTOK); w2 = A.f32(NTOK)
        for cq in range(4):
            yv = ysum[:, cq, tokr]
            op("dve", lambda e, cq=cq, yv=yv: e.scalar_tensor_tensor(yv, uT[:, cq, tokr], dcc[:, cq:cq + 1], yv, ALU.mult, ALU.add),
               r=["dc", "uTall"], w=[("yt", cq)])
            if "dbg_y" in dbg:
                dma("sp", dbg_y[cq], ysum[:, cq, :], r=[("yt", cq)])
            op("pool", lambda e, yv=yv: e.tensor_tensor(w1[:, 0:NTK], yv, yv, ALU.mult), r=[("yt", cq)], w=["w1"])
            op("dve", lambda e: e.tensor_scalar(w1[:, 0:NTK], w1[:, 0:NTK], 0.044715, 1.0, ALU.mult, ALU.add), r=["w1"], w=["w1"])
            op("pool", lambda e, yv=yv: e.tensor_tensor(w2[:, 0:NTK], w1[:, 0:NTK], yv, ALU.mult), r=["w1", ("yt", cq)], w=["w2"])
            op("act", lambda e: e.activation(w1[:, 0:NTK], w2[:, 0:NTK], AF.Sigmoid, scale=1.5957691216057308), r=["w2", "w1"], w=["w1"])
            op("dve", lambda e, yv=yv: e.tensor_tensor(yv, yv, w1[:, 0:NTK], ALU.mult), r=["w1", ("yt", cq)], w=[("yt", cq)])
            op("act", lambda e, cq=cq, yv=yv: e.copy(gl16[:, cq, tokr], yv), r=[("yt", cq)], w=[("gl16", cq)])
        tmp = []
        for i in range(2):
            tmp.append(dict(sg=A.f32(512), o=A.f32(512), sq=A.bf16(512), ms=A.f32(512), sd=A.f32(512), yn=A.bf16(512)))
        it = 0
        for oc in range(4):
            for (tok0, W, RW) in tiles_for(ctx_out):
                T = tmp[it % 2]
                b0 = 4 * (it % 2)
                kp = "hn%d" % (it % 2)
                for k in range(4):
                    op("pe", lambda e, k=k, oc=oc, b0=b0, tok0=tok0, W=W: e.matmul(bank(b0)[:, 0:W], wg[:, k, oc * P:(oc + 1) * P], gl16[:, k, tok0:tok0 + W], start=(k == 0), stop=(k == 3)),
                       r=["wg"] + [("gl16", kk) for kk in range(4)], w=[PSK(b0)])
                op("act", lambda e, T=T, b0=b0, W=W: e.activation(T["sg"][:, 0:W], bank(b0)[:, 0:W], AF.Sigmoid), r=[PSK(b0)], w=[kp + "sg"])
                op("dve", lambda e, T=T, oc=oc, tok0=tok0, W=W: e.tensor_tensor(T["o"][:, 0:W], ysum[:, oc, tok0:tok0 + W], T["sg"][:, 0:W], ALU.mult),
                   r=[kp + "sg", ("yt", oc)], w=[kp + "y"])
                headnorm_store(T["o"], W, gsc[:, oc:oc + 1], 12 + oc, tok0, (T["sq"], T["ms"], T["sd"], T["yn"]), it)
                it += 1
        S.barrier()
        A.release(m)

    def phase_b3(l, ctx_full):
        m = A.mark()
        wo = v3(A.bf16(KC * D), D)
        for cg in range(4):
            dma("pool", wo[:, :, cg * 512:(cg + 1) * 512], w_out[l, cg].rearrange("p (k n) -> p k n", n=512), w=[("wo", cg)])
        g1 = [A.f32(D) for _ in range(2)]
        for r in range(2):
            load_mod_bc(g1[r], r, 2, ("g1", r))
        mts = [v3(A.bf16(KC * 512), 512) for _ in range(2)]
        xs = [A.f32(D) for _ in range(2)]
        xo = [A.f32(D) for _ in range(2)]
        tq = A.f32(512)
        t_list = list(range(0 if ctx_full else 2, 18))
        loaded = {}
        nload = 0
        for idx_t, t in enumerate(t_list):
            s = idx_t % 2
            r = 1 if t < 2 else 0
            grp = -1 if t < 2 else (t - 2) // 4
            if grp not in loaded:
                ms_ = nload % 2
                nload += 1
                tok0, W = (0, 256) if grp < 0 else (256 + grp * 512, 512)
                dma("sp", mts[ms_][:, :, 0:W], mixn[:, :, tok0:tok0 + W].rearrange("c p t -> p c t"), w=[("mt", ms_)])
                loaded[grp] = (ms_, tok0)
            ms_, tok0 = loaded[grp]
            lo = t * P - tok0
            dma("sp", xs[s], tok_src(l, t), w=[("x", s)])
            for cg in range(4):
                pb = (idx_t * 4 + cg) % 8
                cs = slice(cg * 512, (cg + 1) * 512)
                for k in range(KC):
                    op("pe", lambda e, k=k, ms_=ms_, lo=lo, cs=cs, pb=pb: e.matmul(bank(pb), mts[ms_][:, k, lo:lo + P], wo[:, k, cs], start=(k == 0), stop=(k == KC - 1)),
                       r=[("mt", ms_), ("wo", cg)], w=[PSK(pb)])
                op("dve", lambda e, pb=pb, r=r, cs=cs: e.tensor_tensor(tq, bank(pb), g1[r][:, cs], ALU.mult), r=[PSK(pb), ("g1", r)], w=["tq"])
                op("pool", lambda e, s=s, cs=cs: e.tensor_tensor(xo[s][:, cs], tq, xs[s][:, cs], ALU.add), r=["tq", ("x", s)], w=[("xo", s)])
            dma("sp", tok_dst(t), xo[s], r=[("xo", s)])
        S.barrier()
        A.release(m)

    def phase_c0(l, ctx_full, affT):
        m = A.mark()
        gm = [A.f32(D) for _ in range(2)]
        sh = [A.f32(D) for _ in range(2)]
        gtmp = A.f32(D)
        bc_row(gtmp, norm2_g[l:l + 1, :], "g2row")
        for r in range(2):
            load_mod_bc(gm[r], r, 4, ("gm", r))
            load_mod_bc(sh[r], r, 3, ("sh", r))
            op("dve", lambda e, r=r: e.scalar_tensor_tensor(gm[r], gm[r], 1.0, gtmp, ALU.add, ALU.mult),
               r=["g2row", ("gm", r)], w=[("gm", r)])
        rw = v3(A.f32(KC * NE), NE)
        dma("sp", rw, router_w[l].rearrange("(k p) n -> p k n", p=P), w=["rw"])
        xs = [A.f32(D) for _ in range(2)]
        hn = A.f32(D)
        h2f = [A.f32(D) for _ in range(2)]
        h2b = [A.bf16(D) for _ in range(2)]
        h2T = v3(A.f32(D), P)
        junk = A.bf16(D)
        ss = A.f32(2); t1 = A.f32(2); t2 = A.f32(2); rstd = A.f32(2)
        mx = A.f32(2); se = A.f32(2); ex = A.f32(NE); aff = A.f32(NE)
        t_list = list(range(0 if ctx_full else 2, 18))
        for it, t in enumerate(t_list):
            s = it % 2
            r = 1 if t < 2 else 0
            dma("sp", xs[s], tok_dst(t), w=[("x", s)])
            op("act", lambda e, s=s: e.activation(junk, xs[s], AF.Square, accum_out=ss[:, 0:1]), r=[("x", s)], w=["junk", "c0ss"])
            rstd_from_ss(ss[:, 0:1], D, t1[:, 0:1], t2[:, 0:1], rstd[:, 0:1], "c0")
            op("dve", lambda e, s=s, r=r: e.scalar_tensor_tensor(hn, xs[s], rstd[:, 0:1], gm[r], ALU.mult, ALU.mult),
               r=[("x", s), "c0rstd", ("gm", r)], w=["hn"])
            op("pool", lambda e, s=s, r=r: e.tensor_tensor(h2f[s], hn, sh[r], ALU.add), r=["hn", ("sh", r)], w=[("h2f", s)])
            op("act", lambda e, s=s: e.copy(h2b[s], h2f[s]), r=[("h2f", s)], w=[("h2b", s)])
            dst = h2c[t * P:(t + 1) * P, :] if t < 2 else h2t[(t - 2) * P:(t - 1) * P, :]
            dma("sp", dst, h2b[s], r=[("h2b", s)])
            for k4 in range(4):
                bk = k4
                for kk in range(4):
                    k = k4 * 4 + kk
                    op("pe", lambda e, s=s, k=k, kk=kk, bk=bk: e.transpose(bank(bk)[:, kk * P:(kk + 1) * P], h2f[s][:, k * P:(k + 1) * P], ident_f),
                       r=[("h2f", s), "ident_f"], w=[PSK(bk)])
                if k4 % 2 == 0:
                    op("act", lambda e, k4=k4, bk=bk: e.copy(h2T[:, k4 * 4:k4 * 4 + 4, :], v3(bank(bk), P)), r=[PSK(bk)], w=[("h2T", k4)])
                else:
                    op("dve", lambda e, k4=k4, bk=bk: e.tensor_copy(h2T[:, k4 * 4:k4 * 4 + 4, :], v3(bank(bk), P)), r=[PSK(bk)], w=[("h2T", k4)])
            pl = 4 + (it % 2)
            for k in range(KC):
                op("pe", lambda e, k=k, pl=pl: e.matmul(bank(pl)[:, 0:NE], h2T[:, k, :], rw[:, k, :], start=(k == 0), stop=(k == KC - 1)),
                   r=[("h2T", k // 4), "rw"], w=[PSK(pl)])
            op("dve", lambda e, pl=pl: e.reduce_max(mx[:, 0:1], bank(pl)[:, 0:NE], AX.X), r=[PSK(pl)], w=["mx"])
            op("dve", lambda e: e.tensor_scalar(mx[:, 0:1], mx[:, 0:1], -1.0, None, ALU.mult), r=["mx"], w=["mx"])
            op("act", lambda e, pl=pl: e.activation(ex, bank(pl)[:, 0:NE], AF.Exp, bias=mx[:, 0:1], accum_out=se[:, 0:1]), r=[PSK(pl), "mx"], w=["ex", "se"])
            op("dve", lambda e: e.reciprocal(se[:, 0:1], se[:, 0:1]), r=["se"], w=["se"])
            op("dve", lambda e: e.tensor_scalar(aff, ex, se[:, 0:1], None, ALU.mult), r=["ex", "se"], w=["aff"])
            pt = 6 + (it % 2)
            op("pe", lambda e, pt=pt: e.transpose(bank(pt)[0:NE, 0:P], aff, ident_f), r=["aff", "ident_f"], w=[PSK(pt)])
            op("act", lambda e, pt=pt, t=t: e.copy(affT[0:NE, t * P:(t + 1) * P], bank(pt)[0:NE, 0:P]), r=[PSK(pt)], w=[("affT", t)])
        if "dbg_aff" in dbg:
            dma("sp", dbg_aff, affT[0:NE, :], r=[("affT", t) for t in t_list])
        S.barrier()
        A.release(m)

    def phase_c1(ctx_full, affT, idxT, valT):
        m = A.mark()
        work = A.f32(NLAT)
        vals = A.f32(288)
        idx = A.u32(288)
        idxf = A.f32(288)
        op("dve", lambda e: e.tensor_copy(work[0:NE, :], affT[0:NE, NCTX:NTOK]), w=["work"])
        for r_ in range(32):
            vs = slice(r_ * 8, r_ * 8 + 8)
            op("dve", lambda e, vs=vs: e.max(vals[0:NE, vs], work[0:NE, :]), r=["work"], w=["vals"])
            op("dve", lambda e, vs=vs: e.max_index(idx[0:NE, vs], vals[0:NE, vs], work[0:NE, :]), r=["work", "vals"], w=["idx"])
            op("dve", lambda e, vs=vs: e.match_replace(work[0:NE, :], vals[0:NE, vs], work[0:NE, :], -1.0), r=["work", "vals", "idx"], w=["work"])
        if ctx_full:
            wc = work[0:NE, 0:NCTX]
            op("dve", lambda e: e.tensor_copy(wc, affT[0:NE, 0:NCTX]), r=["work"], w=["work"])
            for r_ in range(4):
                vs = slice(256 + r_ * 8, 256 + r_ * 8 + 8)
                op("dve", lambda e, vs=vs: e.max(vals[0:NE, vs], wc), r=["work"], w=["vals"])
                op("dve", lambda e, vs=vs: e.max_index(idx[0:NE, vs], vals[0:NE, vs], wc), r=["work", "vals"], w=["idx"])
                op("dve", lambda e, vs=vs: e.match_replace(wc, vals[0:NE, vs], wc, -1.0), r=["work", "vals", "idx"], w=["work"])
        NS = 288 if ctx_full else 256
        op("dve", lambda e: e.tensor_copy(idxf[0:NE, 0:NS], idx[0:NE, 0:NS]), r=["idx"], w=["idxf"])
        chunks = [(0, 128), (128, 128)] + ([(256, 32)] if ctx_full else [])
        for ci, (c0, cn) in enumerate(chunks):
            op("pe", lambda e, c0=c0, cn=cn, ci=ci: e.transpose(bank(ci)[0:cn, 0:NE], idxf[0:NE, c0:c0 + cn], ident_f[0:NE, 0:NE]), r=["idxf", "ident_f"], w=[PSK(ci)])
            op("dve", lambda e, cn=cn, ci=ci: e.tensor_copy(idxT[ci][0:cn, :], bank(ci)[0:cn, 0:NE]), r=[PSK(ci)], w=[("idxT", ci)])
            op("pe", lambda e, c0=c0, cn=cn, ci=ci: e.transpose(bank(4 + ci)[0:cn, 0:NE], vals[0:NE, c0:c0 + cn], ident_f[0:NE, 0:NE]), r=["vals", "ident_f"], w=[PSK(4 + ci)])
            op("act", lambda e, cn=cn, ci=ci: e.copy(valT[ci][0:cn, :], bank(4 + ci)[0:cn, 0:NE]), r=[PSK(4 + ci)], w=[("valT", ci)])
        S.barrier()
        A.release(m)

    def phase_c2(l, ctx_full, idxT, valT):
        m = A.mark()
        g2 = [A.f32(D) for _ in range(2)]
        for r in range(2):
            load_mod_bc(g2[r], r, 5, ("g2", r))
        NGU = 4
        NDN = 2
        gus = [v3(A.bf16(KC * 512), 512) for _ in range(NGU)]
        dns = [v3(A.bf16(KC * 512), 512) for _ in range(NDN)]
        xs = v3(A.bf16(3 * D), D)
        NS = 288 if ctx_full else 256
        xsT = v3(A.bf16(KC * 288), 288)
        aT = v3(A.bf16(KC * 288), 288)
        sg = [A.f32(288) for _ in range(2)]
        yst = v3(A.f32(3 * D), D)
        chunks = [(0, 128), (128, 128)] + ([(256, 32)] if ctx_full else [])
        jobs_gu = [(e_, fg, wh) for e_ in range(NE) for fg in range(4) for wh in range(2)]
        jobs_dn = [(e_, cg) for e_ in range(NE) for cg in range(4)]
        st = dict(gu_issued=0, gu_done=0, dn_issued=0, dn_done=0)

        def pump_gu():
            while st["gu_issued"] < len(jobs_gu) and st["gu_issued"] < st["gu_done"] + NGU:
                j = st["gu_issued"]
                e_, fg, wh = jobs_gu[j]
                src = (w_gate if wh == 0 else w_up)[l, e_, fg].rearrange("p (k n) -> p k n", n=512)
                dma("pool", gus[j % NGU], src, w=[("gu", j % NGU)])
                st["gu_issued"] += 1

        def pump_dn():
            while st["dn_issued"] < len(jobs_dn) and st["dn_issued"] < st["dn_done"] + NDN:
                j = st["dn_issued"]
                e_, cg = jobs_dn[j]
                src = w_down[l, e_, cg].rearrange("p (k n) -> p k n", n=512)
                dma("pool", dns[j % NDN], src, w=[("dn", j % NDN)])
                st["dn_issued"] += 1

        def gather(e_):
            for ci, (c0, cn) in enumerate(chunks):
                srcd = h2t if ci < 2 else h2c
                op("pool", lambda e, ci=ci, cn=cn, srcd=srcd, e_=e_: e.indirect_dma_start(
                    out=xs[0:cn, ci, :], out_offset=None, in_=srcd,
                    in_offset=bass.IndirectOffsetOnAxis(ap=idxT[ci][0:cn, e_:e_ + 1], axis=0)),
                   r=[("idxT", ci)], w=[("xs", ci)], dma=True)

        def transposeT(e_):
            for ci, (c0, cn) in enumerate(chunks):
                for k4 in range(4):
                    bk = (ci * 4 + k4) % 2
                    for kk in range(4):
                        k = k4 * 4 + kk
                        op("pe", lambda e, ci=ci, cn=cn, k=k, kk=kk, bk=bk: e.transpose(bank16(bk)[:, kk * P:kk * P + cn], xs[0:cn, ci, k * P:(k + 1) * P], ident_b[0:cn, 0:cn]),
                           r=[("xs", ci), "ident_b"], w=[PSK(bk)])
                    src = v3(bank16(bk)[:, 0:512], P)[:, :, 0:cn]
                    dstv = xsT[:, k4 * 4:k4 * 4 + 4, c0:c0 + cn]
                    if k4 % 2 == 0:
                        op("act", lambda e, src=src, dstv=dstv: e.copy(dstv, src), r=[PSK(bk)], w=["xsT"])
                    else:
                        op("dve", lambda e, src=src, dstv=dstv: e.tensor_copy(dstv, src), r=[PSK(bk)], w=["xsT"])

        pump_gu()
        pump_dn()
        gather(0)
        transposeT(0)
        for e_ in range(NE):
            if e_ + 1 < NE:
                gather(e_ + 1)
            for fg in range(4):
                jg = (e_ * 4 + fg) * 2
                sg_ = jg % NGU
                su_ = (jg + 1) % NGU
                for fc in range(4):
                    fcn = fg * 4 + fc
                    pg = 2 + 2 * (fcn % 2)
                    pu = pg + 1
                    for k in range(KC):
                        op("pe", lambda e, k=k, fc=fc, pg=pg, sg_=sg_: e.matmul(bank(pg)[:, 0:NS], gus[sg_][:, k, fc * P:(fc + 1) * P], xsT[:, k, 0:NS], start=(k == 0), stop=(k == KC - 1)),
                           r=[("gu", sg_), "xsT"], w=[PSK(pg)])
                    for k in range(KC):
                        op("pe", lambda e, k=k, fc=fc, pu=pu, su_=su_: e.matmul(bank(pu)[:, 0:NS], gus[su_][:, k, fc * P:(fc + 1) * P], xsT[:, k, 0:NS], start=(k == 0), stop=(k == KC - 1)),
                           r=[("gu", su_), "xsT"], w=[PSK(pu)])
                    sgt = sg[fcn % 2]
                    op("act", lambda e, pg=pg, sgt=sgt: e.activation(sgt[:, 0:NS], bank(pg)[:, 0:NS], AF.Silu), r=[PSK(pg)], w=[("sg", fcn % 2)])
                    op("dve", lambda e, pu=pu, sgt=sgt, fcn=fcn: e.tensor_tensor(aT[:, fcn, 0:NS], sgt[:, 0:NS], bank(pu)[:, 0:NS], ALU.mult),
                       r=[PSK(pu), ("sg", fcn % 2)], w=[("aT", fcn)])
                st["gu_done"] += 2
                pump_gu()
            if e_ + 1 < NE:
                transposeT(e_ + 1)
            for cg in range(4):
                cs = slice(cg * 512, (cg + 1) * 512)
                sd_ = (e_ * 4 + cg) % NDN
                for ci, (c0, cn) in enumerate(chunks):
                    pb = 6 + ((cg * 3 + ci) % 2)
                    r = 1 if ci == 2 else 0
                    for k in range(KC):
                        op("pe", lambda e, k=k, c0=c0, cn=cn, pb=pb, sd_=sd_: e.matmul(bank(pb)[0:cn, :], aT[:, k, c0:c0 + cn], dns[sd_][:, k, :], start=(k == 0), stop=(k == KC - 1)),
                           r=[("dn", sd_)] + [("aT", kk) for kk in range(KC)], w=[PSK(pb)])
                    op("dve", lambda e, ci=ci, cn=cn, pb=pb, r=r, cs=cs, e_=e_: e.scalar_tensor_tensor(yst[0:cn, ci, cs], bank(pb)[0:cn, :], valT[ci][0:cn, e_:e_ + 1], g2[r][0:cn, cs], ALU.mult, ALU.mult),
                       r=[PSK(pb), ("valT", ci), ("g2", r)], w=[("yst", ci)])
                st["dn_done"] += 1
                pump_dn()
            for ci, (c0, cn) in enumerate(chunks):
                dstd = xl if ci < 2 else xc
                op("pool", lambda e, ci=ci, cn=cn, dstd=dstd, e_=e_: e.indirect_dma_start(
                    out=dstd, out_offset=bass.IndirectOffsetOnAxis(ap=idxT[ci][0:cn, e_:e_ + 1], axis=0),
                    in_=yst[0:cn, ci, :], in_offset=None, compute_op=ALU.add),
                   r=[("yst", ci), ("idxT", ci)], w=["xl_scatter" if ci < 2 else "xc_scatter"], dma=True)
        S.barrier()
        A.release(m)

    def phase_final():
        m = A.mark()
        g = A.f32(D)
        bc_row(g, final_g[0:1, :], "fg")
        xs = [A.f32(D) for _ in range(2)]
        xo = [A.f32(D) for _ in range(2)]
        junk = A.bf16(D)
        ss = A.f32(2); t1 = A.f32(2); t2 = A.f32(2); rstd = A.f32(2)
        src = xl if n_layers > 0 else x_in
        for t in range(16):
            s = t % 2
            dma("sp", xs[s], src[t * P:(t + 1) * P, :], w=[("x", s)])
            op("act", lambda e, s=s: e.activation(junk, xs[s], AF.Square, accum_out=ss[:, 0:1]), r=[("x", s)], w=["junk", "fss"])
            rstd_from_ss(ss[:, 0:1], D, t1[:, 0:1], t2[:, 0:1], rstd[:, 0:1], "f")
            op("dve", lambda e, s=s: e.scalar_tensor_tensor(xo[s], xs[s], rstd[:, 0:1], g, ALU.mult, ALU.mult), r=[("x", s), "frstd", "fg"], w=[("xo", s)])
            dma("sp", out[t * P:(t + 1) * P, :], xo[s], r=[("xo", s)])
        S.barrier()
        A.release(m)

    layer_mark = A.mark()

    def _gg(gre, gim):
        return gre
    if only == "b2":
        uT = v3(A.bf16(4 * NTOK), NTOK)
        for cq in range(4):
            op("dve", lambda e, cq=cq: e.memset(uT[:, cq, :], 0.5), w=[("uT", cq)])
        S.barrier()
        phase_b2(0, True, uT)
        n_layers = 0
        stop_after = ("x", 0)
    stages = []
    for l in range(n_layers):
        last = (l == DEPTH - 1)
        ctx_full = not last
        phase_ada(l)
        if stop_after == ("ada", l):
            break
        m0 = A.mark()
        uT = v3(A.bf16(4 * NTOK), NTOK)
        m1 = A.mark()
        hT = v3(A.bf16(KC * NTOK), NTOK)
        phase_b0(l, True, hT)
        phase_b1(l, ctx_full, hT, uT)
        A.release(m1)
        if stop_after == ("b1", l):
            break
        phase_b2(l, ctx_full, uT)
        A.release(m0)
        if stop_after == ("b2", l):
            break
        phase_b3(l, ctx_full)
        if stop_after == ("b3", l):
            break
        mc_ = A.mark()
        idxT = [A.u32(NE) for _ in range(3)]
        valT = [A.f32(NE) for _ in range(3)]
        m_aff = A.mark()
        affT = A.f32(NTOK)
        phase_c0(l, ctx_full, affT)
        phase_c1(ctx_full, affT, idxT, valT)
        A.release(m_aff)
        if stop_after == ("c1", l):
            break
        phase_c2(l, ctx_full, idxT, valT)
        A.release(mc_)
    if stop_after is None:
        phase_final()
    S.barrier()
    S.emit()
    stack.close()
    return nc


def prep_shared(inp):
    f = np.float32
    sh = {}
    def tile_w(w, ncg):
        lead = w.shape[:-2]
        nl = len(lead)
        t = w.reshape(*lead, KC, P, ncg, 512)
        t = t.transpose(*range(nl), nl + 2, nl + 1, nl, nl + 3)
        return np.ascontiguousarray(t, f).reshape(*lead, ncg, P, KC * 512)
    sh["ada_w"] = tile_w(inp["ada_w"], 24)
    sh["ada_b2"] = np.ascontiguousarray(np.repeat(inp["ada_b"][:, None, :], 2, axis=1), f)
    sh["norm1_g"] = np.ascontiguousarray(inp["norm1_g"], f)
    sh["norm2_g"] = np.ascontiguousarray(inp["norm2_g"], f)
    sh["final_g"] = np.ascontiguousarray(inp["final_norm_g"].reshape(1, D), f)
    w_in = inp["w_in"]
    sh["w_in_t"] = np.ascontiguousarray(w_in.reshape(DEPTH, KC, P, 40, P).transpose(0, 3, 2, 1, 4), f)
    cw = inp["conv_w"]
    sh["conv_wc"] = np.ascontiguousarray(cw.reshape(DEPTH, 3, 12, P).transpose(0, 3, 2, 1).reshape(DEPTH, P, 36), f)

    def col16(a):
        return np.ascontiguousarray(a.reshape(DEPTH, 2, 16, 2, 64).transpose(0, 1, 3, 4, 2).reshape(DEPTH, 2, P, 16), f)

    sh["lamre"] = col16(inp["ssm_lam_re"])
    sh["lamim"] = col16(inp["ssm_lam_im"])
    sh["logdt"] = col16(np.repeat(inp["ssm_log_dt"][..., None], 64, axis=-1))

    def bt(b):
        o = np.zeros((DEPTH, 2, 4, P, 4, P), f)
        bb = b.reshape(DEPTH, 2, 4, 4, 2, 64, 16)
        for jl in range(4):
            for gl in range(2):
                o[:, :, :, 32 * jl + 16 * gl:32 * jl + 16 * gl + 16, jl, gl * 64:(gl + 1) * 64] = bb[:, :, :, jl, gl].transpose(0, 1, 2, 4, 3)
        return o.reshape(DEPTH, 2, 4, P, 4 * P)

    def ct(c):
        o = np.zeros((DEPTH, 2, 4, P, 4, P), f)
        cc = c.reshape(DEPTH, 2, 4, 4, 2, 16, 64)
        for jl in range(4):
            for gl in range(2):
                o[:, :, :, gl * 64:(gl + 1) * 64, jl, 32 * jl + 16 * gl:32 * jl + 16 * gl + 16] = cc[:, :, :, jl, gl].transpose(0, 1, 2, 4, 3)
        return o.reshape(DEPTH, 2, 4, P, 4 * P)

    sh["bt_re"] = bt(inp["ssm_b_re"]); sh["bt_im"] = bt(inp["ssm_b_im"])
    sh["ct_re"] = ct(inp["ssm_c_re"]); sh["ct_im"] = ct(inp["ssm_c_im"])
    sh["dcol"] = np.ascontiguousarray(inp["ssm_d"].reshape(DEPTH, 4, P).transpose(0, 2, 1), f)
    sh["w_glu"] = np.ascontiguousarray(inp["ssm_w_glu"], f)
    sh["gconv"] = np.ascontiguousarray(inp["out_norm_conv_g"].reshape(DEPTH, 12, P).transpose(0, 2, 1), f)
    sh["gssm"] = np.ascontiguousarray(inp["out_norm_ssm_g"].reshape(DEPTH, 4, P).transpose(0, 2, 1), f)
    sh["w_out"] = tile_w(inp["w_out"], 4)
    sh["router_w"] = np.ascontiguousarray(inp["router_w"], f)
    sh["w_gate"] = tile_w(inp["exp_w_gate"], 4)
    sh["w_up"] = tile_w(inp["exp_w_up"], 4)
    sh["w_down"] = tile_w(inp["exp_w_down"], 4)
    return sh


def prep_core(inp, b):
    f = np.float32
    d = {}
    d["x"] = np.ascontiguousarray(inp["x"][b], f)
    d["ctx"] = np.ascontiguousarray(inp["ctx"][b], f)
    cc = np.zeros((P, KC, 2), f)
    cc[:, :, 0] = inp["c"][b].reshape(KC, P).T
    cc[:, :, 1] = inp["c_ctx"].reshape(KC, P).T
    d["ccol"] = cc.reshape(P, 32)
    return d


def kernel(**inputs):
    inp = {k: np.asarray(v) for k, v in inputs.items()}
    sh = prep_shared(inp)
    nc = build_program()
    in_maps = []
    for b in range(8):
        m = dict(sh)
        m.update(prep_core(inp, b))
        in_maps.append(m)
    res = run_bass_kernel_spmd(nc, in_maps, core_ids=list(range(8)))
    return np.stack([res.results[b]["out"] for b in range(8)], axis=0).astype(np.float32)
```
